# Optimizing a Trainium2 kernel written in Bass

```python
import math
import jax, jax.numpy as jnp
from jax import lax
import numpy as np

D_MODEL = 1024
BATCH = 8
SEQ = 2048
DEPTH = 4

EPS = 1e-6
HEAD_DIM = 64
ATTN_PATTERNS = ((128, 1), (512, 4), (2048, 16))
N_ATTN_GROUPS = 3
HEADS_PER_GROUP = 8
N_ATTN_HEADS = N_ATTN_GROUPS * HEADS_PER_GROUP
ATTN_WIDTH = N_ATTN_HEADS * HEAD_DIM
ATTN_OUT_WIDTH = HEADS_PER_GROUP * HEAD_DIM
ROPE_THETA = 10000.0
SSD_WIDTH = 2 * D_MODEL
SSD_HEAD_DIM = 64
SSD_HEADS = SSD_WIDTH // SSD_HEAD_DIM
SSD_GROUPS = 8
SSD_HEADS_PER_GROUP = SSD_HEADS // SSD_GROUPS
SSD_STATE = 128
SSD_CHUNK = 128
SSD_CONV = 4
SSD_XBC_WIDTH = SSD_WIDTH + 2 * SSD_GROUPS * SSD_STATE
LRU_BLOCKS = 16
LRU_BLOCK_WIDTH = 80
LRU_WIDTH = LRU_BLOCKS * LRU_BLOCK_WIDTH
LRU_CONV = 4
LRU_C = 8.0
N_BRANCHES = 3
IN_SPLITS = (ATTN_WIDTH, ATTN_WIDTH, ATTN_WIDTH, SSD_WIDTH, SSD_XBC_WIDTH, SSD_HEADS,
             LRU_WIDTH, LRU_WIDTH, N_BRANCHES * D_MODEL)
IN_COLS = sum(IN_SPLITS)
MOE_GROUPS = 4
EXPERTS_PER_GROUP = 8
N_EXPERTS = MOE_GROUPS * EXPERTS_PER_GROUP
TOP_K = 2
EXPERT_FF = 512
MOE_BLOCK = 128

kernel_name = 'hybrid_gated_dilattn_ssd_rglru_hiermoe'


def rmsnorm(x, g):
    xf = x.astype(jnp.float32)
    y = xf * lax.rsqrt(jnp.mean(xf * xf, axis=-1, keepdims=True) + EPS)
    return (y * g.astype(jnp.float32)).astype(x.dtype)


def modulate(h, shift, scale):
    return h * (1 + scale[:, None, :]) + shift[:, None, :]


def rope(t, positions):
    half = t.shape[-1] // 2
    freqs = ROPE_THETA ** (-jnp.arange(half, dtype=jnp.float32) / half)
    ang = positions.astype(jnp.float32)[..., None] * freqs
    cos = jnp.cos(ang)[:, :, None, :]
    sin = jnp.sin(ang)[:, :, None, :]
    t1 = t[..., :half].astype(jnp.float32)
    t2 = t[..., half:].astype(jnp.float32)
    return jnp.concatenate([t1 * cos - t2 * sin, t2 * cos + t1 * sin], axis=-1).astype(t.dtype)


def causal_conv(x, w, b):
    k_w, ch = w.shape
    y = lax.conv_general_dilated(x, w[:, None, :], window_strides=(1,), padding=((k_w - 1, 0),),
                                 dimension_numbers=('NWC', 'WIO', 'NWC'), feature_group_count=ch)
    return y + b


def dilated_window_attention(q, k, v, window, dilation):
    bsz, seq, heads, dh = q.shape
    r = dilation
    band = window // dilation
    sub_len = seq // r
    n_blk = -(-sub_len // band)
    pad_len = n_blk * band - sub_len

    def to_blocks(t):
        t = t.reshape(bsz, sub_len, r, heads, dh).transpose(0, 2, 1, 3, 4)
        t = jnp.pad(t, ((0, 0), (0, 0), (0, pad_len), (0, 0), (0, 0)))
        return t.reshape(bsz, r, n_blk, band, heads, dh)

    def with_prev(t):
        prev = jnp.pad(t, ((0, 0), (0, 0), (1, 0), (0, 0), (0, 0), (0, 0)))[:, :, :-1]
        return jnp.concatenate([prev, t], axis=3)

    qb = to_blocks(q)
    kb = with_prev(to_blocks(k))
    vb = with_prev(to_blocks(v))
    s = jnp.einsum('brnqhd,brnkhd->brnhqk', qb, kb).astype(jnp.float32) * (dh ** -0.5)
    qi = jnp.arange(band)[:, None]
    ki = jnp.arange(2 * band)[None, :]
    dist = qi + band - ki
    key_sub = jnp.arange(n_blk)[:, None, None] * band + ki - band
    valid = (dist >= 0) & (dist <= band) & (key_sub >= 0)
    s = jnp.where(valid[None, None, :, None], s, -jnp.inf)
    m = jnp.max(s, axis=-1, keepdims=True)
    p = jnp.exp(s - m)
    den = jnp.sum(p, axis=-1)
    den_t = jnp.transpose(den, (0, 1, 2, 4, 3))
    o = jnp.einsum('brnhqk,brnkhd->brnqhd', p, vb.astype(jnp.float32)) / den_t[..., None]
    lse = jnp.transpose(m[..., 0], (0, 1, 2, 4, 3)) + jnp.log(den_t)

    def from_blocks(t):
        t = t.reshape((bsz, r, n_blk * band) + t.shape[4:])[:, :, :sub_len]
        t = jnp.moveaxis(t, 1, 2)
        return t.reshape((bsz, seq) + t.shape[3:])

    return from_blocks(o).astype(q.dtype), from_blocks(lse)


def segsum(a):
    t_len = a.shape[-1]
    rep = jnp.broadcast_to(a[..., :, None], a.shape + (t_len,))
    strict = jnp.tril(jnp.ones((t_len, t_len), dtype=bool), -1)
    cs = jnp.cumsum(jnp.where(strict, rep, 0.0), axis=-2)
    return jnp.where(jnp.tril(jnp.ones((t_len, t_len), dtype=bool)), cs, -jnp.inf)


def ssd_chunked(x, a, bm, cm):
    bsz, seq = x.shape[:2]
    nc = seq // SSD_CHUNK
    x = x.reshape(bsz, nc, SSD_CHUNK, SSD_GROUPS, SSD_HEADS_PER_GROUP, SSD_HEAD_DIM)
    bm = bm.reshape(bsz, nc, SSD_CHUNK, SSD_GROUPS, SSD_STATE)
    cm = cm.reshape(bsz, nc, SSD_CHUNK, SSD_GROUPS, SSD_STATE)
    a = a.reshape(bsz, nc, SSD_CHUNK, SSD_GROUPS, SSD_HEADS_PER_GROUP).transpose(0, 3, 4, 1, 2).astype(jnp.float32)
    a_cs = jnp.cumsum(a, axis=-1)
    l_mat = jnp.exp(segsum(a))
    cb = jnp.einsum('bclgn,bcsgn->bcgls', cm, bm)
    y_diag = jnp.einsum('bcgls,bghcls,bcsghp->bclghp', cb, l_mat, x)
    decay_states = jnp.exp(a_cs[..., -1:] - a_cs)
    states = jnp.einsum('bclgn,bghcl,bclghp->bcghpn', bm, decay_states, x)
    states = jnp.concatenate([jnp.zeros_like(states[:, :1]), states], axis=1)
    chunk_decay = jnp.pad(a_cs[..., -1], ((0, 0), (0, 0), (0, 0), (1, 0)))
    dc = jnp.exp(segsum(chunk_decay))
    states = jnp.einsum('bghzc,bcghpn->bzghpn', dc, states)[:, :-1]
    y_off = jnp.einsum('bclgn,bcghpn,bghcl->bclghp', cm, states, jnp.exp(a_cs))
    return (y_diag + y_off).reshape(bsz, seq, SSD_GROUPS, SSD_HEADS_PER_GROUP, SSD_HEAD_DIM)


def ssd_mixer(z, xbc, dt_raw, conv_w, conv_b, dt_bias, a_log, d_skip, norm_g):
    bsz, seq, _ = z.shape
    xbc = jax.nn.silu(causal_conv(xbc, conv_w, conv_b))
    xs, bm, cm = jnp.split(xbc, [SSD_WIDTH, SSD_WIDTH + SSD_GROUPS * SSD_STATE], axis=-1)
    xs = xs.reshape(bsz, seq, SSD_GROUPS, SSD_HEADS_PER_GROUP, SSD_HEAD_DIM)
    bm = bm.reshape(bsz, seq, SSD_GROUPS, SSD_STATE)
    cm = cm.reshape(bsz, seq, SSD_GROUPS, SSD_STATE)
    dt = jax.nn.softplus(dt_raw.astype(jnp.float32) + dt_bias.astype(jnp.float32))
    dt = dt.reshape(bsz, seq, SSD_GROUPS, SSD_HEADS_PER_GROUP)
    a = -jnp.exp(a_log.astype(jnp.float32)).reshape(SSD_GROUPS, SSD_HEADS_PER_GROUP)
    y = ssd_chunked(xs * dt[..., None], dt * a, bm, cm)
    y = y + d_skip.reshape(SSD_GROUPS, SSD_HEADS_PER_GROUP)[..., None] * xs
    y = y.reshape(bsz, seq, SSD_WIDTH).astype(z.dtype)
    return rmsnorm(y * jax.nn.silu(z), norm_g)


def rglru_mixer(gate_in, x_in, conv_w, conv_b, w_r, b_r, w_i, b_i, lam):
    bsz, seq, _ = x_in.shape
    xc = causal_conv(x_in, conv_w, conv_b)
    xb = xc.reshape(bsz, seq, LRU_BLOCKS, LRU_BLOCK_WIDTH)
    r_gate = jax.nn.sigmoid((jnp.einsum('bski,kij->bskj', xb, w_r).reshape(bsz, seq, LRU_WIDTH) + b_r).astype(jnp.float32))
    i_gate = jax.nn.sigmoid((jnp.einsum('bski,kij->bskj', xb, w_i).reshape(bsz, seq, LRU_WIDTH) + b_i).astype(jnp.float32))
    log_a = -LRU_C * r_gate * jax.nn.softplus(-lam.astype(jnp.float32))
    a = jnp.exp(log_a)
    u = jnp.sqrt(-jnp.expm1(2.0 * log_a)) * (i_gate * xc.astype(jnp.float32))

    def combine(left, right):
        a_l, u_l = left
        a_r, u_r = right
        return a_l * a_r, a_r * u_l + u_r

    _, hs = lax.associative_scan(combine, (a, u), axis=1)
    return (hs * jax.nn.gelu(gate_in.astype(jnp.float32))).astype(x_in.dtype)


def mixer_block(h, positions, w_in, ssd_conv_w, ssd_conv_b, ssd_dt_bias, ssd_a_log, ssd_d, ssd_norm_g,
                lru_conv_w, lru_conv_b, lru_w_r, lru_b_r, lru_w_i, lru_b_i, lru_lambda,
                w_br_attn, w_br_ssd, w_br_lru, w_out):
    bsz, seq, _ = h.shape
    proj = h @ w_in
    cuts = np.cumsum(IN_SPLITS)[:-1].tolist()
    q, k, v, z, xbc, dt_raw, lru_gate, lru_x, merge = jnp.split(proj, cuts, axis=-1)
    q = rope(q.reshape(bsz, seq, N_ATTN_HEADS, HEAD_DIM), positions)
    k = rope(k.reshape(bsz, seq, N_ATTN_HEADS, HEAD_DIM), positions)
    q = q.reshape(bsz, seq, N_ATTN_GROUPS, HEADS_PER_GROUP, HEAD_DIM)
    k = k.reshape(bsz, seq, N_ATTN_GROUPS, HEADS_PER_GROUP, HEAD_DIM)
    v = v.reshape(bsz, seq, N_ATTN_GROUPS, HEADS_PER_GROUP, HEAD_DIM)
    outs, lses = [], []
    for gi, (window, dilation) in enumerate(ATTN_PATTERNS):
        o, lse = dilated_window_attention(q[:, :, gi], k[:, :, gi], v[:, :, gi], window, dilation)
        outs.append(o)
        lses.append(lse)
    alpha = jax.nn.softmax(jnp.stack(lses), axis=0)
    y_attn = jnp.einsum('gbsh,gbshd->bshd', alpha, jnp.stack(outs).astype(jnp.float32))
    y_attn = y_attn.reshape(bsz, seq, ATTN_OUT_WIDTH).astype(h.dtype)
    y_ssd = ssd_mixer(z, xbc, dt_raw, ssd_conv_w, ssd_conv_b, ssd_dt_bias, ssd_a_log, ssd_d, ssd_norm_g)
    y_lru = rglru_mixer(lru_gate, lru_x, lru_conv_w, lru_conv_b, lru_w_r, lru_b_r, lru_w_i, lru_b_i, lru_lambda)
    g_attn, g_ssd, g_lru = jnp.split(jax.nn.sigmoid(merge), N_BRANCHES, axis=-1)
    merged = g_attn * (y_attn @ w_br_attn) + g_ssd * (y_ssd @ w_br_ssd) + g_lru * (y_lru @ w_br_lru)
    return merged @ w_out


def hier_moe(h, router_wg, router_bg, router_we, router_be, w_gate, w_up, w_down):
    bsz, seq, dm = h.shape
    n_tok = bsz * seq
    xt = h.reshape(n_tok, dm)
    lg = (xt @ router_wg + router_bg).astype(jnp.float32)
    pg = jax.nn.softmax(lg, axis=-1)
    g_sel = jnp.argmax(lg, axis=-1)
    p_grp = jnp.take_along_axis(pg, g_sel[:, None], axis=-1)
    le = jnp.einsum('td,dge->tge', xt, router_we) + router_be
    le = jnp.take_along_axis(le, g_sel[:, None, None], axis=1)[:, 0].astype(jnp.float32)
    pe = jax.nn.softmax(le, axis=-1)
    top_p, top_i = lax.top_k(pe, TOP_K)
    top_p = top_p / jnp.sum(top_p, axis=-1, keepdims=True)
    weights = p_grp * top_p
    expert = g_sel[:, None] * EXPERTS_PER_GROUP + top_i
    n_asg = n_tok * TOP_K
    flat_e = expert.reshape(n_asg).astype(jnp.int32)
    flat_w = weights.reshape(n_asg)
    flat_tok = jnp.arange(n_asg, dtype=jnp.int32) // TOP_K
    order = jnp.argsort(flat_e)
    se, stok, sw = flat_e[order], flat_tok[order], flat_w[order]
    counts = jnp.bincount(flat_e, length=N_EXPERTS)
    start = jnp.cumsum(counts) - counts
    pcounts = ((counts + MOE_BLOCK - 1) // MOE_BLOCK) * MOE_BLOCK
    pend = jnp.cumsum(pcounts)
    pstart = pend - pcounts
    dest = pstart[se] + jnp.arange(n_asg, dtype=jnp.int32) - start[se]
    n_blocks = -(-n_asg // MOE_BLOCK) + N_EXPERTS
    rows = jnp.full((n_blocks * MOE_BLOCK,), n_tok, dtype=jnp.int32).at[dest].set(stok)
    x_pad = jnp.concatenate([xt, jnp.zeros((1, dm), xt.dtype)], axis=0)
    xd = x_pad[rows].reshape(n_blocks, MOE_BLOCK, dm)
    block_e = jnp.minimum(jnp.searchsorted(pend, jnp.arange(n_blocks) * MOE_BLOCK, side='right'), N_EXPERTS - 1)

    def expert_ffn(args):
        xb, e = args
        return (jax.nn.silu(xb @ w_gate[e]) * (xb @ w_up[e])) @ w_down[e]

    yd = lax.map(expert_ffn, (xd, block_e)).reshape(n_blocks * MOE_BLOCK, dm)
    y = jax.ops.segment_sum(yd[dest] * sw[:, None], stok, num_segments=n_tok)
    return y.reshape(bsz, seq, dm).astype(h.dtype)


def setup_inputs(seed: int = 0) -> dict:
    key = jax.random.key(seed)
    ks = iter(jax.random.split(key, 48))

    def nk():
        return next(ks)

    def dense(shape, fan_in, scale=1.0):
        return (scale * fan_in ** -0.5) * jax.random.normal(nk(), shape, jnp.float32)

    def gain(shape):
        return 1.0 + 0.02 * jax.random.normal(nk(), shape, jnp.float32)

    def small(shape, s=0.02):
        return s * jax.random.normal(nk(), shape, jnp.float32)

    x = jax.random.normal(nk(), (BATCH, SEQ, D_MODEL), jnp.float32)
    c = jax.random.normal(nk(), (BATCH, D_MODEL), jnp.float32)
    offset = jax.random.randint(nk(), (BATCH, 1), 0, 4096, dtype=jnp.int32)
    positions = offset + jnp.arange(SEQ, dtype=jnp.int32)[None, :]
    ada_w = dense((DEPTH, D_MODEL, 6 * D_MODEL), D_MODEL, 0.5)
    ada_b = small((DEPTH, 6 * D_MODEL))
    norm1_g = gain((DEPTH, D_MODEL))
    norm2_g = gain((DEPTH, D_MODEL))
    w_in = dense((DEPTH, D_MODEL, IN_COLS), D_MODEL)
    ssd_conv_w = dense((DEPTH, SSD_CONV, SSD_XBC_WIDTH), SSD_CONV)
    ssd_conv_b = small((DEPTH, SSD_XBC_WIDTH))
    u = jax.random.uniform(nk(), (DEPTH, SSD_HEADS), jnp.float32)
    dt0 = jnp.exp(u * (math.log(0.1) - math.log(0.001)) + math.log(0.001))
    ssd_dt_bias = dt0 + jnp.log(-jnp.expm1(-dt0))
    ssd_a_log = jnp.log(jax.random.uniform(nk(), (DEPTH, SSD_HEADS), jnp.float32, 1.0, 16.0))
    ssd_d = gain((DEPTH, SSD_HEADS))
    ssd_norm_g = gain((DEPTH, SSD_WIDTH))
    lru_conv_w = dense((DEPTH, LRU_CONV, LRU_WIDTH), LRU_CONV)
    lru_conv_b = small((DEPTH, LRU_WIDTH))
    lru_w_r = dense((DEPTH, LRU_BLOCKS, LRU_BLOCK_WIDTH, LRU_BLOCK_WIDTH), LRU_BLOCK_WIDTH)
    lru_b_r = small((DEPTH, LRU_WIDTH))
    lru_w_i = dense((DEPTH, LRU_BLOCKS, LRU_BLOCK_WIDTH, LRU_BLOCK_WIDTH), LRU_BLOCK_WIDTH)
    lru_b_i = small((DEPTH, LRU_WIDTH))
    a0 = jax.random.uniform(nk(), (DEPTH, LRU_WIDTH), jnp.float32, 0.9, 0.999)
    s0 = a0 ** (1.0 / LRU_C)
    lru_lambda = jnp.log(s0) - jnp.log1p(-s0)
    w_br_attn = dense((DEPTH, ATTN_OUT_WIDTH, D_MODEL), ATTN_OUT_WIDTH)
    w_br_ssd = dense((DEPTH, SSD_WIDTH, D_MODEL), SSD_WIDTH)
    w_br_lru = dense((DEPTH, LRU_WIDTH, D_MODEL), LRU_WIDTH)
    w_out = dense((DEPTH, D_MODEL, D_MODEL), D_MODEL)
    router_wg = dense((DEPTH, D_MODEL, MOE_GROUPS), D_MODEL)
    router_bg = small((DEPTH, MOE_GROUPS), 0.01)
    router_we = dense((DEPTH, D_MODEL, MOE_GROUPS, EXPERTS_PER_GROUP), D_MODEL)
    router_be = small((DEPTH, MOE_GROUPS, EXPERTS_PER_GROUP), 0.01)
    exp_w_gate = dense((DEPTH, N_EXPERTS, D_MODEL, EXPERT_FF), D_MODEL)
    exp_w_up = dense((DEPTH, N_EXPERTS, D_MODEL, EXPERT_FF), D_MODEL)
    exp_w_down = dense((DEPTH, N_EXPERTS, EXPERT_FF, D_MODEL), EXPERT_FF)
    final_g = gain((D_MODEL,))
    return {'x': x, 'c': c, 'positions': positions, 'ada_w': ada_w, 'ada_b': ada_b,
            'norm1_g': norm1_g, 'norm2_g': norm2_g, 'w_in': w_in,
            'ssd_conv_w': ssd_conv_w, 'ssd_conv_b': ssd_conv_b, 'ssd_dt_bias': ssd_dt_bias,
            'ssd_a_log': ssd_a_log, 'ssd_d': ssd_d, 'ssd_norm_g': ssd_norm_g,
            'lru_conv_w': lru_conv_w, 'lru_conv_b': lru_conv_b, 'lru_w_r': lru_w_r, 'lru_b_r': lru_b_r,
            'lru_w_i': lru_w_i, 'lru_b_i': lru_b_i, 'lru_lambda': lru_lambda,
            'w_br_attn': w_br_attn, 'w_br_ssd': w_br_ssd, 'w_br_lru': w_br_lru, 'w_out': w_out,
            'router_wg': router_wg, 'router_bg': router_bg, 'router_we': router_we, 'router_be': router_be,
            'exp_w_gate': exp_w_gate, 'exp_w_up': exp_w_up, 'exp_w_down': exp_w_down, 'final_g': final_g}


def reference(x, c, positions, ada_w, ada_b, norm1_g, norm2_g, w_in,
              ssd_conv_w, ssd_conv_b, ssd_dt_bias, ssd_a_log, ssd_d, ssd_norm_g,
              lru_conv_w, lru_conv_b, lru_w_r, lru_b_r, lru_w_i, lru_b_i, lru_lambda,
              w_br_attn, w_br_ssd, w_br_lru, w_out,
              router_wg, router_bg, router_we, router_be,
              exp_w_gate, exp_w_up, exp_w_down, final_g):
    c_act = jax.nn.silu(c)
    for l in range(DEPTH):
        cond = c_act @ ada_w[l] + ada_b[l]
        sh1, sc1, g1, sh2, sc2, g2 = jnp.split(cond, 6, axis=-1)
        h = modulate(rmsnorm(x, norm1_g[l]), sh1, sc1)
        y = mixer_block(h, positions, w_in[l], ssd_conv_w[l], ssd_conv_b[l], ssd_dt_bias[l], ssd_a_log[l],
                        ssd_d[l], ssd_norm_g[l], lru_conv_w[l], lru_conv_b[l], lru_w_r[l], lru_b_r[l],
                        lru_w_i[l], lru_b_i[l], lru_lambda[l], w_br_attn[l], w_br_ssd[l], w_br_lru[l], w_out[l])
        x = x + g1[:, None, :] * y
        h = modulate(rmsnorm(x, norm2_g[l]), sh2, sc2)
        y = hier_moe(h, router_wg[l], router_bg[l], router_we[l], router_be[l],
                     exp_w_gate[l], exp_w_up[l], exp_w_down[l])
        x = x + g2[:, None, :] * y
    return rmsnorm(x, final_g)
```

```python
import numpy as np
import concourse.bass as bass
import concourse.mybir as mybir
from concourse.bass_utils import run_bass_kernel_spmd

F32 = mybir.dt.float32
BF16 = mybir.dt.bfloat16
I32 = mybir.dt.int32
ALU = mybir.AluOpType
AF = mybir.ActivationFunctionType
AX = mybir.AxisListType

_DTSIZE = {F32: 4, BF16: 2, I32: 4}
SEM_ROT = 30000

D = 1024
S = 2048
DEPTH = 4
NEXP = 32
FF = 512
INC = 16416
EPS = 1e-6
OQ, OK_, OV, OZ, OXBC, ODT, OLG, OLX, OMG = 0, 1536, 3072, 4608, 6656, 10752, 10784, 12064, 13344


def _box(ap):
    t = ap.tensor
    es = _DTSIZE[ap.dtype]
    pat = ap.ap
    off = int(ap.offset)
    if type(t).__name__.startswith('DRam'):
        ext = 1
        for st, cnt in pat:
            ext += (cnt - 1) * abs(st)
        return (t.name, 0, 1, off * es, (off + ext) * es)
    if type(t).__name__.startswith('PSum'):
        return (t.name, 0, 128, 0, 2048)
    free = 1
    for s in list(t.shape)[1:]:
        free *= s
    p0 = off // free
    f0 = off % free
    np_ = pat[0][1] if pat[0][0] != 0 else 1
    ext = 1
    for st, cnt in pat[1:]:
        ext += (cnt - 1) * abs(st)
    return (t.name, p0, p0 + np_, f0 * es, (f0 + ext) * es)


class Chan:
    def __init__(self, kb, name):
        self.kb = kb
        self.name = name
        self.sem = kb.nc.alloc_semaphore(name)
        kb.chan_by_sem[self.sem.num] = self
        self.cnt = 0
        self.gen = 0

    def next_event(self):
        if self.cnt + 16 > SEM_ROT:
            self.gen += 1
            self.sem = self.kb.nc.alloc_semaphore(f"{self.name}_g{self.gen}")
            self.kb.chan_by_sem[self.sem.num] = self
            self.cnt = 0
        self.cnt += 16
        return (self.sem, self.cnt)


class KB:
    def __init__(self, nc):
        self.nc = nc
        self.engs = {'pe': nc.tensor, 'act': nc.scalar, 'dve': nc.vector, 'pool': nc.gpsimd, 'sp': nc.sync}
        self.sem, self.cnt, self.gen = {}, {}, {}
        for e in ('pe', 'act', 'dve', 'pool'):
            self.sem[e] = nc.alloc_semaphore(f"s_{e}")
            self.cnt[e] = 0
            self.gen[e] = 0
        self.oldsems = []
        self.seen = {e: {} for e in self.engs}
        self.recs = {}
        self.pending = {e: False for e in self.engs}
        self.ninst = {e: 0 for e in self.engs}
        self.chans = {}
        self.chan_by_sem = {}

    def chan(self, name):
        if name not in self.chans:
            self.chans[name] = Chan(self, name)
        return self.chans[name]

    def _deps(self, ins, outs):
        deps = []
        for ap in ins:
            b = _box(ap)
            for r in self.recs.get(b[0], ()):
                if r[5] and r[1] < b[2] and b[1] < r[2] and r[3] < b[4] and b[3] < r[4]:
                    deps.append(r[6])
        for ap in outs:
            b = _box(ap)
            for r in self.recs.get(b[0], ()):
                if r[1] < b[2] and b[1] < r[2] and r[3] < b[4] and b[3] < r[4]:
                    deps.append(r[6])
        return deps

    def _record(self, ins, outs, ev, tag):
        for ap in outs:
            b = _box(ap)
            lst = self.recs.setdefault(b[0], [])
            lst[:] = [r for r in lst if not (b[1] <= r[1] and r[2] <= b[2] and b[3] <= r[3] and r[4] <= b[4])]
            lst.append((b[0], b[1], b[2], b[3], b[4], True, ev, tag))
        for ap in ins:
            b = _box(ap)
            lst = self.recs.setdefault(b[0], [])
            lst[:] = [r for r in lst if not ((not r[5]) and r[7] == tag and r[1:5] == b[1:5])]
            lst.append((b[0], b[1], b[2], b[3], b[4], False, ev, tag))

    def _wait(self, e, deps, skip_self=False):
        eng = self.engs[e]
        seen = self.seen[e]
        best = {}
        for sem, val in deps:
            if skip_self and e in self.sem and sem is self.sem[e]:
                continue
            k = sem.num
            ch = self.chan_by_sem.get(k)
            if ch is not None and ch.sem.num == k:
                val = max(val, ch.cnt)
            if seen.get(k, 0) >= val:
                continue
            if k not in best or best[k][1] < val:
                best[k] = (sem, val)
        for k, (sem, val) in best.items():
            eng.wait_ge(sem, val)
            seen[k] = val

    def op(self, e, fn, outs, ins, signal=True):
        deps = self._deps(ins, outs)
        self._wait(e, deps, skip_self=(e == 'pe'))
        inst = fn()
        self.ninst[e] += 1
        if signal and self.cnt[e] + 1 > SEM_ROT:
            self.oldsems.append((self.sem[e], self.cnt[e]))
            self.gen[e] += 1
            self.sem[e] = self.nc.alloc_semaphore(f"s_{e}_g{self.gen[e]}")
            self.cnt[e] = 0
        if signal:
            self.cnt[e] += 1
            inst.then_inc(self.sem[e], 1)
            ev = (self.sem[e], self.cnt[e])
            self.pending[e] = False
        else:
            assert self.cnt[e] + 1 <= SEM_ROT
            ev = (self.sem[e], self.cnt[e] + 1)
            self.pending[e] = True
        self._record(ins, outs, ev, e)
        return inst

    def dma(self, out, in_, chan, q='sp', **kw):
        ch = self.chan(chan)
        deps = self._deps([in_], [out])
        self._wait(q, deps)
        ev = ch.next_event()
        inst = self.engs[q].dma_start(out=out, in_=in_, **kw)
        inst.then_inc(ev[0], 16)
        self.ninst[q] += 1
        self._record([in_], [out], ev, 'dma_' + ch.name)
        return inst

    def all_events(self):
        evs = [(self.sem[e], self.cnt[e]) for e in ('pe', 'act', 'dve', 'pool') if self.cnt[e] > 0]
        evs += [(ch.sem, ch.cnt) for ch in self.chans.values() if ch.cnt > 0]
        return evs

    def barrier(self, drop=()):
        for e in ('pe', 'act', 'dve', 'pool'):
            assert not self.pending[e], e
        evs = self.all_events()
        for e in self.engs:
            self._wait(e, evs)
        self.recs.clear()

    def finish(self):
        for e in ('pe', 'act', 'dve', 'pool'):
            assert not self.pending[e], e
        self._wait('sp', self.all_events())


PK_COLS = {}
_o = 0
for _n, _w in (('n1g', 8), ('n2g', 8), ('adab', 48), ('scw', 128), ('scb', 32), ('dtb', 32), ('alog', 32),
               ('sdd', 16), ('sng', 16), ('rb', 36), ('lcw', 64), ('lcb', 16), ('lbr', 16), ('lbi', 16), ('llam', 16)):
    PK_COLS[_n] = (_o, _w)
    _o += _w
NPK = _o
GK_COLS = {}
_o = 0
for _n, _w in (('c', 8), ('fg', 8), ('freq', 1), ('ident', 128), ('mle', 128), ('mgt', 128), ('mge', 128), ('ones', 128),
               ('phase', 1)):
    GK_COLS[_n] = (_o, _w)
    _o += _w
NGK = _o


def _fm(v, p=128):
    return np.ascontiguousarray(v.reshape(-1, p).T)


def _pack_layer(inp, l):
    pk = np.zeros((128, NPK), np.float32)

    def put(name, arr):
        o, w = PK_COLS[name]
        assert arr.shape[1] == w, (name, arr.shape)
        pk[:arr.shape[0], o:o + w] = arr
    put('n1g', _fm(inp['norm1_g'][l]))
    put('n2g', _fm(inp['norm2_g'][l]))
    put('adab', _fm(inp['ada_b'][l]))
    scw = inp['ssd_conv_w'][l]
    put('scw', np.ascontiguousarray(scw.T.reshape(32, 128, 4).transpose(1, 0, 2)).reshape(128, 128))
    put('scb', _fm(inp['ssd_conv_b'][l]))
    put('dtb', np.broadcast_to(inp['ssd_dt_bias'][l][None, :], (128, 32)))
    put('alog', np.broadcast_to(inp['ssd_a_log'][l][None, :], (128, 32)))
    put('sdd', _fm(np.repeat(inp['ssd_d'][l], 64)))
    put('sng', _fm(inp['ssd_norm_g'][l]))
    rb = np.concatenate([inp['router_bg'][l].reshape(-1), inp['router_be'][l].reshape(-1)])
    put('rb', np.broadcast_to(rb[None, :], (128, 36)))
    lcw = inp['lru_conv_w'][l]
    put('lcw', np.ascontiguousarray(lcw.T.reshape(16, 80, 4).transpose(1, 0, 2)).reshape(80, 64))
    put('lcb', _fm(inp['lru_conv_b'][l], 80))
    put('lbr', _fm(inp['lru_b_r'][l], 80))
    put('lbi', _fm(inp['lru_b_i'][l], 80))
    put('llam', _fm(inp['lru_lambda'][l], 80))
    return pk


def _pack_global(c_b, final_g):
    gk = np.zeros((128, NGK), np.float32)

    def put(name, arr):
        o, w = GK_COLS[name]
        gk[:, o:o + w] = arr
    put('c', _fm(c_b))
    put('fg', _fm(final_g))
    half = 32
    freqs = (10000.0 ** (-np.arange(half, dtype=np.float32) / half)).astype(np.float32)
    put('freq', np.tile(freqs, 4)[:, None])
    i = np.arange(128)
    put('ident', (i[:, None] == i[None, :]).astype(np.float32))
    put('mle', (i[:, None] <= i[None, :]).astype(np.float32))
    put('mgt', (i[:, None] > i[None, :]).astype(np.float32))
    put('mge', (i[:, None] >= i[None, :]).astype(np.float32))
    put('ones', np.ones((128, 128), np.float32))
    put('phase', np.where((i % 64) < 32, -1.0, 1.0).astype(np.float32)[:, None])
    return gk


from contextlib import ExitStack
import math


def build(L=DEPTH, dbg=False, phases=None):
    nc = bass.Bass("TRN2", target_bir_lowering=False)
    kb = KB(nc)

    def din(name, shape, dt=F32):
        return nc.dram_tensor(name, list(shape), dt, kind="ExternalInput").ap()

    def dscr(name, shape, dt):
        return nc.dram_tensor(name, list(shape), dt, kind=("ExternalOutput" if dbg else "Internal")).ap()

    x_d = din("x", [S, D])
    pos_d = din("pos", [128, S], I32)
    gk_d = din("gk", [128, NGK])
    pk_d = din("pk", [L, 128, NPK])
    adaw_d = din("ada_w", [L, D, 6 * D])
    win_d = din("w_in", [L, D, INC])
    lwr_d = din("lru_w_r", [L, 16, 80, 80])
    lwi_d = din("lru_w_i", [L, 16, 80, 80])
    wba_d = din("w_br_attn", [L, 512, D])
    wbs_d = din("w_br_ssd", [L, 2048, D])
    wbl_d = din("w_br_lru", [L, 1280, D])
    wo_d = din("w_out", [L, D, D])
    rwg_d = din("router_wg", [L, D, 4])
    rwe_d = din("router_we", [L, D, 32])
    eg_d = din("exp_w_gate", [L, NEXP, D, FF])
    eu_d = din("exp_w_up", [L, NEXP, D, FF])
    ed_d = din("exp_w_down", [L, NEXP, FF, D])
    out_d = nc.dram_tensor("out", [S, D], F32, kind="ExternalOutput").ap()

    xs_d = dscr("xs_scr", [128, 8, S], F32)
    ya_d = dscr("ya_scr", [64, 8, S], BF16)
    ys_d = dscr("ys_scr", [128, 16, S], BF16)
    yl_d = dscr("yl_scr", [80, 16, S], BF16)
    xbc_d = nc.dram_tensor("xbc_scr", [128, 32, S], BF16).ap()
    z_d = nc.dram_tensor("z_scr", [128, 16, S], BF16).ap()
    cs_d = nc.dram_tensor("cs_scr", [128, 2, S], F32).ap()

    ps = [nc.alloc_psum_tensor(f"ps{i}", [128, 512], F32) for i in range(8)]
    pctr = [0]

    def P():
        t = ps[pctr[0] % 8]
        pctr[0] += 1
        return t

    def mm(out, lhsT, rhs, start=True, stop=True):
        return kb.op('pe', lambda: nc.tensor.matmul(out, lhsT=lhsT, rhs=rhs, start=start, stop=stop),
                     [out], [lhsT, rhs], signal=True)

    def tr(out, in_, ident):
        return kb.op('pe', lambda: nc.tensor.transpose(out=out, in_=in_, identity=ident), [out], [in_, ident])

    def V(e, fn, outs, ins):
        return kb.op(e, fn, outs, ins)

    def tt_(e, out, in0, in1, op):
        eng = kb.engs[e]
        return kb.op(e, lambda: eng.tensor_tensor(out=out, in0=in0, in1=in1, op=op), [out], [in0, in1])

    def ts_(e, out, in0, s1, s2, op0, op1=ALU.bypass):
        eng = kb.engs[e]
        ins = [in0] + [s for s in (s1, s2) if not isinstance(s, (int, float)) and s is not None]
        if s2 is None:
            return kb.op(e, lambda: eng.tensor_scalar(out=out, in0=in0, scalar1=s1, scalar2=None, op0=op0), [out], ins)
        return kb.op(e, lambda: eng.tensor_scalar(out=out, in0=in0, scalar1=s1, scalar2=s2, op0=op0, op1=op1), [out], ins)

    def stt_(out, in0, scalar, in1, op0, op1):
        ins = [in0, in1] + ([] if isinstance(scalar, (int, float)) else [scalar])
        return kb.op('dve', lambda: nc.vector.scalar_tensor_tensor(out=out, in0=in0, scalar=scalar, in1=in1, op0=op0, op1=op1),
                     [out], ins)

    def act_(out, in_, func, bias=0.0, scale=1.0):
        ins = [in_] + [s for s in (bias, scale) if not isinstance(s, (int, float))]
        return kb.op('act', lambda: nc.scalar.activation(out=out, in_=in_, func=func, bias=bias, scale=scale), [out], ins)

    def copy_(e, out, in_):
        if e == 'act':
            return kb.op('act', lambda: nc.scalar.copy(out=out, in_=in_), [out], [in_])
        eng = kb.engs[e]
        return kb.op(e, lambda: eng.tensor_copy(out=out, in_=in_), [out], [in_])

    def memset_(e, ap, val):
        eng = kb.engs[e]
        return kb.op(e, lambda: eng.memset(ap, val), [ap], [])

    def winv(l):
        return win_d[l].rearrange("(kc p) c -> p kc c", p=128)

    gk = nc.alloc_sbuf_tensor("gk_sb", [128, NGK], F32)
    kb.dma(gk[:], gk_d[:, :], 'c0')

    def G(name):
        o, w = GK_COLS[name]
        return gk[:, o:o + w]
    identf, mle, mgt, mge, onesf = G('ident'), G('mle'), G('mgt'), G('mge'), G('ones')
    cb = nc.alloc_sbuf_tensor("cb", [128, 3, 128], BF16)
    for i, nm in enumerate(('ident', 'mle', 'mge')):
        copy_('dve', cb[:, i, :], G(nm))
    identb = cb[:, 0, :]
    hT = nc.alloc_sbuf_tensor("hT", [128, 8, S], BF16)
    pk = nc.alloc_sbuf_tensor("pk_sb", [128, NPK], F32)
    cond = nc.alloc_sbuf_tensor("cond", [128, 48], F32)
    gm = nc.alloc_sbuf_tensor("gm", [128, 16], F32)
    cact = nc.alloc_sbuf_tensor("cact", [128, 8], F32)
    dtraw = nc.alloc_sbuf_tensor("dtraw", [128, 16, 32], F32)
    act_(cact[:], G('c'), AF.Silu)

    def PK(name, rows=128):
        o, w = PK_COLS[name]
        return pk[0:rows, o:o + w]

    def load_x_phase():
        with ExitStack() as es:
            xin = [es.enter_context(nc.sbuf_tensor(f"xin{i}", [128, D], F32)) for i in range(2)]
            xo = [es.enter_context(nc.sbuf_tensor(f"xo{i}", [128, 8, 128], F32)) for i in range(2)]
            for t in range(16):
                xi = xin[t % 2]
                kb.dma(xi[:], x_d[t * 128:(t + 1) * 128, :], f'xin{t % 2}')
                for half in range(2):
                    p = P()
                    for j in range(4):
                        kc = half * 4 + j
                        tr(p[:, j * 128:(j + 1) * 128], xi[:, kc * 128:(kc + 1) * 128], identf)
                    copy_('dve' if half == 0 else 'act', xo[t % 2][:, half * 4:(half + 1) * 4, :],
                          p[:, :].rearrange("p (j t) -> p j t", j=4))
                kb.dma(xs_d[:, :, t * 128:(t + 1) * 128], xo[t % 2][:], f'xo{t % 2}')
            kb.barrier()

    def rope_phase():
        with ExitStack() as es:
            posi = es.enter_context(nc.sbuf_tensor("posi", [128, S], I32))
            ki = es.enter_context(nc.sbuf_tensor("rki", [128, S], I32))
            ang = es.enter_context(nc.sbuf_tensor("ang", [128, S], F32))
            u = es.enter_context(nc.sbuf_tensor("ru", [128, S], F32))
            kf = es.enter_context(nc.sbuf_tensor("rkf", [128, S], F32))
            r = es.enter_context(nc.sbuf_tensor("rr", [128, S], F32))
            m = es.enter_context(nc.sbuf_tensor("rm", [128, S], F32))
            kb.dma(posi[:], pos_d[:, :], 'c0')
            copy_('dve', ang[:], posi[:])
            ts_('dve', ang[:], ang[:], G('freq'), None, ALU.mult)
            TWO_PI = 2.0 * math.pi
            C1 = 6.28125
            C2 = TWO_PI - C1
            for which, phi in ((0, math.pi / 2), (1, 0.0)):
                ts_('dve', u[:], ang[:], 1.0 / TWO_PI, phi / TWO_PI + 0.5, ALU.mult, ALU.add)
                copy_('dve', ki[:], u[:])
                copy_('dve', kf[:], ki[:])
                stt_(r[:], kf[:], -C1, ang[:], ALU.mult, ALU.add)
                stt_(r[:], kf[:], -C2, r[:], ALU.mult, ALU.add)
                if phi != 0.0:
                    ts_('dve', r[:], r[:], phi, None, ALU.add)
                ts_('dve', m[:], r[:], -math.pi, None, ALU.is_lt)
                stt_(r[:], m[:], TWO_PI, r[:], ALU.mult, ALU.add)
                ts_('dve', m[:], r[:], math.pi, None, ALU.is_gt)
                stt_(r[:], m[:], -TWO_PI, r[:], ALU.mult, ALU.add)
                ts_('dve', r[:], r[:], -3.1415925, 3.1415925, ALU.max, ALU.min)
                act_(u[:], r[:], AF.Sin)
                if which == 1:
                    ts_('dve', u[:], u[:], G('phase'), None, ALU.mult)
                kb.dma(cs_d[:, which, :], u[:], 'cs_st')
            kb.barrier()

    def cond_phase(l):
        with ExitStack() as es:
            aw = [es.enter_context(nc.sbuf_tensor(f"aw{l}_{i}", [128, 8, 512], F32)) for i in range(2)]
            kb.dma(pk[:], pk_d[l], 'pk')
            pc = P()
            src = adaw_d[l].rearrange("(kc p) c -> p kc c", p=128)
            for cc in range(12):
                w = aw[cc % 2]
                kb.dma(w[:], src[:, :, cc * 512:(cc + 1) * 512], f'aw{cc % 2}')
                for j in range(4):
                    col = cc * 4 + j
                    for kc in range(8):
                        mm(pc[:, col:col + 1], w[:, kc, j * 128:(j + 1) * 128], cact[:, kc:kc + 1], kc == 0, kc == 7)
            tt_('dve', cond[:], pc[:, 0:48], PK('adab'), ALU.add)
            stt_(gm[:, 0:8], cond[:, 8:16], 1.0, PK('n1g'), ALU.add, ALU.mult)
            stt_(gm[:, 8:16], cond[:, 32:40], 1.0, PK('n2g'), ALU.add, ALU.mult)
            kb.barrier()

    def norm_phase(l, which, tag):
        with ExitStack() as es:
            xt = [es.enter_context(nc.sbuf_tensor(f"nx{tag}_{i}", [128, 8, 512], F32)) for i in range(2)]
            sq = [es.enter_context(nc.sbuf_tensor(f"nsq{tag}_{i}", [128, 512], F32)) for i in range(2)]
            rs = [es.enter_context(nc.sbuf_tensor(f"nrs{tag}_{i}", [128, 512], F32)) for i in range(2)]
            t2 = [es.enter_context(nc.sbuf_tensor(f"nt2{tag}_{i}", [128, 512], F32)) for i in range(2)]
            sh0 = 0 if which == 0 else 24
            for tt in range(4):
                X = xt[tt % 2]
                kb.dma(X[:], xs_d[:, :, tt * 512:(tt + 1) * 512], f'nx{tt % 2}')
                pss = P()
                for kc in range(8):
                    act_(sq[kc % 2][:], X[:, kc, :], AF.Square)
                    mm(pss[:, :], onesf, sq[kc % 2][:], kc == 0, kc == 7)
                R_ = rs[tt % 2]
                act_(R_[:], pss[:, :], AF.Sqrt, bias=EPSC[:, 0:1], scale=1.0 / D)
                V('dve', lambda: nc.vector.reciprocal(out=R_[:], in_=R_[:]), [R_[:]], [R_[:]])
                for kc in range(8):
                    T = t2[kc % 2]
                    tt_('dve', T[:], X[:, kc, :], R_[:], ALU.mult)
                    ts_('pool', hT[:, kc, tt * 512:(tt + 1) * 512], T[:], gm[:, which * 8 + kc:which * 8 + kc + 1],
                        cond[:, sh0 + kc:sh0 + kc + 1], ALU.mult, ALU.add)
            kb.barrier()

    epsc = nc.alloc_sbuf_tensor("epsc", [128, 2], F32)
    EPSC = epsc
    memset_('dve', epsc[:, 0:1], EPS)
    memset_('dve', epsc[:, 1:2], 1.0)

    def attn_tok(g, ti):
        if g == 0:
            return slice(128 * ti, 128 * ti + 128, 1)
        if g == 1:
            st = 512 * (ti // 4) + (ti % 4)
            return slice(st, st + 4 * 127 + 1, 4)
        return slice(ti, ti + 16 * 127 + 1, 16)

    def attn_prev(g, ti):
        if g == 0:
            return ti - 1 if ti >= 1 else None
        if g == 1:
            return ti - 4 if ti >= 4 else None
        return None

    def attn_phase(l):
        with ExitStack() as es:
            A = lambda n, sh, dt: es.enter_context(nc.sbuf_tensor(f"{n}_{l}", sh, dt))
            cs = A("acs", [128, 2, S], F32)
            qk = A("aqk", [128, 2, 2, S], BF16)
            vaug = A("avaug", [128, 16, 4, 128], BF16)
            acc = A("aacc", [128, 4, S], F32)
            wq = [A(f"awq{i}", [128, 8, 256], BF16) for i in range(3)]
            wst = [A(f"awst{i}", [128, 8, 256], F32) for i in range(2)]
            ta = [A(f"ata{i}", [128, 512], F32) for i in range(2)]
            tb = [A(f"atb{i}", [128, 512], F32) for i in range(2)]
            pt = [A(f"apt{i}", [128, 512], BF16) for i in range(4)]
            ptm = [A(f"aptm{i}", [128, 4, 128], BF16) for i in range(4)]
            rd = A("ard", [64, 4, 512], F32)
            yo = [A(f"ayo{i}", [64, 4, 512], BF16) for i in range(2)]
            kb.dma(cs[:], cs_d[:, :, :], 'acs')
            memset_('pool', vaug[:, :, :, 64:128], 1.0)
            wi = 0
            pi = 0
            for hh in range(2):
                for g in range(3):
                    for which, off in ((0, OQ), (1, OK_)):
                        c0 = off + g * 512 + hh * 256
                        W = wq[wi % 3]
                        kb.dma(wst[wi % 2][:], winv(l)[:, :, c0:c0 + 256], f'awst{wi % 2}')
                        copy_('pool', W[:], wst[wi % 2][:])
                        wi += 1
                        for j in range(2):
                            for tt in range(4):
                                tsl = slice(tt * 512, (tt + 1) * 512)
                                p = P()
                                for kc in range(8):
                                    mm(p[:, :], W[:, kc, j * 128:(j + 1) * 128], hT[:, kc, tsl], kc == 0, kc == 7)
                                TA, TB = ta[tt % 2], tb[tt % 2]
                                tt_('dve', TA[:], p[:, :], cs[:, 0, tsl], ALU.mult)
                                for q4 in range(4):
                                    o0 = q4 * 32
                                    i0 = o0 + 32 if q4 % 2 == 0 else o0 - 32
                                    tt_('dve', TB[o0:o0 + 32, :], p[i0:i0 + 32, :], cs[o0:o0 + 32, 1, tsl], ALU.mult)
                                tt_('pool', qk[:, which, j, tsl], TA[:], TB[:], ALU.add)
                    c0 = OV + g * 512 + hh * 256
                    W = wq[wi % 3]
                    kb.dma(wst[wi % 2][:], winv(l)[:, :, c0:c0 + 256], f'awst{wi % 2}')
                    copy_('pool', W[:], wst[wi % 2][:])
                    wi += 1
                    for ti in range(16):
                        tok = attn_tok(g, ti)
                        p = P()
                        for kc in range(8):
                            mm(p[:, 0:256], hT[:, kc, tok], W[:, kc, :], kc == 0, kc == 7)
                        copy_('act', vaug[:, ti, :, 0:64], p[:, 0:256].rearrange("p (h d) -> p h d", h=4))
                    for ti in range(16):
                        tq = attn_tok(g, ti)
                        pv = attn_prev(g, ti)
                        kts = ([(pv, 2)] if pv is not None else []) + [(ti, 1)]
                        pms = []
                        for kt, mk in kts:
                            tk = attn_tok(g, kt)
                            PT, PM = pt[pi % 4], ptm[pi % 4]
                            pi += 1
                            PT3 = PT[:, :].rearrange("p (h q) -> p h q", h=4)
                            for par in range(2):
                                p = P()
                                bp = par * 64
                                for j in range(2):
                                    mm(p[:, j * 128:(j + 1) * 128], qk[bp:bp + 64, 1, j, tk], qk[bp:bp + 64, 0, j, tq])
                                act_(PT3[:, par::2, :], p[:, 0:256].rearrange("p (j q) -> p j q", j=2), AF.Exp, scale=0.125)
                            tt_('dve', PM[:], PT[:, :].rearrange("p (h q) -> p h q", h=4),
                                cb[:, mk, :][:, None, :].broadcast_to([128, 4, 128]), ALU.mult)
                            pms.append((kt, PM))
                        po = P()
                        for h in range(4):
                            for idx, (kt, PM) in enumerate(pms):
                                mm(po[:, h * 128:(h + 1) * 128], vaug[:, kt, h, :], PM[:, h, :], idx == 0, idx == len(pms) - 1)
                        pov = po[:, :].rearrange("p (h q) -> p h q", h=4)
                        if g == 0:
                            copy_('dve', acc[:, :, tq], pov)
                        else:
                            tt_('dve', acc[:, :, tq], pov, acc[:, :, tq], ALU.add)
                for tt in range(4):
                    tsl = slice(tt * 512, (tt + 1) * 512)
                    V('dve', lambda: nc.vector.reciprocal(out=rd[:], in_=acc[64:128, :, tsl]), [rd[:]], [acc[64:128, :, tsl]])
                    Y = yo[tt % 2]
                    tt_('dve', Y[:], acc[0:64, :, tsl], rd[:], ALU.mult)
                    kb.dma(ya_d[:, hh * 4:(hh + 1) * 4, tsl], Y[:], f'ayo{tt % 2}')
            kb.barrier()

    def lru_phase(l):
        with ExitStack() as es:
            A = lambda n, sh, dt: es.enter_context(nc.sbuf_tensor(f"{n}_{l}", sh, dt))
            wlx = A("lwlx", [128, 8, 1280], BF16)
            wlg = A("lwlg", [128, 8, 1280], BF16)
            wr = A("lwr", [80, 16, 80], BF16)
            wi_ = A("lwi", [80, 16, 80], BF16)
            cneg = A("lcneg", [80, 16], F32)
            c2 = A("lc2", [80, 16], F32)
            xpre = A("lxpre", [80, 3 + S], F32)
            xc = A("lxc", [80, S], F32)
            xcb = A("lxcb", [80, S], BF16)
            rg = A("lrg", [80, S], F32)
            ig = A("lig", [80, S], F32)
            Aa = A("laa", [80, S], F32)
            T1 = A("lt1", [80, S], F32)
            uu = A("luu", [80, S], F32)
            hs = A("lhs", [80, S], F32)
            gl = A("lgl", [80, S], F32)
            yo = [A(f"lyo{i}", [80, S], BF16) for i in range(2)]
            for (w, off, nm) in ((wlx, OLX, 'lwlx'), (wlg, OLG, 'lwlg')):
                for c0 in range(0, 1280, 320):
                    kb.dma(w[:, :, c0:c0 + 320], winv(l)[:, :, off + c0:off + c0 + 320], nm, q='pool')
            kb.dma(wr[:], lwr_d[l].rearrange("k i j -> i k j"), 'lwr', q='pool')
            kb.dma(wi_[:], lwi_d[l].rearrange("k i j -> i k j"), 'lwi', q='pool')
            lcw = PK('lcw', 80)
            lcb, lbr, lbi, llam = PK('lcb', 80), PK('lbr', 80), PK('lbi', 80), PK('llam', 80)
            act_(cneg[:], llam, AF.Exp, scale=-1.0)
            act_(cneg[:], cneg[:], AF.Ln, bias=EPSC[0:80, 1:2])
            ts_('dve', c2[:], cneg[:], -16.0, None, ALU.mult)
            ts_('dve', cneg[:], cneg[:], -8.0, None, ALU.mult)
            memset_('dve', xpre[:, 0:3], 0.0)
            for k in range(16):
                ksl = slice(k * 80, (k + 1) * 80)
                for tt in range(4):
                    tsl = slice(tt * 512, (tt + 1) * 512)
                    p = P()
                    for kc in range(8):
                        mm(p[0:80, :], wlx[:, kc, ksl], hT[:, kc, tsl], kc == 0, kc == 7)
                    copy_('act', xpre[:, 3 + tt * 512:3 + (tt + 1) * 512], p[0:80, :])
                ts_('dve', xc[:], xpre[:, 0:S], lcw[:, k * 4:k * 4 + 1], lcb[:, k:k + 1], ALU.mult, ALU.add)
                for j in range(1, 4):
                    stt_(xc[:], xpre[:, j:j + S], lcw[:, k * 4 + j:k * 4 + j + 1], xc[:], ALU.mult, ALU.add)
                copy_('pool', xcb[:], xc[:])
                for (wg, bb, dst) in ((wr, lbr, rg), (wi_, lbi, ig)):
                    for tt in range(4):
                        tsl = slice(tt * 512, (tt + 1) * 512)
                        p = P()
                        mm(p[0:80, :], wg[:, k, :], xcb[:, tsl])
                        act_(dst[:, tsl], p[0:80, :], AF.Sigmoid, bias=bb[:, k:k + 1])
                act_(Aa[:], rg[:], AF.Exp, scale=cneg[:, k:k + 1])
                act_(T1[:], rg[:], AF.Exp, scale=c2[:, k:k + 1])
                ts_('dve', T1[:], T1[:], -1.0, 1.0, ALU.mult, ALU.add)
                ts_('dve', T1[:], T1[:], 1e-12, None, ALU.max)
                act_(T1[:], T1[:], AF.Sqrt)
                tt_('pool', uu[:], ig[:], xc[:], ALU.mult)
                tt_('dve', uu[:], uu[:], T1[:], ALU.mult)
                V('dve', lambda: nc.vector.tensor_tensor_scan(out=hs[:], data0=Aa[:], data1=uu[:], initial=0.0,
                                                              op0=ALU.mult, op1=ALU.add), [hs[:]], [Aa[:], uu[:]])
                for tt in range(4):
                    tsl = slice(tt * 512, (tt + 1) * 512)
                    p = P()
                    for kc in range(8):
                        mm(p[0:80, :], wlg[:, kc, ksl], hT[:, kc, tsl], kc == 0, kc == 7)
                    act_(gl[:, tsl], p[0:80, :], AF.Gelu)
                Y = yo[k % 2]
                tt_('dve', Y[:], hs[:], gl[:], ALU.mult)
                kb.dma(yl_d[:, k, :], Y[:], f'lyo{k % 2}')
            kb.barrier()

    def ssd_proj_phase(l):
        with ExitStack() as es:
            A = lambda n, sh, dt: es.enter_context(nc.sbuf_tensor(f"{n}_{l}", sh, dt))
            W = [A(f"sw{i}", [128, 8, 512], BF16) for i in range(2)]
            wdt = A("swdt", [128, 8, 32], BF16)
            pre = [A(f"spre{i}", [128, 3 + S], F32) for i in range(2)]
            cv = [A(f"scv{i}", [128, S], F32) for i in range(2)]
            ob = [A(f"sob{i}", [128, S], BF16) for i in range(2)]
            scw, scb = PK('scw'), PK('scb')
            for i in range(2):
                memset_('dve', pre[i][:, 0:3], 0.0)
            n = 0
            for cg in range(8):
                Wt = W[cg % 2]
                kb.dma(Wt[:], winv(l)[:, :, OXBC + cg * 512:OXBC + (cg + 1) * 512], f'sw{cg % 2}', q='pool')
                for j in range(4):
                    fc = cg * 4 + j
                    PR, CV, OB = pre[n % 2], cv[n % 2], ob[n % 2]
                    n += 1
                    for tt in range(4):
                        p = P()
                        for kc in range(8):
                            mm(p[:, :], Wt[:, kc, j * 128:(j + 1) * 128], hT[:, kc, tt * 512:(tt + 1) * 512], kc == 0, kc == 7)
                        copy_('act', PR[:, 3 + tt * 512:3 + (tt + 1) * 512], p[:, :])
                    ts_('dve', CV[:], PR[:, 0:S], scw[:, fc * 4:fc * 4 + 1], None, ALU.mult)
                    for jj in range(1, 4):
                        stt_(CV[:], PR[:, jj:jj + S], scw[:, fc * 4 + jj:fc * 4 + jj + 1], CV[:], ALU.mult, ALU.add)
                    act_(OB[:], CV[:], AF.Silu, bias=scb[:, fc:fc + 1])
                    kb.dma(xbc_d[:, fc, :], OB[:], f'sob{(n - 1) % 2}')
            for cg in range(4):
                Wt = W[cg % 2]
                kb.dma(Wt[:], winv(l)[:, :, OZ + cg * 512:OZ + (cg + 1) * 512], f'sw{cg % 2}', q='pool')
                for j in range(4):
                    fc = cg * 4 + j
                    OB = ob[n % 2]
                    n += 1
                    for tt in range(4):
                        p = P()
                        for kc in range(8):
                            mm(p[:, :], Wt[:, kc, j * 128:(j + 1) * 128], hT[:, kc, tt * 512:(tt + 1) * 512], kc == 0, kc == 7)
                        act_(OB[:, tt * 512:(tt + 1) * 512], p[:, :], AF.Silu)
                    kb.dma(z_d[:, fc, :], OB[:], f'sob{(n - 1) % 2}')
            kb.dma(wdt[:], winv(l)[:, :, ODT:ODT + 32], 'swdt', q='pool')
            for c in range(16):
                p = P()
                for kc in range(8):
                    mm(p[:, 0:32], hT[:, kc, c * 128:(c + 1) * 128], wdt[:, kc, :], kc == 0, kc == 7)
                tt_('dve', dtraw[:, c, :], p[:, 0:32], PK('dtb'), ALU.add)
            kb.barrier()

    def ssd_scan_phase(l):
        with ExitStack() as es:
            A = lambda n, sh, dt: es.enter_context(nc.sbuf_tensor(f"{n}_{l}", sh, dt))
            H = A("cH", [128, 32, 64], F32)
            Hb = A("cHb", [128, 32, 64], BF16)
            expA = A("cexpA", [128, 32], F32)
            xsT = [A(f"cxs{i}", [128, 16, 128], BF16) for i in range(2)]
            BT = [A(f"cbt{i}", [128, 8, 128], BF16) for i in range(2)]
            CT = [A(f"cct{i}", [128, 8, 128], BF16) for i in range(2)]
            zT = [A(f"czt{i}", [128, 16, 128], BF16) for i in range(2)]
            dt_ = A("cdt", [128, 32], F32)
            dtw = A("cdtw", [128, 32], F32)
            a_ = A("ca", [128, 32], F32)
            ex = A("cex", [128, 3, 32], F32)
            abig = A("cabig", [128, 32, 64], F32)
            xdt = A("cxdt", [128, 32, 64], BF16)
            xdtw = A("cxdtw", [128, 32, 64], BF16)
            Btok = A("cbtok", [128, 8, 128], BF16)
            ab = [A(f"cab{i}", [128, 4, 128], F32) for i in range(2)]
            CBm = [A(f"ccbm{i}", [128, 128], F32) for i in range(2)]
            Lm = [A(f"clm{i}", [128, 512], F32) for i in range(2)]
            Gm = [A(f"cgm{i}", [128, 4, 128], BF16) for i in range(2)]
            ds = [A(f"cds{i}", [128, 256], F32) for i in range(2)]
            t1 = [A(f"ct1{i}", [128, 256], F32) for i in range(2)]
            yT = A("cyT", [128, 16, 128], F32)
            yg = A("cyg", [128, 16, 128], F32)
            sq = A("csq", [128, 16, 128], F32)
            rs = A("crs", [128, 128], F32)
            yo = [A(f"cyo{i}", [128, 16, 128], BF16) for i in range(2)]
            sdd, sng = PK('sdd'), PK('sng')
            memset_('dve', H[:], 0.0)
            memset_('pool', Hb[:], 0.0)
            act_(expA[:], PK('alog'), AF.Exp)
            for c in range(16):
                csl = slice(c * 128, (c + 1) * 128)
                b = c % 2
                kb.dma(xsT[b][:], xbc_d[:, 0:16, csl], f'cxs{b}')
                kb.dma(BT[b][:], xbc_d[:, 16:24, csl], f'cbt{b}')
                kb.dma(CT[b][:], xbc_d[:, 24:32, csl], f'cct{b}')
                kb.dma(zT[b][:], z_d[:, :, csl], f'czt{b}')
                act_(dt_[:], dtraw[:, c, :], AF.Exp)
                act_(dt_[:], dt_[:], AF.Ln, bias=EPSC[:, 1:2])
                stt_(a_[:], dt_[:], -1.0, expA[:], ALU.mult, ALU.mult)
                p3 = P()
                mm(p3[:, 0:32], mle, a_[:])
                mm(p3[:, 32:64], mgt, a_[:])
                mm(p3[:, 64:96], onesf, a_[:])
                act_(ex[:], p3[:, 0:96].rearrange("p (k h) -> p k h", k=3), AF.Exp)
                tt_('dve', dtw[:], dt_[:], ex[:, 1, :], ALU.mult)
                copy_('pool', abig[:], a_[:, :][:, :, None].broadcast_to([128, 32, 64]))
                for bb in range(2):
                    pT = P()[:, :].bitcast(BF16)
                    for j in range(8):
                        tr(pT[:, j * 128:(j + 1) * 128], xsT[b][:, bb * 8 + j, :], identb)
                    pv = pT.rearrange("p (h d) -> p h d", h=16)
                    hsl = slice(bb * 16, (bb + 1) * 16)
                    tt_('dve', xdt[:, hsl, :], pv, dt_[:, hsl][:, :, None].broadcast_to([128, 16, 64]), ALU.mult)
                    tt_('dve', xdtw[:, hsl, :], pv, dtw[:, hsl][:, :, None].broadcast_to([128, 16, 64]), ALU.mult)
                pB = P()[:, :].bitcast(BF16)
                for g in range(8):
                    tr(pB[:, g * 128:(g + 1) * 128], BT[b][:, g, :], identb)
                copy_('act', Btok[:], pB.rearrange("p (g n) -> p g n", g=8))
                for g in range(8):
                    i2 = g % 2
                    pcb = P()
                    mm(pcb[:, 0:128], BT[b][:, g, :], CT[b][:, g, :])
                    tt_('dve', CBm[i2][:], pcb[:, 0:128], mle, ALU.mult)
                    tt_('pool', ab[i2][:], mgt[:, None, :].broadcast_to([128, 4, 128]),
                        a_[:, 4 * g:4 * g + 4][:, :, None].broadcast_to([128, 4, 128]), ALU.mult)
                    pD = P()
                    for h in range(4):
                        mm(pD[:, h * 128:(h + 1) * 128], ab[i2][:, h, :], mle)
                    act_(Lm[i2][:], pD[:, :], AF.Exp)
                    tt_('dve', Gm[i2][:], Lm[i2][:, :].rearrange("p (h l) -> p h l", h=4),
                        CBm[i2][:, None, :].broadcast_to([128, 4, 128]), ALU.mult)
                    pY = P()
                    for hp in range(2):
                        for hh in range(2):
                            h = 2 * hp + hh
                            mm(pY[hh * 64:(hh + 1) * 64, hp * 128:(hp + 1) * 128], xdt[:, 4 * g + h, :], Gm[i2][:, h, :])
                    pZ = P()
                    for hp in range(2):
                        h0 = 4 * g + 2 * hp
                        mm(pZ[:, hp * 128:(hp + 1) * 128], Hb[:, h0:h0 + 2, :].rearrange("p h d -> p (h d)"), CT[b][:, g, :])
                    pS = P()
                    for hp in range(2):
                        h0 = 4 * g + 2 * hp
                        mm(pS[:, hp * 128:(hp + 1) * 128], abig[:, h0:h0 + 2, :].rearrange("p h d -> p (h d)"), mle)
                    act_(ds[i2][:], pS[:, 0:256], AF.Exp)
                    tt_('dve', t1[i2][:], pZ[:, 0:256], ds[i2][:], ALU.mult)
                    tt_('dve', yT[:, 2 * g:2 * g + 2, :], pY[:, 0:256].rearrange("p (f l) -> p f l", f=2),
                        t1[i2][:, :].rearrange("p (f l) -> p f l", f=2), ALU.add)
                for fc in range(16):
                    stt_(yT[:, fc, :], xsT[b][:, fc, :], sdd[:, fc:fc + 1], yT[:, fc, :], ALU.mult, ALU.add)
                tt_('dve', H[:], H[:], ex[:, 2, :][:, :, None].broadcast_to([128, 32, 64]), ALU.mult)
                for gp in range(4):
                    pst = P()
                    for gg in range(2):
                        g = 2 * gp + gg
                        mm(pst[:, gg * 256:(gg + 1) * 256], Btok[:, g, :],
                           xdtw[:, 4 * g:4 * g + 4, :].rearrange("p h d -> p (h d)"))
                    tt_('dve', H[:, 8 * gp:8 * gp + 8, :], pst[:, :].rearrange("p (h d) -> p h d", h=8),
                        H[:, 8 * gp:8 * gp + 8, :], ALU.add)
                copy_('pool', Hb[:], H[:])
                tt_('dve', yg[:], yT[:], zT[b][:], ALU.mult)
                act_(sq[:], yg[:], AF.Square)
                pss = P()
                for fc in range(16):
                    mm(pss[:, 0:128], onesf, sq[:, fc, :], fc == 0, fc == 15)
                act_(rs[:], pss[:, 0:128], AF.Sqrt, bias=EPSC[:, 0:1], scale=1.0 / 2048)
                V('dve', lambda: nc.vector.reciprocal(out=rs[:], in_=rs[:]), [rs[:]], [rs[:]])
                tt_('dve', yg[:], yg[:], rs[:, None, :].broadcast_to([128, 16, 128]), ALU.mult)
                tt_('pool', yo[b][:], yg[:], sng[:, :, None].broadcast_to([128, 16, 128]), ALU.mult)
                kb.dma(ys_d[:, :, csl], yo[b][:], f'cyo{b}')
            kb.barrier()

    def merge_phase(l):
        with ExitStack() as es0:
            mT = es0.enter_context(nc.sbuf_tensor(f"gmT_{l}", [128, 8, S], BF16))
            with ExitStack() as es:
                A = lambda n, sh, dt: es.enter_context(nc.sbuf_tensor(f"{n}_{l}", sh, dt))
                wa = A("gwa", [64, 8, 512], BF16)
                ws = A("gws", [128, 16, 512], BF16)
                wl = A("gwl", [80, 16, 512], BF16)
                wm = A("gwm", [128, 3, 8, 512], BF16)
                ya = A("gya", [64, 8, 512], BF16)
                ys = A("gys", [128, 16, 512], BF16)
                yl = A("gyl", [80, 16, 512], BF16)
                sgt = [A(f"gsg{i}", [128, 512], F32) for i in range(2)]
                mac = [A(f"gmac{i}", [128, 512], F32) for i in range(2)]
                tmp = [A(f"gtmp{i}", [128, 512], F32) for i in range(2)]
                n = 0
                for half in range(2):
                    hsl = slice(half * 512, (half + 1) * 512)
                    kb.dma(wa[:], wba_d[l].rearrange("(h d) c -> d h c", d=64)[:, :, hsl], 'gwa', q='pool')
                    for k0 in range(0, 16, 8):
                        kb.dma(ws[:, k0:k0 + 8, :], wbs_d[l].rearrange("(k p) c -> p k c", p=128)[:, k0:k0 + 8, hsl], 'gws', q='pool')
                        kb.dma(wl[:, k0:k0 + 8, :], wbl_d[l].rearrange("(k p) c -> p k c", p=80)[:, k0:k0 + 8, hsl], 'gwl', q='pool')
                    for b in range(3):
                        c0 = OMG + b * 1024 + half * 512
                        kb.dma(wm[:, b, :, :], winv(l)[:, :, c0:c0 + 512], 'gwm', q='pool')
                    for tt in range(4):
                        tsl = slice(tt * 512, (tt + 1) * 512)
                        kb.dma(ya[:], ya_d[:, :, tsl], 'gya')
                        kb.dma(ys[:], ys_d[:, :, tsl], 'gys')
                        kb.dma(yl[:], yl_d[:, :, tsl], 'gyl')
                        for oc in range(4):
                            osl = slice(oc * 128, (oc + 1) * 128)
                            M = mac[n % 2]
                            n += 1
                            for b, (y, w, nk) in enumerate(((ya, wa, 8), (ys, ws, 16), (yl, wl, 16))):
                                pP = P()
                                for k in range(nk):
                                    mm(pP[:, :], w[:, k, osl], y[:, k, :], k == 0, k == nk - 1)
                                pG = P()
                                for kc in range(8):
                                    mm(pG[:, :], wm[:, b, kc, osl], hT[:, kc, tsl], kc == 0, kc == 7)
                                SG = sgt[b % 2]
                                act_(SG[:], pG[:, :], AF.Sigmoid)
                                if b == 0:
                                    tt_('dve', M[:], SG[:], pP[:, :], ALU.mult)
                                else:
                                    T = tmp[b % 2]
                                    tt_('dve', T[:], SG[:], pP[:, :], ALU.mult)
                                    tt_('pool', M[:], M[:], T[:], ALU.add)
                            copy_('pool', mT[:, half * 4 + oc, tsl], M[:])
                kb.barrier()
            with ExitStack() as es:
                A = lambda n, sh, dt: es.enter_context(nc.sbuf_tensor(f"{n}_{l}", sh, dt))
                wo = A("gwo", [128, 8, 1024], BF16)
                X = [A(f"gX{i}", [128, 8, 512], F32) for i in range(2)]
                for c0 in range(0, 1024, 512):
                    kb.dma(wo[:, :, c0:c0 + 512], wo_d[l].rearrange("(kc p) c -> p kc c", p=128)[:, :, c0:c0 + 512], 'gwo', q='pool')
                for tt in range(4):
                    tsl = slice(tt * 512, (tt + 1) * 512)
                    Xt = X[tt % 2]
                    kb.dma(Xt[:], xs_d[:, :, tsl], f'gX{tt % 2}')
                    for oc in range(8):
                        p = P()
                        for kc in range(8):
                            mm(p[:, :], wo[:, kc, oc * 128:(oc + 1) * 128], mT[:, kc, tsl], kc == 0, kc == 7)
                        stt_(Xt[:, oc, :], p[:, :], cond[:, 16 + oc:17 + oc], Xt[:, oc, :], ALU.mult, ALU.add)
                    kb.dma(xs_d[:, :, tsl], Xt[:], f'gX{tt % 2}')
                kb.barrier()

    def moe_phase(l):
        with ExitStack() as es:
            A = lambda n, sh, dt: es.enter_context(nc.sbuf_tensor(f"{n}_{l}", sh, dt))
            yacc = A("myacc", [128, 16, 1024], F32)
            wt = A("mwt", [128, 16, 32], F32)
            wrt = A("mwrt", [128, 8, 36], BF16)
            lg = A("mlg", [128, 36], F32)
            sm = A("msm", [128, 16], F32)
            oh = A("moh", [128, 4], F32)
            pen = A("mpen", [128, 4], F32)
            eg4 = A("meg4", [128, 4], F32)
            lem = A("mlem", [128, 4, 8], F32)
            m8 = A("mm8", [128, 8], F32)
            eq1 = A("meq1", [128, 32], F32)
            eq2 = A("meq2", [128, 32], F32)
            actb = [A(f"mact{i}", [128, 4, S], BF16) for i in range(2)]
            wg = [A(f"mwg{i}", [128, 8, 512], BF16) for i in range(2)]
            wu = [A(f"mwu{i}", [128, 8, 512], BF16) for i in range(2)]
            wd = [A(f"mwd{i}", [128, 4, 1024], BF16) for i in range(2)]
            sgt = [A(f"msg{i}", [128, 512], F32) for i in range(2)]
            X = [A(f"mX{i}", [128, 8, 128], F32) for i in range(2)]
            kb.dma(wrt[:, :, 0:4], rwg_d[l].rearrange("(kc p) g -> p kc g", p=128), 'mwrt', q='pool')
            kb.dma(wrt[:, :, 4:36], rwe_d[l].rearrange("(kc p) g -> p kc g", p=128), 'mwrt', q='pool')
            rb = PK('rb')
            for t in range(16):
                tk = slice(t * 128, (t + 1) * 128)
                p = P()
                for kc in range(8):
                    mm(p[:, 0:36], hT[:, kc, tk], wrt[:, kc, :], kc == 0, kc == 7)
                tt_('dve', lg[:], p[:, 0:36], rb, ALU.add)
                c = lambda i: sm[:, i:i + 1]
                V('dve', lambda: nc.vector.reduce_max(out=c(0), in_=lg[:, 0:4], axis=AX.X), [c(0)], [lg[:, 0:4]])
                ts_('dve', oh[:], lg[:, 0:4], c(0), None, ALU.is_ge)
                ts_('dve', c(1), c(0), -1.0, None, ALU.mult)
                act_(eg4[:], lg[:, 0:4], AF.Exp, bias=c(1))
                V('dve', lambda: nc.vector.reduce_sum(out=c(2), in_=eg4[:], axis=AX.X), [c(2)], [eg4[:]])
                V('dve', lambda: nc.vector.reciprocal(out=c(3), in_=c(2)), [c(3)], [c(2)])
                ts_('dve', pen[:], oh[:], -1.0, 30000.0, ALU.add, ALU.mult)
                tt_('dve', lem[:], lg[:, 4:36].rearrange("p (g e) -> p g e", g=4),
                    oh[:, :][:, :, None].broadcast_to([128, 4, 8]), ALU.mult)
                tt_('dve', lem[:], lem[:], pen[:, :][:, :, None].broadcast_to([128, 4, 8]), ALU.add)
                lemf = lem[:, :, :].rearrange("p g e -> p (g e)")
                V('dve', lambda: nc.vector.max(out=m8[:], in_=lemf), [m8[:]], [lemf])
                ts_('dve', eq1[:], lemf, m8[:, 0:1], None, ALU.is_ge)
                ts_('dve', eq2[:], lemf, m8[:, 1:2], None, ALU.is_ge)
                tt_('dve', c(4), m8[:, 1:2], m8[:, 0:1], ALU.subtract)
                act_(c(5), c(4), AF.Exp)
                ts_('dve', c(6), c(5), 1.0, None, ALU.add)
                V('dve', lambda: nc.vector.reciprocal(out=c(7), in_=c(6)), [c(7)], [c(6)])
                tt_('dve', c(8), c(3), c(7), ALU.mult)
                tt_('dve', c(9), c(8), c(5), ALU.mult)
                tt_('dve', c(10), c(8), c(9), ALU.subtract)
                ts_('dve', wt[:, t, :], eq2[:], c(9), None, ALU.mult)
                stt_(wt[:, t, :], eq1[:], c(10), wt[:, t, :], ALU.mult, ALU.add)
            for e in range(NEXP):
                b = e % 2
                kb.dma(wg[b][:], eg_d[l, e].rearrange("(kc p) f -> p kc f", p=128), f'mwg{b}', q='pool')
                kb.dma(wu[b][:], eu_d[l, e].rearrange("(kc p) f -> p kc f", p=128), f'mwu{b}', q='pool')
                for c0 in range(0, 1024, 512):
                    kb.dma(wd[b][:, :, c0:c0 + 512], ed_d[l, e].rearrange("(k p) c -> p k c", p=128)[:, :, c0:c0 + 512], f'mwd{b}', q='pool')
                n = 0
                for fcn in range(4):
                    fsl = slice(fcn * 128, (fcn + 1) * 128)
                    for tt in range(4):
                        tsl = slice(tt * 512, (tt + 1) * 512)
                        pg = P()
                        for kc in range(8):
                            mm(pg[:, :], wg[b][:, kc, fsl], hT[:, kc, tsl], kc == 0, kc == 7)
                        pu = P()
                        for kc in range(8):
                            mm(pu[:, :], wu[b][:, kc, fsl], hT[:, kc, tsl], kc == 0, kc == 7)
                        SG = sgt[n % 2]
                        n += 1
                        act_(SG[:], pg[:, :], AF.Silu)
                        tt_('dve', actb[b][:, fcn, tsl], SG[:], pu[:, :], ALU.mult)
                for t in range(16):
                    tk = slice(t * 128, (t + 1) * 128)
                    for half in range(2):
                        hsl = slice(half * 512, (half + 1) * 512)
                        pd = P()
                        for k in range(4):
                            mm(pd[:, :], actb[b][:, k, tk], wd[b][:, k, hsl], k == 0, k == 3)
                        if e == 0:
                            ts_('dve', yacc[:, t, hsl], pd[:, :], wt[:, t, e:e + 1], None, ALU.mult)
                        else:
                            stt_(yacc[:, t, hsl], pd[:, :], wt[:, t, e:e + 1], yacc[:, t, hsl], ALU.mult, ALU.add)
            for t in range(16):
                tk = slice(t * 128, (t + 1) * 128)
                Xt = X[t % 2]
                kb.dma(Xt[:], xs_d[:, :, tk], f'mX{t % 2}')
                for half in range(2):
                    p = P()
                    for j in range(4):
                        kc = half * 4 + j
                        tr(p[:, j * 128:(j + 1) * 128], yacc[:, t, kc * 128:(kc + 1) * 128], identf)
                    for j in range(4):
                        kc = half * 4 + j
                        stt_(Xt[:, kc, :], p[:, j * 128:(j + 1) * 128], cond[:, 40 + kc:41 + kc], Xt[:, kc, :], ALU.mult, ALU.add)
                kb.dma(xs_d[:, :, tk], Xt[:], f'mX{t % 2}')
            kb.barrier()

    def final_phase():
        with ExitStack() as es:
            A = lambda n, sh, dt: es.enter_context(nc.sbuf_tensor(n, sh, dt))
            xt = [A(f"fx{i}", [128, 8, 512], F32) for i in range(2)]
            sq = [A(f"fsq{i}", [128, 512], F32) for i in range(2)]
            rs = A("frs", [128, 512], F32)
            yf = A("fyf", [128, 8, 512], F32)
            ot = [A(f"fot{i}", [128, 1024], F32) for i in range(2)]
            fg = G('fg')
            n = 0
            for tt in range(4):
                Xt = xt[tt % 2]
                kb.dma(Xt[:], xs_d[:, :, tt * 512:(tt + 1) * 512], f'fx{tt % 2}')
                pss = P()
                for kc in range(8):
                    act_(sq[kc % 2][:], Xt[:, kc, :], AF.Square)
                    mm(pss[:, :], onesf, sq[kc % 2][:], kc == 0, kc == 7)
                act_(rs[:], pss[:, :], AF.Sqrt, bias=EPSC[:, 0:1], scale=1.0 / D)
                V('dve', lambda: nc.vector.reciprocal(out=rs[:], in_=rs[:]), [rs[:]], [rs[:]])
                for kc in range(8):
                    stt_(yf[:, kc, :], Xt[:, kc, :], fg[:, kc:kc + 1], rs[:], ALU.mult, ALU.mult)
                for t4 in range(4):
                    O = ot[n % 2]
                    n += 1
                    for half in range(2):
                        p = P()
                        for j in range(4):
                            kc = half * 4 + j
                            tr(p[:, j * 128:(j + 1) * 128], yf[:, kc, t4 * 128:(t4 + 1) * 128], identf)
                        copy_('act' if half else 'dve', O[:, half * 512:(half + 1) * 512], p[:, :])
                    r0 = tt * 512 + t4 * 128
                    kb.dma(out_d[r0:r0 + 128, :], O[:], f'fot{(n - 1) % 2}')
            kb.barrier()

    ph = phases
    load_x_phase()
    if ph is None or 'rope' in ph:
        rope_phase()
    for l in range(L):
        if ph is None or 'cond' in ph:
            cond_phase(l)
        if ph is None or 'norm1' in ph:
            norm_phase(l, 0, f"a{l}")
        if ph is None or 'attn' in ph:
            attn_phase(l)
        if ph is None or 'lru' in ph:
            lru_phase(l)
        if ph is None or 'ssd' in ph:
            ssd_proj_phase(l)
            ssd_scan_phase(l)
        if ph is None or 'merge' in ph:
            merge_phase(l)
        if ph is None or 'moe' in ph:
            norm_phase(l, 1, f"b{l}")
            moe_phase(l)
    if ph is None or 'final' in ph:
        final_phase()
    kb.finish()
    return nc, kb


_CACHE = {}


def _in_maps(inp, L, cores):
    pk = np.stack([_pack_layer(inp, l) for l in range(L)])
    maps = []
    shared = {
        "pk": pk,
        "ada_w": inp['ada_w'][:L], "w_in": inp['w_in'][:L],
        "lru_w_r": inp['lru_w_r'][:L], "lru_w_i": inp['lru_w_i'][:L],
        "w_br_attn": inp['w_br_attn'][:L], "w_br_ssd": inp['w_br_ssd'][:L], "w_br_lru": inp['w_br_lru'][:L],
        "w_out": inp['w_out'][:L], "router_wg": inp['router_wg'][:L],
        "router_we": inp['router_we'][:L].reshape(L, D, 32),
        "exp_w_gate": inp['exp_w_gate'][:L], "exp_w_up": inp['exp_w_up'][:L], "exp_w_down": inp['exp_w_down'][:L],
    }
    for b in cores:
        m = dict(shared)
        m["x"] = np.ascontiguousarray(inp['x'][b])
        m["pos"] = np.ascontiguousarray(np.broadcast_to(inp['positions'][b][None, :], (128, S))).astype(np.int32)
        m["gk"] = _pack_global(inp['c'][b], inp['final_g'])
        maps.append(m)
    return maps


def kernel(**inputs):
    inp = {k: np.asarray(v) for k, v in inputs.items()}
    if 'nc' not in _CACHE:
        _CACHE['nc'] = build(DEPTH)[0]
    nc = _CACHE['nc']
    maps = _in_maps(inp, DEPTH, list(range(8)))
    res = run_bass_kernel_spmd(nc, maps, core_ids=list(range(8)))
    out = np.stack([np.asarray(res.results[c]["out"]) for c in range(8)], axis=0)
    return out.astype(np.float32)
```

```python
import numpy as np
import concourse.bass as bass
import concourse.mybir as mybir
from concourse.bass_utils import run_bass_kernel_spmd

F32 = mybir.dt.float32
BF16 = mybir.dt.bfloat16
I32 = mybir.dt.int32
ALU = mybir.AluOpType
AF = mybir.ActivationFunctionType
AX = mybir.AxisListType

_DTSIZE = {F32: 4, BF16: 2, I32: 4}
SEM_ROT = 30000

D = 1024
S = 2048
DEPTH = 4
NEXP = 32
FF = 512
INC = 16416
EPS = 1e-6
OQ, OK_, OV, OZ, OXBC, ODT, OLG, OLX, OMG = 0, 1536, 3072, 4608, 6656, 10752, 10784, 12064, 13344


def _box(ap):
    t = ap.tensor
    es = _DTSIZE[ap.dtype]
    pat = ap.ap
    off = int(ap.offset)
    if type(t).__name__.startswith('DRam'):
        ext = 1
        for st, cnt in pat:
            ext += (cnt - 1) * abs(st)
        return (t.name, 0, 1, off * es, (off + ext) * es)
    if type(t).__name__.startswith('PSum'):
        return (t.name, 0, 128, 0, 2048)
    free = 1
    for s in list(t.shape)[1:]:
        free *= s
    p0 = off // free
    f0 = off % free
    np_ = pat[0][1] if pat[0][0] != 0 else 1
    ext = 1
    for st, cnt in pat[1:]:
        ext += (cnt - 1) * abs(st)
    return (t.name, p0, p0 + np_, f0 * es, (f0 + ext) * es)


class Chan:
    def __init__(self, kb, name):
        self.kb = kb
        self.name = name
        self.sem = kb.nc.alloc_semaphore(name)
        kb.chan_by_sem[self.sem.num] = self
        self.cnt = 0
        self.gen = 0

    def next_event(self):
        if self.cnt + 16 > SEM_ROT:
            self.gen += 1
            self.sem = self.kb.nc.alloc_semaphore(f"{self.name}_g{self.gen}")
            self.kb.chan_by_sem[self.sem.num] = self
            self.cnt = 0
        self.cnt += 16
        return (self.sem, self.cnt)


class KB:
    def __init__(self, nc):
        self.nc = nc
        self.engs = {'pe': nc.tensor, 'act': nc.scalar, 'dve': nc.vector, 'pool': nc.gpsimd, 'sp': nc.sync}
        self.sem, self.cnt, self.gen = {}, {}, {}
        for e in ('pe', 'act', 'dve', 'pool'):
            self.sem[e] = nc.alloc_semaphore(f"s_{e}")
            self.cnt[e] = 0
            self.gen[e] = 0
        self.oldsems = []
        self.seen = {e: {} for e in self.engs}
        self.recs = {}
        self.pending = {e: False for e in self.engs}
        self.ninst = {e: 0 for e in self.engs}
        self.chans = {}
        self.chan_by_sem = {}

    def chan(self, name):
        if name not in self.chans:
            self.chans[name] = Chan(self, name)
        return self.chans[name]

    def _deps(self, ins, outs):
        deps = []
        for ap in ins:
            b = _box(ap)
            for r in self.recs.get(b[0], ()):
                if r[5] and r[1] < b[2] and b[1] < r[2] and r[3] < b[4] and b[3] < r[4]:
                    deps.append(r[6])
        for ap in outs:
            b = _box(ap)
            for r in self.recs.get(b[0], ()):
                if r[1] < b[2] and b[1] < r[2] and r[3] < b[4] and b[3] < r[4]:
                    deps.append(r[6])
        return deps

    def _record(self, ins, outs, ev, tag):
        for ap in outs:
            b = _box(ap)
            lst = self.recs.setdefault(b[0], [])
            lst[:] = [r for r in lst if not (b[1] <= r[1] and r[2] <= b[2] and b[3] <= r[3] and r[4] <= b[4])]
            lst.append((b[0], b[1], b[2], b[3], b[4], True, ev, tag))
        for ap in ins:
            b = _box(ap)
            lst = self.recs.setdefault(b[0], [])
            lst[:] = [r for r in lst if not ((not r[5]) and r[7] == tag and r[1:5] == b[1:5])]
            lst.append((b[0], b[1], b[2], b[3], b[4], False, ev, tag))

    def _wait(self, e, deps, skip_self=False):
        eng = self.engs[e]
        seen = self.seen[e]
        best = {}
        for sem, val in deps:
            if skip_self and e in self.sem and sem is self.sem[e]:
                continue
            k = sem.num
            ch = self.chan_by_sem.get(k)
            if ch is not None and ch.sem.num == k:
                val = max(val, ch.cnt)
            if seen.get(k, 0) >= val:
                continue
            if k not in best or best[k][1] < val:
                best[k] = (sem, val)
        for k, (sem, val) in best.items():
            eng.wait_ge(sem, val)
            seen[k] = val

    def op(self, e, fn, outs, ins, signal=True):
        deps = self._deps(ins, outs)
        self._wait(e, deps, skip_self=(e == 'pe'))
        inst = fn()
        self.ninst[e] += 1
        if signal and self.cnt[e] + 1 > SEM_ROT:
            self.oldsems.append((self.sem[e], self.cnt[e]))
            self.gen[e] += 1
            self.sem[e] = self.nc.alloc_semaphore(f"s_{e}_g{self.gen[e]}")
            self.cnt[e] = 0
        if signal:
            self.cnt[e] += 1
            inst.then_inc(self.sem[e], 1)
            ev = (self.sem[e], self.cnt[e])
            self.pending[e] = False
        else:
            assert self.cnt[e] + 1 <= SEM_ROT
            ev = (self.sem[e], self.cnt[e] + 1)
            self.pending[e] = True
        self._record(ins, outs, ev, e)
        return inst

    def dma(self, out, in_, chan, q='sp', **kw):
        ch = self.chan(chan)
        deps = self._deps([in_], [out])
        self._wait(q, deps)
        ev = ch.next_event()
        inst = self.engs[q].dma_start(out=out, in_=in_, **kw)
        inst.then_inc(ev[0], 16)
        self.ninst[q] += 1
        self._record([in_], [out], ev, 'dma_' + ch.name)
        return inst

    def all_events(self):
        evs = [(self.sem[e], self.cnt[e]) for e in ('pe', 'act', 'dve', 'pool') if self.cnt[e] > 0]
        evs += [(ch.sem, ch.cnt) for ch in self.chans.values() if ch.cnt > 0]
        return evs

    def barrier(self, drop=()):
        for e in ('pe', 'act', 'dve', 'pool'):
            assert not self.pending[e], e
        evs = self.all_events()
        for e in self.engs:
            self._wait(e, evs)
        self.recs.clear()

    def finish(self):
        for e in ('pe', 'act', 'dve', 'pool'):
            assert not self.pending[e], e
        self._wait('sp', self.all_events())


PK_COLS = {}
_o = 0
for _n, _w in (('n1g', 8), ('n2g', 8), ('adab', 48), ('scw', 128), ('scb', 32), ('dtb', 32), ('alog', 32),
               ('sdd', 16), ('sng', 16), ('rb', 36), ('lcw', 64), ('lcb', 16), ('lbr', 16), ('lbi', 16), ('llam', 16)):
    PK_COLS[_n] = (_o, _w)
    _o += _w
NPK = _o
GK_COLS = {}
_o = 0
for _n, _w in (('c', 8), ('fg', 8), ('freq', 1), ('ident', 128), ('mle', 128), ('mgt', 128), ('mge', 128), ('ones', 128),
               ('phase', 1)):
    GK_COLS[_n] = (_o, _w)
    _o += _w
NGK = _o


def _fm(v, p=128):
    return np.ascontiguousarray(v.reshape(-1, p).T)


def _pack_layer(inp, l):
    pk = np.zeros((128, NPK), np.float32)

    def put(name, arr):
        o, w = PK_COLS[name]
        assert arr.shape[1] == w, (name, arr.shape)
        pk[:arr.shape[0], o:o + w] = arr
    put('n1g', _fm(inp['norm1_g'][l]))
    put('n2g', _fm(inp['norm2_g'][l]))
    put('adab', _fm(inp['ada_b'][l]))
    scw = inp['ssd_conv_w'][l]
    put('scw', np.ascontiguousarray(scw.T.reshape(32, 128, 4).transpose(1, 0, 2)).reshape(128, 128))
    put('scb', _fm(inp['ssd_conv_b'][l]))
    put('dtb', np.broadcast_to(inp['ssd_dt_bias'][l][None, :], (128, 32)))
    put('alog', np.broadcast_to(inp['ssd_a_log'][l][None, :], (128, 32)))
    put('sdd', _fm(np.repeat(inp['ssd_d'][l], 64)))
    put('sng', _fm(inp['ssd_norm_g'][l]))
    rb = np.concatenate([inp['router_bg'][l].reshape(-1), inp['router_be'][l].reshape(-1)])
    put('rb', np.broadcast_to(rb[None, :], (128, 36)))
    lcw = inp['lru_conv_w'][l]
    put('lcw', np.ascontiguousarray(lcw.T.reshape(16, 80, 4).transpose(1, 0, 2)).reshape(80, 64))
    put('lcb', _fm(inp['lru_conv_b'][l], 80))
    put('lbr', _fm(inp['lru_b_r'][l], 80))
    put('lbi', _fm(inp['lru_b_i'][l], 80))
    put('llam', _fm(inp['lru_lambda'][l], 80))
    return pk


def _pack_global(c_b, final_g):
    gk = np.zeros((128, NGK), np.float32)

    def put(name, arr):
        o, w = GK_COLS[name]
        gk[:, o:o + w] = arr
    put('c', _fm(c_b))
    put('fg', _fm(final_g))
    half = 32
    freqs = (10000.0 ** (-np.arange(half, dtype=np.float32) / half)).astype(np.float32)
    put('freq', np.tile(freqs, 4)[:, None])
    i = np.arange(128)
    put('ident', (i[:, None] == i[None, :]).astype(np.float32))
    put('mle', (i[:, None] <= i[None, :]).astype(np.float32))
    put('mgt', (i[:, None] > i[None, :]).astype(np.float32))
    put('mge', (i[:, None] >= i[None, :]).astype(np.float32))
    put('ones', np.ones((128, 128), np.float32))
    put('phase', np.where((i % 64) < 32, -1.0, 1.0).astype(np.float32)[:, None])
    return gk


from contextlib import ExitStack
import math


def build(L=DEPTH, dbg=False, phases=None):
    nc = bass.Bass("TRN2", target_bir_lowering=False)
    kb = KB(nc)

    def din(name, shape, dt=F32):
        return nc.dram_tensor(name, list(shape), dt, kind="ExternalInput").ap()

    def dscr(name, shape, dt):
        return nc.dram_tensor(name, list(shape), dt, kind=("ExternalOutput" if dbg else "Internal")).ap()

    x_d = din("x", [S, D])
    pos_d = din("pos", [128, S], I32)
    gk_d = din("gk", [128, NGK])
    pk_d = din("pk", [L, 128, NPK])
    adaw_d = din("ada_w", [L, D, 6 * D])
    win_d = din("w_in", [L, D, INC])
    lwr_d = din("lru_w_r", [L, 16, 80, 80])
    lwi_d = din("lru_w_i", [L, 16, 80, 80])
    wba_d = din("w_br_attn", [L, 512, D])
    wbs_d = din("w_br_ssd", [L, 2048, D])
    wbl_d = din("w_br_lru", [L, 1280, D])
    wo_d = din("w_out", [L, D, D])
    rwg_d = din("router_wg", [L, D, 4])
    rwe_d = din("router_we", [L, D, 32])
    eg_d = din("exp_w_gate", [L, NEXP, D, FF])
    eu_d = din("exp_w_up", [L, NEXP, D, FF])
    ed_d = din("exp_w_down", [L, NEXP, FF, D])
    out_d = nc.dram_tensor("out", [S, D], F32, kind="ExternalOutput").ap()

    xs_d = dscr("xs_scr", [128, 8, S], F32)
    ya_d = dscr("ya_scr", [64, 8, S], BF16)
    ys_d = dscr("ys_scr", [128, 16, S], BF16)
    yl_d = dscr("yl_scr", [80, 16, S], BF16)
    xbc_d = nc.dram_tensor("xbc_scr", [128, 32, S], BF16).ap()
    z_d = nc.dram_tensor("z_scr", [128, 16, S], BF16).ap()
    cs_d = nc.dram_tensor("cs_scr", [128, 2, S], F32).ap()

    ps = [nc.alloc_psum_tensor(f"ps{i}", [128, 512], F32) for i in range(8)]
    pctr = [0]

    def P():
        t = ps[pctr[0] % 8]
        pctr[0] += 1
        return t

    def mm(out, lhsT, rhs, start=True, stop=True):
        return kb.op('pe', lambda: nc.tensor.matmul(out, lhsT=lhsT, rhs=rhs, start=start, stop=stop),
                     [out], [lhsT, rhs], signal=True)

    def tr(out, in_, ident):
        return kb.op('pe', lambda: nc.tensor.transpose(out=out, in_=in_, identity=ident), [out], [in_, ident])

    def V(e, fn, outs, ins):
        return kb.op(e, fn, outs, ins)

    def tt_(e, out, in0, in1, op):
        eng = kb.engs[e]
        return kb.op(e, lambda: eng.tensor_tensor(out=out, in0=in0, in1=in1, op=op), [out], [in0, in1])

    def ts_(e, out, in0, s1, s2, op0, op1=ALU.bypass):
        eng = kb.engs[e]
        ins = [in0] + [s for s in (s1, s2) if not isinstance(s, (int, float)) and s is not None]
        if s2 is None:
            return kb.op(e, lambda: eng.tensor_scalar(out=out, in0=in0, scalar1=s1, scalar2=None, op0=op0), [out], ins)
        return kb.op(e, lambda: eng.tensor_scalar(out=out, in0=in0, scalar1=s1, scalar2=s2, op0=op0, op1=op1), [out], ins)

    def stt_(out, in0, scalar, in1, op0, op1):
        ins = [in0, in1] + ([] if isinstance(scalar, (int, float)) else [scalar])
        return kb.op('dve', lambda: nc.vector.scalar_tensor_tensor(out=out, in0=in0, scalar=scalar, in1=in1, op0=op0, op1=op1),
                     [out], ins)

    def act_(out, in_, func, bias=0.0, scale=1.0):
        ins = [in_] + [s for s in (bias, scale) if not isinstance(s, (int, float))]
        return kb.op('act', lambda: nc.scalar.activation(out=out, in_=in_, func=func, bias=bias, scale=scale), [out], ins)

    def copy_(e, out, in_):
        if e == 'act':
            return kb.op('act', lambda: nc.scalar.copy(out=out, in_=in_), [out], [in_])
        eng = kb.engs[e]
        return kb.op(e, lambda: eng.tensor_copy(out=out, in_=in_), [out], [in_])

    def memset_(e, ap, val):
        eng = kb.engs[e]
        return kb.op(e, lambda: eng.memset(ap, val), [ap], [])

    def winv(l):
        return win_d[l].rearrange("(kc p) c -> p kc c", p=128)

    gk = nc.alloc_sbuf_tensor("gk_sb", [128, NGK], F32)
    kb.dma(gk[:], gk_d[:, :], 'c0')

    def G(name):
        o, w = GK_COLS[name]
        return gk[:, o:o + w]
    identf, mle, mgt, mge, onesf = G('ident'), G('mle'), G('mgt'), G('mge'), G('ones')
    cb = nc.alloc_sbuf_tensor("cb", [128, 3, 128], BF16)
    for i, nm in enumerate(('ident', 'mle', 'mge')):
        copy_('dve', cb[:, i, :], G(nm))
    identb = cb[:, 0, :]
    hT = nc.alloc_sbuf_tensor("hT", [128, 8, S], BF16)
    pk = nc.alloc_sbuf_tensor("pk_sb", [128, NPK], F32)
    cond = nc.alloc_sbuf_tensor("cond", [128, 48], F32)
    gm = nc.alloc_sbuf_tensor("gm", [128, 16], F32)
    cact = nc.alloc_sbuf_tensor("cact", [128, 8], F32)
    dtraw = nc.alloc_sbuf_tensor("dtraw", [128, 16, 32], F32)
    act_(cact[:], G('c'), AF.Silu)

    def PK(name, rows=128):
        o, w = PK_COLS[name]
        return pk[0:rows, o:o + w]

    def load_x_phase():
        with ExitStack() as es:
            xin = [es.enter_context(nc.sbuf_tensor(f"xin{i}", [128, D], F32)) for i in range(2)]
            xo = [es.enter_context(nc.sbuf_tensor(f"xo{i}", [128, 8, 128], F32)) for i in range(2)]
            for t in range(16):
                xi = xin[t % 2]
                kb.dma(xi[:], x_d[t * 128:(t + 1) * 128, :], f'xin{t % 2}')
                for half in range(2):
                    p = P()
                    for j in range(4):
                        kc = half * 4 + j
                        tr(p[:, j * 128:(j + 1) * 128], xi[:, kc * 128:(kc + 1) * 128], identf)
                    copy_('dve' if half == 0 else 'act', xo[t % 2][:, half * 4:(half + 1) * 4, :],
                          p[:, :].rearrange("p (j t) -> p j t", j=4))
                kb.dma(xs_d[:, :, t * 128:(t + 1) * 128], xo[t % 2][:], f'xo{t % 2}')
            kb.barrier()

    def rope_phase():
        with ExitStack() as es:
            posi = es.enter_context(nc.sbuf_tensor("posi", [128, S], I32))
            ki = es.enter_context(nc.sbuf_tensor("rki", [128, S], I32))
            ang = es.enter_context(nc.sbuf_tensor("ang", [128, S], F32))
            u = es.enter_context(nc.sbuf_tensor("ru", [128, S], F32))
            kf = es.enter_context(nc.sbuf_tensor("rkf", [128, S], F32))
            r = es.enter_context(nc.sbuf_tensor("rr", [128, S], F32))
            m = es.enter_context(nc.sbuf_tensor("rm", [128, S], F32))
            kb.dma(posi[:], pos_d[:, :], 'c0')
            copy_('dve', ang[:], posi[:])
            ts_('dve', ang[:], ang[:], G('freq'), None, ALU.mult)
            TWO_PI = 2.0 * math.pi
            C1 = 6.28125
            C2 = TWO_PI - C1
            for which, phi in ((0, math.pi / 2), (1, 0.0)):
                ts_('dve', u[:], ang[:], 1.0 / TWO_PI, phi / TWO_PI + 0.5, ALU.mult, ALU.add)
                copy_('dve', ki[:], u[:])
                copy_('dve', kf[:], ki[:])
                stt_(r[:], kf[:], -C1, ang[:], ALU.mult, ALU.add)
                stt_(r[:], kf[:], -C2, r[:], ALU.mult, ALU.add)
                if phi != 0.0:
                    ts_('dve', r[:], r[:], phi, None, ALU.add)
                ts_('dve', m[:], r[:], -math.pi, None, ALU.is_lt)
                stt_(r[:], m[:], TWO_PI, r[:], ALU.mult, ALU.add)
                ts_('dve', m[:], r[:], math.pi, None, ALU.is_gt)
                stt_(r[:], m[:], -TWO_PI, r[:], ALU.mult, ALU.add)
                ts_('dve', r[:], r[:], -3.1415925, 3.1415925, ALU.max, ALU.min)
                act_(u[:], r[:], AF.Sin)
                if which == 1:
                    ts_('dve', u[:], u[:], G('phase'), None, ALU.mult)
                kb.dma(cs_d[:, which, :], u[:], 'cs_st')
            kb.barrier()

    def cond_phase(l):
        with ExitStack() as es:
            aw = [es.enter_context(nc.sbuf_tensor(f"aw{l}_{i}", [128, 8, 512], F32)) for i in range(2)]
            kb.dma(pk[:], pk_d[l], 'pk')
            pc = P()
            src = adaw_d[l].rearrange("(kc p) c -> p kc c", p=128)
            for cc in range(12):
                w = aw[cc % 2]
                kb.dma(w[:], src[:, :, cc * 512:(cc + 1) * 512], f'aw{cc % 2}')
                for j in range(4):
                    col = cc * 4 + j
                    for kc in range(8):
                        mm(pc[:, col:col + 1], w[:, kc, j * 128:(j + 1) * 128], cact[:, kc:kc + 1], kc == 0, kc == 7)
            tt_('dve', cond[:], pc[:, 0:48], PK('adab'), ALU.add)
            stt_(gm[:, 0:8], cond[:, 8:16], 1.0, PK('n1g'), ALU.add, ALU.mult)
            stt_(gm[:, 8:16], cond[:, 32:40], 1.0, PK('n2g'), ALU.add, ALU.mult)
            kb.barrier()

    def norm_phase(l, which, tag):
        with ExitStack() as es:
            xt = [es.enter_context(nc.sbuf_tensor(f"nx{tag}_{i}", [128, 8, 512], F32)) for i in range(2)]
            sq = [es.enter_context(nc.sbuf_tensor(f"nsq{tag}_{i}", [128, 512], F32)) for i in range(2)]
            rs = [es.enter_context(nc.sbuf_tensor(f"nrs{tag}_{i}", [128, 512], F32)) for i in range(2)]
            t2 = [es.enter_context(nc.sbuf_tensor(f"nt2{tag}_{i}", [128, 512], F32)) for i in range(2)]
            sh0 = 0 if which == 0 else 24
            for tt in range(4):
                X = xt[tt % 2]
                kb.dma(X[:], xs_d[:, :, tt * 512:(tt + 1) * 512], f'nx{tt % 2}')
                pss = P()
                for kc in range(8):
                    act_(sq[kc % 2][:], X[:, kc, :], AF.Square)
                    mm(pss[:, :], onesf, sq[kc % 2][:], kc == 0, kc == 7)
                R_ = rs[tt % 2]
                act_(R_[:], pss[:, :], AF.Sqrt, bias=EPSC[:, 0:1], scale=1.0 / D)
                V('dve', lambda: nc.vector.reciprocal(out=R_[:], in_=R_[:]), [R_[:]], [R_[:]])
                for kc in range(8):
                    T = t2[kc % 2]
                    tt_('dve', T[:], X[:, kc, :], R_[:], ALU.mult)
                    ts_('pool', hT[:, kc, tt * 512:(tt + 1) * 512], T[:], gm[:, which * 8 + kc:which * 8 + kc + 1],
                        cond[:, sh0 + kc:sh0 + kc + 1], ALU.mult, ALU.add)
            kb.barrier()

    epsc = nc.alloc_sbuf_tensor("epsc", [128, 2], F32)
    EPSC = epsc
    memset_('dve', epsc[:, 0:1], EPS)
    memset_('dve', epsc[:, 1:2], 1.0)

    def attn_tok(g, ti):
        if g == 0:
            return slice(128 * ti, 128 * ti + 128, 1)
        if g == 1:
            st = 512 * (ti // 4) + (ti % 4)
            return slice(st, st + 4 * 127 + 1, 4)
        return slice(ti, ti + 16 * 127 + 1, 16)

    def attn_prev(g, ti):
        if g == 0:
            return ti - 1 if ti >= 1 else None
        if g == 1:
            return ti - 4 if ti >= 4 else None
        return None

    def attn_phase(l):
        with ExitStack() as es:
            A = lambda n, sh, dt: es.enter_context(nc.sbuf_tensor(f"{n}_{l}", sh, dt))
            cs = A("acs", [128, 2, S], F32)
            qk = A("aqk", [128, 2, 2, S], BF16)
            vaug = A("avaug", [128, 16, 4, 128], BF16)
            acc = A("aacc", [128, 4, S], F32)
            wq = [A(f"awq{i}", [128, 8, 256], BF16) for i in range(3)]
            wst = [A(f"awst{i}", [128, 8, 256], F32) for i in range(2)]
            ta = [A(f"ata{i}", [128, 512], F32) for i in range(2)]
            tb = [A(f"atb{i}", [128, 512], F32) for i in range(2)]
            NPT = 6
            pt = [A(f"apt{i}", [128, 512], BF16) for i in range(NPT)]
            ptm = [A(f"aptm{i}", [128, 4, 128], BF16) for i in range(NPT)]
            rd = A("ard", [64, 4, 512], F32)
            yo = [A(f"ayo{i}", [64, 4, 512], BF16) for i in range(2)]
            kb.dma(cs[:], cs_d[:, :, :], 'acs')
            memset_('pool', vaug[:, :, :, 64:128], 1.0)
            wi = 0
            pi = 0
            for hh in range(2):
                for g in range(3):
                    for which, off in ((0, OQ), (1, OK_)):
                        c0 = off + g * 512 + hh * 256
                        W = wq[wi % 3]
                        kb.dma(wst[wi % 2][:], winv(l)[:, :, c0:c0 + 256], f'awst{wi % 2}')
                        copy_('pool', W[:], wst[wi % 2][:])
                        wi += 1
                        for j in range(2):
                            for tt in range(4):
                                tsl = slice(tt * 512, (tt + 1) * 512)
                                p = P()
                                for kc in range(8):
                                    mm(p[:, :], W[:, kc, j * 128:(j + 1) * 128], hT[:, kc, tsl], kc == 0, kc == 7)
                                TA, TB = ta[tt % 2], tb[tt % 2]
                                tt_('dve', TA[:], p[:, :], cs[:, 0, tsl], ALU.mult)
                                for q4 in range(4):
                                    o0 = q4 * 32
                                    i0 = o0 + 32 if q4 % 2 == 0 else o0 - 32
                                    tt_('dve', TB[o0:o0 + 32, :], p[i0:i0 + 32, :], cs[o0:o0 + 32, 1, tsl], ALU.mult)
                                tt_('pool', qk[:, which, j, tsl], TA[:], TB[:], ALU.add)
                    c0 = OV + g * 512 + hh * 256
                    W = wq[wi % 3]
                    kb.dma(wst[wi % 2][:], winv(l)[:, :, c0:c0 + 256], f'awst{wi % 2}')
                    copy_('pool', W[:], wst[wi % 2][:])
                    wi += 1
                    for ti in range(16):
                        tok = attn_tok(g, ti)
                        p = P()
                        for kc in range(8):
                            mm(p[:, 0:256], hT[:, kc, tok], W[:, kc, :], kc == 0, kc == 7)
                        copy_('act', vaug[:, ti, :, 0:64], p[:, 0:256].rearrange("p (h d) -> p h d", h=4))
                    def stage_s(ti):
                        nonlocal pi
                        tq = attn_tok(g, ti)
                        pv = attn_prev(g, ti)
                        kts = ([(pv, 2)] if pv is not None else []) + [(ti, 1)]
                        pms = []
                        for kt, mk in kts:
                            tk = attn_tok(g, kt)
                            PT, PM = pt[pi % NPT], ptm[pi % NPT]
                            pi += 1
                            PT3 = PT[:, :].rearrange("p (h q) -> p h q", h=4)
                            for par in range(2):
                                p = P()
                                bp = par * 64
                                for j in range(2):
                                    mm(p[:, j * 128:(j + 1) * 128], qk[bp:bp + 64, 1, j, tk], qk[bp:bp + 64, 0, j, tq])
                                act_(PT3[:, par::2, :], p[:, 0:256].rearrange("p (j q) -> p j q", j=2), AF.Exp, scale=0.125)
                            tt_('dve', PM[:], PT3, cb[:, mk, :][:, None, :].broadcast_to([128, 4, 128]), ALU.mult)
                            pms.append((kt, PM))
                        return pms

                    def stage_pv(ti, pms):
                        tq = attn_tok(g, ti)
                        po = P()
                        for h in range(4):
                            for idx, (kt, PM) in enumerate(pms):
                                mm(po[:, h * 128:(h + 1) * 128], vaug[:, kt, h, :], PM[:, h, :], idx == 0, idx == len(pms) - 1)
                        pov = po[:, :].rearrange("p (h q) -> p h q", h=4)
                        if g == 0:
                            copy_('dve', acc[:, :, tq], pov)
                        else:
                            tt_('dve', acc[:, :, tq], pov, acc[:, :, tq], ALU.add)

                    prev = None
                    for ti in range(16):
                        cur = (ti, stage_s(ti))
                        if prev is not None:
                            stage_pv(*prev)
                        prev = cur
                    stage_pv(*prev)
                for tt in range(4):
                    tsl = slice(tt * 512, (tt + 1) * 512)
                    V('dve', lambda: nc.vector.reciprocal(out=rd[:], in_=acc[64:128, :, tsl]), [rd[:]], [acc[64:128, :, tsl]])
                    Y = yo[tt % 2]
                    tt_('dve', Y[:], acc[0:64, :, tsl], rd[:], ALU.mult)
                    kb.dma(ya_d[:, hh * 4:(hh + 1) * 4, tsl], Y[:], f'ayo{tt % 2}')
            kb.barrier()

    def lru_phase(l):
        with ExitStack() as es:
            A = lambda n, sh, dt: es.enter_context(nc.sbuf_tensor(f"{n}_{l}", sh, dt))
            wlx = A("lwlx", [128, 8, 1280], BF16)
            wlg = A("lwlg", [128, 8, 1280], BF16)
            wr = A("lwr", [80, 16, 80], BF16)
            wi_ = A("lwi", [80, 16, 80], BF16)
            cneg = A("lcneg", [80, 16], F32)
            c2 = A("lc2", [80, 16], F32)
            xpre = A("lxpre", [80, 3 + S], F32)
            xc = A("lxc", [80, S], F32)
            xcb = A("lxcb", [80, S], BF16)
            rg = A("lrg", [80, S], F32)
            ig = A("lig", [80, S], F32)
            Aa = A("laa", [80, S], F32)
            T1 = A("lt1", [80, S], F32)
            uu = A("luu", [80, S], F32)
            hs = A("lhs", [80, S], F32)
            gl = A("lgl", [80, S], F32)
            yo = [A(f"lyo{i}", [80, S], BF16) for i in range(2)]
            for (w, off, nm) in ((wlx, OLX, 'lwlx'), (wlg, OLG, 'lwlg')):
                for c0 in range(0, 1280, 320):
                    kb.dma(w[:, :, c0:c0 + 320], winv(l)[:, :, off + c0:off + c0 + 320], nm, q='pool')
            kb.dma(wr[:], lwr_d[l].rearrange("k i j -> i k j"), 'lwr', q='pool')
            kb.dma(wi_[:], lwi_d[l].rearrange("k i j -> i k j"), 'lwi', q='pool')
            lcw = PK('lcw', 80)
            lcb, lbr, lbi, llam = PK('lcb', 80), PK('lbr', 80), PK('lbi', 80), PK('llam', 80)
            act_(cneg[:], llam, AF.Exp, scale=-1.0)
            act_(cneg[:], cneg[:], AF.Ln, bias=EPSC[0:80, 1:2])
            ts_('dve', c2[:], cneg[:], -16.0, None, ALU.mult)
            ts_('dve', cneg[:], cneg[:], -8.0, None, ALU.mult)
            memset_('dve', xpre[:, 0:3], 0.0)
            for k in range(16):
                ksl = slice(k * 80, (k + 1) * 80)
                for tt in range(4):
                    tsl = slice(tt * 512, (tt + 1) * 512)
                    p = P()
                    for kc in range(8):
                        mm(p[0:80, :], wlx[:, kc, ksl], hT[:, kc, tsl], kc == 0, kc == 7)
                    copy_('act', xpre[:, 3 + tt * 512:3 + (tt + 1) * 512], p[0:80, :])
                ts_('dve', xc[:], xpre[:, 0:S], lcw[:, k * 4:k * 4 + 1], lcb[:, k:k + 1], ALU.mult, ALU.add)
                for j in range(1, 4):
                    stt_(xc[:], xpre[:, j:j + S], lcw[:, k * 4 + j:k * 4 + j + 1], xc[:], ALU.mult, ALU.add)
                copy_('pool', xcb[:], xc[:])
                for (wg, bb, dst) in ((wr, lbr, rg), (wi_, lbi, ig)):
                    for tt in range(4):
                        tsl = slice(tt * 512, (tt + 1) * 512)
                        p = P()
                        mm(p[0:80, :], wg[:, k, :], xcb[:, tsl])
                        act_(dst[:, tsl], p[0:80, :], AF.Sigmoid, bias=bb[:, k:k + 1])
                act_(Aa[:], rg[:], AF.Exp, scale=cneg[:, k:k + 1])
                act_(T1[:], rg[:], AF.Exp, scale=c2[:, k:k + 1])
                ts_('dve', T1[:], T1[:], -1.0, 1.0, ALU.mult, ALU.add)
                ts_('dve', T1[:], T1[:], 1e-12, None, ALU.max)
                act_(T1[:], T1[:], AF.Sqrt)
                tt_('pool', uu[:], ig[:], xc[:], ALU.mult)
                tt_('dve', uu[:], uu[:], T1[:], ALU.mult)
                V('dve', lambda: nc.vector.tensor_tensor_scan(out=hs[:], data0=Aa[:], data1=uu[:], initial=0.0,
                                                              op0=ALU.mult, op1=ALU.add), [hs[:]], [Aa[:], uu[:]])
                for tt in range(4):
                    tsl = slice(tt * 512, (tt + 1) * 512)
                    p = P()
                    for kc in range(8):
                        mm(p[0:80, :], wlg[:, kc, ksl], hT[:, kc, tsl], kc == 0, kc == 7)
                    act_(gl[:, tsl], p[0:80, :], AF.Gelu)
                Y = yo[k % 2]
                tt_('dve', Y[:], hs[:], gl[:], ALU.mult)
                kb.dma(yl_d[:, k, :], Y[:], f'lyo{k % 2}')
            kb.barrier()

    def ssd_proj_phase(l):
        with ExitStack() as es:
            A = lambda n, sh, dt: es.enter_context(nc.sbuf_tensor(f"{n}_{l}", sh, dt))
            W = [A(f"sw{i}", [128, 8, 512], BF16) for i in range(2)]
            wdt = A("swdt", [128, 8, 32], BF16)
            pre = [A(f"spre{i}", [128, 3 + S], F32) for i in range(2)]
            cv = [A(f"scv{i}", [128, S], F32) for i in range(2)]
            ob = [A(f"sob{i}", [128, S], BF16) for i in range(2)]
            scw, scb = PK('scw'), PK('scb')
            for i in range(2):
                memset_('dve', pre[i][:, 0:3], 0.0)
            n = 0
            for cg in range(8):
                Wt = W[cg % 2]
                kb.dma(Wt[:], winv(l)[:, :, OXBC + cg * 512:OXBC + (cg + 1) * 512], f'sw{cg % 2}', q='pool')
                for j in range(4):
                    fc = cg * 4 + j
                    PR, CV, OB = pre[n % 2], cv[n % 2], ob[n % 2]
                    n += 1
                    for tt in range(4):
                        p = P()
                        for kc in range(8):
                            mm(p[:, :], Wt[:, kc, j * 128:(j + 1) * 128], hT[:, kc, tt * 512:(tt + 1) * 512], kc == 0, kc == 7)
                        copy_('act', PR[:, 3 + tt * 512:3 + (tt + 1) * 512], p[:, :])
                    ts_('dve', CV[:], PR[:, 0:S], scw[:, fc * 4:fc * 4 + 1], None, ALU.mult)
                    for jj in range(1, 4):
                        stt_(CV[:], PR[:, jj:jj + S], scw[:, fc * 4 + jj:fc * 4 + jj + 1], CV[:], ALU.mult, ALU.add)
                    act_(OB[:], CV[:], AF.Silu, bias=scb[:, fc:fc + 1])
                    kb.dma(xbc_d[:, fc, :], OB[:], f'sob{(n - 1) % 2}')
            for cg in range(4):
                Wt = W[cg % 2]
                kb.dma(Wt[:], winv(l)[:, :, OZ + cg * 512:OZ + (cg + 1) * 512], f'sw{cg % 2}', q='pool')
                for j in range(4):
                    fc = cg * 4 + j
                    OB = ob[n % 2]
                    n += 1
                    for tt in range(4):
                        p = P()
                        for kc in range(8):
                            mm(p[:, :], Wt[:, kc, j * 128:(j + 1) * 128], hT[:, kc, tt * 512:(tt + 1) * 512], kc == 0, kc == 7)
                        act_(OB[:, tt * 512:(tt + 1) * 512], p[:, :], AF.Silu)
                    kb.dma(z_d[:, fc, :], OB[:], f'sob{(n - 1) % 2}')
            kb.dma(wdt[:], winv(l)[:, :, ODT:ODT + 32], 'swdt', q='pool')
            for c in range(16):
                p = P()
                for kc in range(8):
                    mm(p[:, 0:32], hT[:, kc, c * 128:(c + 1) * 128], wdt[:, kc, :], kc == 0, kc == 7)
                tt_('dve', dtraw[:, c, :], p[:, 0:32], PK('dtb'), ALU.add)
            kb.barrier()

    def ssd_scan_phase(l):
        with ExitStack() as es:
            A = lambda n, sh, dt: es.enter_context(nc.sbuf_tensor(f"{n}_{l}", sh, dt))
            H = A("cH", [128, 32, 64], F32)
            Hb = A("cHb", [128, 32, 64], BF16)
            expA = A("cexpA", [128, 32], F32)
            xsT = [A(f"cxs{i}", [128, 16, 128], BF16) for i in range(2)]
            BT = [A(f"cbt{i}", [128, 8, 128], BF16) for i in range(2)]
            CT = [A(f"cct{i}", [128, 8, 128], BF16) for i in range(2)]
            zT = [A(f"czt{i}", [128, 16, 128], BF16) for i in range(2)]
            dt_ = A("cdt", [128, 32], F32)
            dtw = A("cdtw", [128, 32], F32)
            a_ = A("ca", [128, 32], F32)
            ex = A("cex", [128, 3, 32], F32)
            abig = A("cabig", [128, 32, 64], F32)
            xdt = A("cxdt", [128, 32, 64], BF16)
            xdtw = A("cxdtw", [128, 32, 64], BF16)
            Btok = A("cbtok", [128, 8, 128], BF16)
            ab = [A(f"cab{i}", [128, 4, 128], F32) for i in range(2)]
            CBm = [A(f"ccbm{i}", [128, 128], F32) for i in range(2)]
            Lm = [A(f"clm{i}", [128, 512], F32) for i in range(2)]
            Gm = [A(f"cgm{i}", [128, 4, 128], BF16) for i in range(2)]
            ds = [A(f"cds{i}", [128, 256], F32) for i in range(2)]
            t1 = [A(f"ct1{i}", [128, 256], F32) for i in range(2)]
            yT = A("cyT", [128, 16, 128], F32)
            yg = A("cyg", [128, 16, 128], F32)
            sq = A("csq", [128, 16, 128], F32)
            rs = A("crs", [128, 128], F32)
            yo = [A(f"cyo{i}", [128, 16, 128], BF16) for i in range(2)]
            sdd, sng = PK('sdd'), PK('sng')
            memset_('dve', H[:], 0.0)
            memset_('pool', Hb[:], 0.0)
            act_(expA[:], PK('alog'), AF.Exp)
            for c in range(16):
                csl = slice(c * 128, (c + 1) * 128)
                b = c % 2
                kb.dma(xsT[b][:], xbc_d[:, 0:16, csl], f'cxs{b}')
                kb.dma(BT[b][:], xbc_d[:, 16:24, csl], f'cbt{b}')
                kb.dma(CT[b][:], xbc_d[:, 24:32, csl], f'cct{b}')
                kb.dma(zT[b][:], z_d[:, :, csl], f'czt{b}')
                act_(dt_[:], dtraw[:, c, :], AF.Exp)
                act_(dt_[:], dt_[:], AF.Ln, bias=EPSC[:, 1:2])
                stt_(a_[:], dt_[:], -1.0, expA[:], ALU.mult, ALU.mult)
                p3 = P()
                mm(p3[:, 0:32], mle, a_[:])
                mm(p3[:, 32:64], mgt, a_[:])
                mm(p3[:, 64:96], onesf, a_[:])
                act_(ex[:], p3[:, 0:96].rearrange("p (k h) -> p k h", k=3), AF.Exp)
                tt_('dve', dtw[:], dt_[:], ex[:, 1, :], ALU.mult)
                copy_('pool', abig[:], a_[:, :][:, :, None].broadcast_to([128, 32, 64]))
                for bb in range(2):
                    pT = P()[:, :].bitcast(BF16)
                    for j in range(8):
                        tr(pT[:, j * 128:(j + 1) * 128], xsT[b][:, bb * 8 + j, :], identb)
                    pv = pT.rearrange("p (h d) -> p h d", h=16)
                    hsl = slice(bb * 16, (bb + 1) * 16)
                    tt_('dve', xdt[:, hsl, :], pv, dt_[:, hsl][:, :, None].broadcast_to([128, 16, 64]), ALU.mult)
                    tt_('dve', xdtw[:, hsl, :], pv, dtw[:, hsl][:, :, None].broadcast_to([128, 16, 64]), ALU.mult)
                pB = P()[:, :].bitcast(BF16)
                for g in range(8):
                    tr(pB[:, g * 128:(g + 1) * 128], BT[b][:, g, :], identb)
                copy_('act', Btok[:], pB.rearrange("p (g n) -> p g n", g=8))
                def stage_a(g):
                    i2 = g % 2
                    pcb = P()
                    mm(pcb[:, 0:128], BT[b][:, g, :], CT[b][:, g, :])
                    tt_('dve', CBm[i2][:], pcb[:, 0:128], mle, ALU.mult)
                    tt_('pool', ab[i2][:], mgt[:, None, :].broadcast_to([128, 4, 128]),
                        a_[:, 4 * g:4 * g + 4][:, :, None].broadcast_to([128, 4, 128]), ALU.mult)
                    pD = P()
                    for h in range(4):
                        mm(pD[:, h * 128:(h + 1) * 128], ab[i2][:, h, :], mle)
                    act_(Lm[i2][:], pD[:, :], AF.Exp)
                    tt_('dve', Gm[i2][:], Lm[i2][:, :].rearrange("p (h l) -> p h l", h=4),
                        CBm[i2][:, None, :].broadcast_to([128, 4, 128]), ALU.mult)
                    pS = P()
                    for hp in range(2):
                        h0 = 4 * g + 2 * hp
                        mm(pS[:, hp * 128:(hp + 1) * 128], abig[:, h0:h0 + 2, :].rearrange("p h d -> p (h d)"), mle)
                    act_(ds[i2][:], pS[:, 0:256], AF.Exp)

                def stage_b(g):
                    i2 = g % 2
                    pY = P()
                    for hp in range(2):
                        for hh in range(2):
                            h = 2 * hp + hh
                            mm(pY[hh * 64:(hh + 1) * 64, hp * 128:(hp + 1) * 128], xdt[:, 4 * g + h, :], Gm[i2][:, h, :])
                    pZ = P()
                    for hp in range(2):
                        h0 = 4 * g + 2 * hp
                        mm(pZ[:, hp * 128:(hp + 1) * 128], Hb[:, h0:h0 + 2, :].rearrange("p h d -> p (h d)"), CT[b][:, g, :])
                    tt_('dve', t1[i2][:], pZ[:, 0:256], ds[i2][:], ALU.mult)
                    tt_('dve', yT[:, 2 * g:2 * g + 2, :], pY[:, 0:256].rearrange("p (f l) -> p f l", f=2),
                        t1[i2][:, :].rearrange("p (f l) -> p f l", f=2), ALU.add)

                stage_a(0)
                for g in range(8):
                    if g + 1 < 8:
                        stage_a(g + 1)
                    stage_b(g)
                for fc in range(16):
                    stt_(yT[:, fc, :], xsT[b][:, fc, :], sdd[:, fc:fc + 1], yT[:, fc, :], ALU.mult, ALU.add)
                tt_('dve', H[:], H[:], ex[:, 2, :][:, :, None].broadcast_to([128, 32, 64]), ALU.mult)
                for gp in range(4):
                    pst = P()
                    for gg in range(2):
                        g = 2 * gp + gg
                        mm(pst[:, gg * 256:(gg + 1) * 256], Btok[:, g, :],
                           xdtw[:, 4 * g:4 * g + 4, :].rearrange("p h d -> p (h d)"))
                    tt_('dve', H[:, 8 * gp:8 * gp + 8, :], pst[:, :].rearrange("p (h d) -> p h d", h=8),
                        H[:, 8 * gp:8 * gp + 8, :], ALU.add)
                copy_('pool', Hb[:], H[:])
                tt_('dve', yg[:], yT[:], zT[b][:], ALU.mult)
                act_(sq[:], yg[:], AF.Square)
                pss = P()
                for fc in range(16):
                    mm(pss[:, 0:128], onesf, sq[:, fc, :], fc == 0, fc == 15)
                act_(rs[:], pss[:, 0:128], AF.Sqrt, bias=EPSC[:, 0:1], scale=1.0 / 2048)
                V('dve', lambda: nc.vector.reciprocal(out=rs[:], in_=rs[:]), [rs[:]], [rs[:]])
                tt_('dve', yg[:], yg[:], rs[:, None, :].broadcast_to([128, 16, 128]), ALU.mult)
                tt_('pool', yo[b][:], yg[:], sng[:, :, None].broadcast_to([128, 16, 128]), ALU.mult)
                kb.dma(ys_d[:, :, csl], yo[b][:], f'cyo{b}')
            kb.barrier()

    def merge_phase(l):
        with ExitStack() as es0:
            mT = es0.enter_context(nc.sbuf_tensor(f"gmT_{l}", [128, 8, S], BF16))
            with ExitStack() as es:
                A = lambda n, sh, dt: es.enter_context(nc.sbuf_tensor(f"{n}_{l}", sh, dt))
                wa = A("gwa", [64, 8, 512], BF16)
                ws = A("gws", [128, 16, 512], BF16)
                wl = A("gwl", [80, 16, 512], BF16)
                wm = A("gwm", [128, 3, 8, 512], BF16)
                ya = A("gya", [64, 8, 512], BF16)
                ys = A("gys", [128, 16, 512], BF16)
                yl = A("gyl", [80, 16, 512], BF16)
                sgt = [A(f"gsg{i}", [128, 512], F32) for i in range(2)]
                mac = [A(f"gmac{i}", [128, 512], F32) for i in range(2)]
                tmp = [A(f"gtmp{i}", [128, 512], F32) for i in range(2)]
                n = 0
                for half in range(2):
                    hsl = slice(half * 512, (half + 1) * 512)
                    kb.dma(wa[:], wba_d[l].rearrange("(h d) c -> d h c", d=64)[:, :, hsl], 'gwa', q='pool')
                    for k0 in range(0, 16, 8):
                        kb.dma(ws[:, k0:k0 + 8, :], wbs_d[l].rearrange("(k p) c -> p k c", p=128)[:, k0:k0 + 8, hsl], 'gws', q='pool')
                        kb.dma(wl[:, k0:k0 + 8, :], wbl_d[l].rearrange("(k p) c -> p k c", p=80)[:, k0:k0 + 8, hsl], 'gwl', q='pool')
                    for b in range(3):
                        c0 = OMG + b * 1024 + half * 512
                        kb.dma(wm[:, b, :, :], winv(l)[:, :, c0:c0 + 512], 'gwm', q='pool')
                    for tt in range(4):
                        tsl = slice(tt * 512, (tt + 1) * 512)
                        kb.dma(ya[:], ya_d[:, :, tsl], 'gya')
                        kb.dma(ys[:], ys_d[:, :, tsl], 'gys')
                        kb.dma(yl[:], yl_d[:, :, tsl], 'gyl')
                        for oc in range(4):
                            osl = slice(oc * 128, (oc + 1) * 128)
                            M = mac[n % 2]
                            n += 1
                            for b, (y, w, nk) in enumerate(((ya, wa, 8), (ys, ws, 16), (yl, wl, 16))):
                                pP = P()
                                for k in range(nk):
                                    mm(pP[:, :], w[:, k, osl], y[:, k, :], k == 0, k == nk - 1)
                                pG = P()
                                for kc in range(8):
                                    mm(pG[:, :], wm[:, b, kc, osl], hT[:, kc, tsl], kc == 0, kc == 7)
                                SG = sgt[b % 2]
                                act_(SG[:], pG[:, :], AF.Sigmoid)
                                if b == 0:
                                    tt_('dve', M[:], SG[:], pP[:, :], ALU.mult)
                                else:
                                    T = tmp[b % 2]
                                    tt_('dve', T[:], SG[:], pP[:, :], ALU.mult)
                                    tt_('pool', M[:], M[:], T[:], ALU.add)
                            copy_('pool', mT[:, half * 4 + oc, tsl], M[:])
                kb.barrier()
            with ExitStack() as es:
                A = lambda n, sh, dt: es.enter_context(nc.sbuf_tensor(f"{n}_{l}", sh, dt))
                wo = A("gwo", [128, 8, 1024], BF16)
                X = [A(f"gX{i}", [128, 8, 512], F32) for i in range(2)]
                for c0 in range(0, 1024, 512):
                    kb.dma(wo[:, :, c0:c0 + 512], wo_d[l].rearrange("(kc p) c -> p kc c", p=128)[:, :, c0:c0 + 512], 'gwo', q='pool')
                for tt in range(4):
                    tsl = slice(tt * 512, (tt + 1) * 512)
                    Xt = X[tt % 2]
                    kb.dma(Xt[:], xs_d[:, :, tsl], f'gX{tt % 2}')
                    for oc in range(8):
                        p = P()
                        for kc in range(8):
                            mm(p[:, :], wo[:, kc, oc * 128:(oc + 1) * 128], mT[:, kc, tsl], kc == 0, kc == 7)
                        stt_(Xt[:, oc, :], p[:, :], cond[:, 16 + oc:17 + oc], Xt[:, oc, :], ALU.mult, ALU.add)
                    kb.dma(xs_d[:, :, tsl], Xt[:], f'gX{tt % 2}')
                kb.barrier()

    def moe_phase(l):
        with ExitStack() as es:
            A = lambda n, sh, dt: es.enter_context(nc.sbuf_tensor(f"{n}_{l}", sh, dt))
            yacc = A("myacc", [128, 16, 1024], F32)
            wt = A("mwt", [128, 16, 32], F32)
            wrt = A("mwrt", [128, 8, 36], BF16)
            lg = A("mlg", [128, 36], F32)
            sm = A("msm", [128, 16], F32)
            oh = A("moh", [128, 4], F32)
            pen = A("mpen", [128, 4], F32)
            eg4 = A("meg4", [128, 4], F32)
            lem = A("mlem", [128, 4, 8], F32)
            m8 = A("mm8", [128, 8], F32)
            eq1 = A("meq1", [128, 32], F32)
            eq2 = A("meq2", [128, 32], F32)
            actb = [A(f"mact{i}", [128, 4, S], BF16) for i in range(2)]
            wg = [A(f"mwg{i}", [128, 8, 512], BF16) for i in range(2)]
            wu = [A(f"mwu{i}", [128, 8, 512], BF16) for i in range(2)]
            wd = [A(f"mwd{i}", [128, 4, 1024], BF16) for i in range(2)]
            sgt = [A(f"msg{i}", [128, 512], F32) for i in range(2)]
            X = [A(f"mX{i}", [128, 8, 128], F32) for i in range(2)]
            kb.dma(wrt[:, :, 0:4], rwg_d[l].rearrange("(kc p) g -> p kc g", p=128), 'mwrt', q='pool')
            kb.dma(wrt[:, :, 4:36], rwe_d[l].rearrange("(kc p) g -> p kc g", p=128), 'mwrt', q='pool')
            rb = PK('rb')
            for t in range(16):
                tk = slice(t * 128, (t + 1) * 128)
                p = P()
                for kc in range(8):
                    mm(p[:, 0:36], hT[:, kc, tk], wrt[:, kc, :], kc == 0, kc == 7)
                tt_('dve', lg[:], p[:, 0:36], rb, ALU.add)
                c = lambda i: sm[:, i:i + 1]
                V('dve', lambda: nc.vector.reduce_max(out=c(0), in_=lg[:, 0:4], axis=AX.X), [c(0)], [lg[:, 0:4]])
                ts_('dve', oh[:], lg[:, 0:4], c(0), None, ALU.is_ge)
                ts_('dve', c(1), c(0), -1.0, None, ALU.mult)
                act_(eg4[:], lg[:, 0:4], AF.Exp, bias=c(1))
                V('dve', lambda: nc.vector.reduce_sum(out=c(2), in_=eg4[:], axis=AX.X), [c(2)], [eg4[:]])
                V('dve', lambda: nc.vector.reciprocal(out=c(3), in_=c(2)), [c(3)], [c(2)])
                ts_('dve', pen[:], oh[:], -1.0, 30000.0, ALU.add, ALU.mult)
                tt_('dve', lem[:], lg[:, 4:36].rearrange("p (g e) -> p g e", g=4),
                    oh[:, :][:, :, None].broadcast_to([128, 4, 8]), ALU.mult)
                tt_('dve', lem[:], lem[:], pen[:, :][:, :, None].broadcast_to([128, 4, 8]), ALU.add)
                lemf = lem[:, :, :].rearrange("p g e -> p (g e)")
                V('dve', lambda: nc.vector.max(out=m8[:], in_=lemf), [m8[:]], [lemf])
                ts_('dve', eq1[:], lemf, m8[:, 0:1], None, ALU.is_ge)
                ts_('dve', eq2[:], lemf, m8[:, 1:2], None, ALU.is_ge)
                tt_('dve', c(4), m8[:, 1:2], m8[:, 0:1], ALU.subtract)
                act_(c(5), c(4), AF.Exp)
                ts_('dve', c(6), c(5), 1.0, None, ALU.add)
                V('dve', lambda: nc.vector.reciprocal(out=c(7), in_=c(6)), [c(7)], [c(6)])
                tt_('dve', c(8), c(3), c(7), ALU.mult)
                tt_('dve', c(9), c(8), c(5), ALU.mult)
                tt_('dve', c(10), c(8), c(9), ALU.subtract)
                ts_('dve', wt[:, t, :], eq2[:], c(9), None, ALU.mult)
                stt_(wt[:, t, :], eq1[:], c(10), wt[:, t, :], ALU.mult, ALU.add)
            for e in range(NEXP):
                b = e % 2
                kb.dma(wg[b][:], eg_d[l, e].rearrange("(kc p) f -> p kc f", p=128), f'mwg{b}', q='pool')
                kb.dma(wu[b][:], eu_d[l, e].rearrange("(kc p) f -> p kc f", p=128), f'mwu{b}', q='pool')
                for c0 in range(0, 1024, 512):
                    kb.dma(wd[b][:, :, c0:c0 + 512], ed_d[l, e].rearrange("(k p) c -> p k c", p=128)[:, :, c0:c0 + 512], f'mwd{b}', q='pool')
                n = 0
                for fcn in range(4):
                    fsl = slice(fcn * 128, (fcn + 1) * 128)
                    for tt in range(4):
                        tsl = slice(tt * 512, (tt + 1) * 512)
                        pg = P()
                        for kc in range(8):
                            mm(pg[:, :], wg[b][:, kc, fsl], hT[:, kc, tsl], kc == 0, kc == 7)
                        pu = P()
                        for kc in range(8):
                            mm(pu[:, :], wu[b][:, kc, fsl], hT[:, kc, tsl], kc == 0, kc == 7)
                        SG = sgt[n % 2]
                        n += 1
                        act_(SG[:], pg[:, :], AF.Silu)
                        tt_('dve', actb[b][:, fcn, tsl], SG[:], pu[:, :], ALU.mult)
                for t in range(16):
                    tk = slice(t * 128, (t + 1) * 128)
                    for half in range(2):
                        hsl = slice(half * 512, (half + 1) * 512)
                        pd = P()
                        for k in range(4):
                            mm(pd[:, :], actb[b][:, k, tk], wd[b][:, k, hsl], k == 0, k == 3)
                        if e == 0:
                            ts_('dve', yacc[:, t, hsl], pd[:, :], wt[:, t, e:e + 1], None, ALU.mult)
                        else:
                            stt_(yacc[:, t, hsl], pd[:, :], wt[:, t, e:e + 1], yacc[:, t, hsl], ALU.mult, ALU.add)
            for t in range(16):
                tk = slice(t * 128, (t + 1) * 128)
                Xt = X[t % 2]
                kb.dma(Xt[:], xs_d[:, :, tk], f'mX{t % 2}')
                for half in range(2):
                    p = P()
                    for j in range(4):
                        kc = half * 4 + j
                        tr(p[:, j * 128:(j + 1) * 128], yacc[:, t, kc * 128:(kc + 1) * 128], identf)
                    for j in range(4):
                        kc = half * 4 + j
                        stt_(Xt[:, kc, :], p[:, j * 128:(j + 1) * 128], cond[:, 40 + kc:41 + kc], Xt[:, kc, :], ALU.mult, ALU.add)
                kb.dma(xs_d[:, :, tk], Xt[:], f'mX{t % 2}')
            kb.barrier()

    def final_phase():
        with ExitStack() as es:
            A = lambda n, sh, dt: es.enter_context(nc.sbuf_tensor(n, sh, dt))
            xt = [A(f"fx{i}", [128, 8, 512], F32) for i in range(2)]
            sq = [A(f"fsq{i}", [128, 512], F32) for i in range(2)]
            rs = A("frs", [128, 512], F32)
            yf = A("fyf", [128, 8, 512], F32)
            ot = [A(f"fot{i}", [128, 1024], F32) for i in range(2)]
            fg = G('fg')
            n = 0
            for tt in range(4):
                Xt = xt[tt % 2]
                kb.dma(Xt[:], xs_d[:, :, tt * 512:(tt + 1) * 512], f'fx{tt % 2}')
                pss = P()
                for kc in range(8):
                    act_(sq[kc % 2][:], Xt[:, kc, :], AF.Square)
                    mm(pss[:, :], onesf, sq[kc % 2][:], kc == 0, kc == 7)
                act_(rs[:], pss[:, :], AF.Sqrt, bias=EPSC[:, 0:1], scale=1.0 / D)
                V('dve', lambda: nc.vector.reciprocal(out=rs[:], in_=rs[:]), [rs[:]], [rs[:]])
                for kc in range(8):
                    stt_(yf[:, kc, :], Xt[:, kc, :], fg[:, kc:kc + 1], rs[:], ALU.mult, ALU.mult)
                for t4 in range(4):
                    O = ot[n % 2]
                    n += 1
                    for half in range(2):
                        p = P()
                        for j in range(4):
                            kc = half * 4 + j
                            tr(p[:, j * 128:(j + 1) * 128], yf[:, kc, t4 * 128:(t4 + 1) * 128], identf)
                        copy_('act' if half else 'dve', O[:, half * 512:(half + 1) * 512], p[:, :])
                    r0 = tt * 512 + t4 * 128
                    kb.dma(out_d[r0:r0 + 128, :], O[:], f'fot{(n - 1) % 2}')
            kb.barrier()

    ph = phases
    load_x_phase()
    if ph is None or 'rope' in ph:
        rope_phase()
    for l in range(L):
        if ph is None or 'cond' in ph:
            cond_phase(l)
        if ph is None or 'norm1' in ph:
            norm_phase(l, 0, f"a{l}")
        if ph is None or 'attn' in ph:
            attn_phase(l)
        if ph is None or 'lru' in ph:
            lru_phase(l)
        if ph is None or 'ssd' in ph:
            ssd_proj_phase(l)
            ssd_scan_phase(l)
        if ph is None or 'merge' in ph:
            merge_phase(l)
        if ph is None or 'moe' in ph:
            norm_phase(l, 1, f"b{l}")
            moe_phase(l)
    if ph is None or 'final' in ph:
        final_phase()
    kb.finish()
    return nc, kb


_CACHE = {}


def _in_maps(inp, L, cores):
    pk = np.stack([_pack_layer(inp, l) for l in range(L)])
    maps = []
    shared = {
        "pk": pk,
        "ada_w": inp['ada_w'][:L], "w_in": inp['w_in'][:L],
        "lru_w_r": inp['lru_w_r'][:L], "lru_w_i": inp['lru_w_i'][:L],
        "w_br_attn": inp['w_br_attn'][:L], "w_br_ssd": inp['w_br_ssd'][:L], "w_br_lru": inp['w_br_lru'][:L],
        "w_out": inp['w_out'][:L], "router_wg": inp['router_wg'][:L],
        "router_we": inp['router_we'][:L].reshape(L, D, 32),
        "exp_w_gate": inp['exp_w_gate'][:L], "exp_w_up": inp['exp_w_up'][:L], "exp_w_down": inp['exp_w_down'][:L],
    }
    for b in cores:
        m = dict(shared)
        m["x"] = np.ascontiguousarray(inp['x'][b])
        m["pos"] = np.ascontiguousarray(np.broadcast_to(inp['positions'][b][None, :], (128, S))).astype(np.int32)
        m["gk"] = _pack_global(inp['c'][b], inp['final_g'])
        maps.append(m)
    return maps


def kernel(**inputs):
    inp = {k: np.asarray(v) for k, v in inputs.items()}
    if 'nc' not in _CACHE:
        _CACHE['nc'] = build(DEPTH)[0]
    nc = _CACHE['nc']
    maps = _in_maps(inp, DEPTH, list(range(8)))
    res = run_bass_kernel_spmd(nc, maps, core_ids=list(range(8)))
    out = np.stack([np.asarray(res.results[c]["out"]) for c in range(8)], axis=0)
    return out.astype(np.float32)
```

```python
import numpy as np
import concourse.bass as bass
import concourse.mybir as mybir
from concourse.bass_utils import run_bass_kernel_spmd

F32 = mybir.dt.float32
BF16 = mybir.dt.bfloat16
I32 = mybir.dt.int32
ALU = mybir.AluOpType
AF = mybir.ActivationFunctionType
AX = mybir.AxisListType

_DTSIZE = {F32: 4, BF16: 2, I32: 4}
SEM_ROT = 30000

D = 1024
S = 2048
DEPTH = 4
NEXP = 32
FF = 512
INC = 16416
EPS = 1e-6
OQ, OK_, OV, OZ, OXBC, ODT, OLG, OLX, OMG = 0, 1536, 3072, 4608, 6656, 10752, 10784, 12064, 13344


def _box(ap):
    t = ap.tensor
    es = _DTSIZE[ap.dtype]
    pat = ap.ap
    off = int(ap.offset)
    if type(t).__name__.startswith('DRam'):
        ext = 1
        for st, cnt in pat:
            ext += (cnt - 1) * abs(st)
        return (t.name, 0, 1, off * es, (off + ext) * es)
    if type(t).__name__.startswith('PSum'):
        return (t.name, 0, 128, 0, 2048)
    free = 1
    for s in list(t.shape)[1:]:
        free *= s
    p0 = off // free
    f0 = off % free
    np_ = pat[0][1] if pat[0][0] != 0 else 1
    ext = 1
    for st, cnt in pat[1:]:
        ext += (cnt - 1) * abs(st)
    return (t.name, p0, p0 + np_, f0 * es, (f0 + ext) * es)


class Chan:
    def __init__(self, kb, name):
        self.kb = kb
        self.name = name
        self.sem = kb.nc.alloc_semaphore(name)
        kb.chan_by_sem[self.sem.num] = self
        self.cnt = 0
        self.gen = 0

    def next_event(self):
        if self.cnt + 16 > SEM_ROT:
            self.gen += 1
            self.sem = self.kb.nc.alloc_semaphore(f"{self.name}_g{self.gen}")
            self.kb.chan_by_sem[self.sem.num] = self
            self.cnt = 0
        self.cnt += 16
        return (self.sem, self.cnt)


class KB:
    def __init__(self, nc):
        self.nc = nc
        self.engs = {'pe': nc.tensor, 'act': nc.scalar, 'dve': nc.vector, 'pool': nc.gpsimd, 'sp': nc.sync}
        self.sem, self.cnt, self.gen = {}, {}, {}
        for e in ('pe', 'act', 'dve', 'pool'):
            self.sem[e] = nc.alloc_semaphore(f"s_{e}")
            self.cnt[e] = 0
            self.gen[e] = 0
        self.oldsems = []
        self.seen = {e: {} for e in self.engs}
        self.recs = {}
        self.pending = {e: False for e in self.engs}
        self.ninst = {e: 0 for e in self.engs}
        self.chans = {}
        self.chan_by_sem = {}

    def chan(self, name):
        if name not in self.chans:
            self.chans[name] = Chan(self, name)
        return self.chans[name]

    def _deps(self, ins, outs):
        deps = []
        for ap in ins:
            b = _box(ap)
            for r in self.recs.get(b[0], ()):
                if r[5] and r[1] < b[2] and b[1] < r[2] and r[3] < b[4] and b[3] < r[4]:
                    deps.append(r[6])
        for ap in outs:
            b = _box(ap)
            for r in self.recs.get(b[0], ()):
                if r[1] < b[2] and b[1] < r[2] and r[3] < b[4] and b[3] < r[4]:
                    deps.append(r[6])
        return deps

    def _record(self, ins, outs, ev, tag):
        for ap in outs:
            b = _box(ap)
            lst = self.recs.setdefault(b[0], [])
            lst[:] = [r for r in lst if not (b[1] <= r[1] and r[2] <= b[2] and b[3] <= r[3] and r[4] <= b[4])]
            lst.append((b[0], b[1], b[2], b[3], b[4], True, ev, tag))
        for ap in ins:
            b = _box(ap)
            lst = self.recs.setdefault(b[0], [])
            lst[:] = [r for r in lst if not ((not r[5]) and r[7] == tag and r[1:5] == b[1:5])]
            lst.append((b[0], b[1], b[2], b[3], b[4], False, ev, tag))

    def _wait(self, e, deps, skip_self=False):
        eng = self.engs[e]
        seen = self.seen[e]
        best = {}
        for sem, val in deps:
            if skip_self and e in self.sem and sem is self.sem[e]:
                continue
            k = sem.num
            ch = self.chan_by_sem.get(k)
            if ch is not None and ch.sem.num == k:
                val = max(val, ch.cnt)
            if seen.get(k, 0) >= val:
                continue
            if k not in best or best[k][1] < val:
                best[k] = (sem, val)
        for k, (sem, val) in best.items():
            eng.wait_ge(sem, val)
            seen[k] = val

    def op(self, e, fn, outs, ins, signal=True):
        deps = self._deps(ins, outs)
        self._wait(e, deps, skip_self=(e == 'pe'))
        inst = fn()
        self.ninst[e] += 1
        if signal and self.cnt[e] + 1 > SEM_ROT:
            self.oldsems.append((self.sem[e], self.cnt[e]))
            self.gen[e] += 1
            self.sem[e] = self.nc.alloc_semaphore(f"s_{e}_g{self.gen[e]}")
            self.cnt[e] = 0
        if signal:
            self.cnt[e] += 1
            inst.then_inc(self.sem[e], 1)
            ev = (self.sem[e], self.cnt[e])
            self.pending[e] = False
        else:
            assert self.cnt[e] + 1 <= SEM_ROT
            ev = (self.sem[e], self.cnt[e] + 1)
            self.pending[e] = True
        self._record(ins, outs, ev, e)
        return inst

    def dma(self, out, in_, chan, q='sp', **kw):
        ch = self.chan(chan)
        deps = self._deps([in_], [out])
        self._wait(q, deps)
        ev = ch.next_event()
        inst = self.engs[q].dma_start(out=out, in_=in_, **kw)
        inst.then_inc(ev[0], 16)
        self.ninst[q] += 1
        self._record([in_], [out], ev, 'dma_' + ch.name)
        return inst

    def all_events(self):
        evs = [(self.sem[e], self.cnt[e]) for e in ('pe', 'act', 'dve', 'pool') if self.cnt[e] > 0]
        evs += [(ch.sem, ch.cnt) for ch in self.chans.values() if ch.cnt > 0]
        return evs

    def barrier(self, drop=()):
        for e in ('pe', 'act', 'dve', 'pool'):
            assert not self.pending[e], e
        evs = self.all_events()
        for e in self.engs:
            self._wait(e, evs)
        self.recs.clear()

    def finish(self):
        for e in ('pe', 'act', 'dve', 'pool'):
            assert not self.pending[e], e
        self._wait('sp', self.all_events())


PK_COLS = {}
_o = 0
for _n, _w in (('n1g', 8), ('n2g', 8), ('adab', 48), ('scw', 128), ('scb', 32), ('dtb', 32), ('alog', 32),
               ('sdd', 16), ('sng', 16), ('rb', 36), ('lcw', 64), ('lcb', 16), ('lbr', 16), ('lbi', 16), ('llam', 16)):
    PK_COLS[_n] = (_o, _w)
    _o += _w
NPK = _o
GK_COLS = {}
_o = 0
for _n, _w in (('c', 8), ('fg', 8), ('freq', 1), ('ident', 128), ('mle', 128), ('mgt', 128), ('mge', 128), ('ones', 128),
               ('phase', 1)):
    GK_COLS[_n] = (_o, _w)
    _o += _w
NGK = _o


def _fm(v, p=128):
    return np.ascontiguousarray(v.reshape(-1, p).T)


def _pack_layer(inp, l):
    pk = np.zeros((128, NPK), np.float32)

    def put(name, arr):
        o, w = PK_COLS[name]
        assert arr.shape[1] == w, (name, arr.shape)
        pk[:arr.shape[0], o:o + w] = arr
    put('n1g', _fm(inp['norm1_g'][l]))
    put('n2g', _fm(inp['norm2_g'][l]))
    put('adab', _fm(inp['ada_b'][l]))
    scw = inp['ssd_conv_w'][l]
    put('scw', np.ascontiguousarray(scw.T.reshape(32, 128, 4).transpose(1, 0, 2)).reshape(128, 128))
    put('scb', _fm(inp['ssd_conv_b'][l]))
    put('dtb', np.broadcast_to(inp['ssd_dt_bias'][l][None, :], (128, 32)))
    put('alog', np.broadcast_to(inp['ssd_a_log'][l][None, :], (128, 32)))
    put('sdd', _fm(np.repeat(inp['ssd_d'][l], 64)))
    put('sng', _fm(inp['ssd_norm_g'][l]))
    rb = np.concatenate([inp['router_bg'][l].reshape(-1), inp['router_be'][l].reshape(-1)])
    put('rb', np.broadcast_to(rb[None, :], (128, 36)))
    lcw = inp['lru_conv_w'][l]
    put('lcw', np.ascontiguousarray(lcw.T.reshape(16, 80, 4).transpose(1, 0, 2)).reshape(80, 64))
    put('lcb', _fm(inp['lru_conv_b'][l], 80))
    put('lbr', _fm(inp['lru_b_r'][l], 80))
    put('lbi', _fm(inp['lru_b_i'][l], 80))
    put('llam', _fm(inp['lru_lambda'][l], 80))
    return pk


def _pack_global(c_b, final_g):
    gk = np.zeros((128, NGK), np.float32)

    def put(name, arr):
        o, w = GK_COLS[name]
        gk[:, o:o + w] = arr
    put('c', _fm(c_b))
    put('fg', _fm(final_g))
    half = 32
    freqs = (10000.0 ** (-np.arange(half, dtype=np.float32) / half)).astype(np.float32)
    put('freq', np.tile(freqs, 4)[:, None])
    i = np.arange(128)
    put('ident', (i[:, None] == i[None, :]).astype(np.float32))
    put('mle', (i[:, None] <= i[None, :]).astype(np.float32))
    put('mgt', (i[:, None] > i[None, :]).astype(np.float32))
    put('mge', (i[:, None] >= i[None, :]).astype(np.float32))
    put('ones', np.ones((128, 128), np.float32))
    put('phase', np.where((i % 64) < 32, -1.0, 1.0).astype(np.float32)[:, None])
    return gk


from contextlib import ExitStack
import math


def build(L=DEPTH, dbg=False, phases=None):
    nc = bass.Bass("TRN2", target_bir_lowering=False)
    kb = KB(nc)

    def din(name, shape, dt=F32):
        return nc.dram_tensor(name, list(shape), dt, kind="ExternalInput").ap()

    def dscr(name, shape, dt):
        return nc.dram_tensor(name, list(shape), dt, kind=("ExternalOutput" if dbg else "Internal")).ap()

    x_d = din("x", [S, D])
    pos_d = din("pos", [128, S], I32)
    gk_d = din("gk", [128, NGK])
    pk_d = din("pk", [L, 128, NPK])
    adaw_d = din("ada_w", [L, D, 6 * D])
    win_d = din("w_in", [L, D, INC])
    lwr_d = din("lru_w_r", [L, 16, 80, 80])
    lwi_d = din("lru_w_i", [L, 16, 80, 80])
    wba_d = din("w_br_attn", [L, 512, D])
    wbs_d = din("w_br_ssd", [L, 2048, D])
    wbl_d = din("w_br_lru", [L, 1280, D])
    wo_d = din("w_out", [L, D, D])
    rwg_d = din("router_wg", [L, D, 4])
    rwe_d = din("router_we", [L, D, 32])
    eg_d = din("exp_w_gate", [L, NEXP, D, FF])
    eu_d = din("exp_w_up", [L, NEXP, D, FF])
    ed_d = din("exp_w_down", [L, NEXP, FF, D])
    out_d = nc.dram_tensor("out", [S, D], F32, kind="ExternalOutput").ap()

    xs_d = dscr("xs_scr", [128, 8, S], F32)
    ya_d = dscr("ya_scr", [64, 8, S], BF16)
    ys_d = dscr("ys_scr", [128, 16, S], BF16)
    yl_d = dscr("yl_scr", [80, 16, S], BF16)
    xbc_d = nc.dram_tensor("xbc_scr", [128, 32, S], BF16).ap()
    z_d = nc.dram_tensor("z_scr", [128, 16, S], BF16).ap()
    cs_d = nc.dram_tensor("cs_scr", [128, 2, S], F32).ap()

    ps = [nc.alloc_psum_tensor(f"ps{i}", [128, 512], F32) for i in range(8)]
    pctr = [0]

    def P():
        t = ps[pctr[0] % 8]
        pctr[0] += 1
        return t

    def mm(out, lhsT, rhs, start=True, stop=True):
        return kb.op('pe', lambda: nc.tensor.matmul(out, lhsT=lhsT, rhs=rhs, start=start, stop=stop),
                     [out], [lhsT, rhs], signal=True)

    def tr(out, in_, ident):
        return kb.op('pe', lambda: nc.tensor.transpose(out=out, in_=in_, identity=ident), [out], [in_, ident])

    def V(e, fn, outs, ins):
        return kb.op(e, fn, outs, ins)

    def tt_(e, out, in0, in1, op):
        eng = kb.engs[e]
        return kb.op(e, lambda: eng.tensor_tensor(out=out, in0=in0, in1=in1, op=op), [out], [in0, in1])

    def ts_(e, out, in0, s1, s2, op0, op1=ALU.bypass):
        eng = kb.engs[e]
        ins = [in0] + [s for s in (s1, s2) if not isinstance(s, (int, float)) and s is not None]
        if s2 is None:
            return kb.op(e, lambda: eng.tensor_scalar(out=out, in0=in0, scalar1=s1, scalar2=None, op0=op0), [out], ins)
        return kb.op(e, lambda: eng.tensor_scalar(out=out, in0=in0, scalar1=s1, scalar2=s2, op0=op0, op1=op1), [out], ins)

    def stt_(out, in0, scalar, in1, op0, op1):
        ins = [in0, in1] + ([] if isinstance(scalar, (int, float)) else [scalar])
        return kb.op('dve', lambda: nc.vector.scalar_tensor_tensor(out=out, in0=in0, scalar=scalar, in1=in1, op0=op0, op1=op1),
                     [out], ins)

    def act_(out, in_, func, bias=0.0, scale=1.0):
        ins = [in_] + [s for s in (bias, scale) if not isinstance(s, (int, float))]
        return kb.op('act', lambda: nc.scalar.activation(out=out, in_=in_, func=func, bias=bias, scale=scale), [out], ins)

    def copy_(e, out, in_):
        if e == 'act':
            return kb.op('act', lambda: nc.scalar.copy(out=out, in_=in_), [out], [in_])
        eng = kb.engs[e]
        return kb.op(e, lambda: eng.tensor_copy(out=out, in_=in_), [out], [in_])

    def memset_(e, ap, val):
        eng = kb.engs[e]
        return kb.op(e, lambda: eng.memset(ap, val), [ap], [])

    def winv(l):
        return win_d[l].rearrange("(kc p) c -> p kc c", p=128)

    gk = nc.alloc_sbuf_tensor("gk_sb", [128, NGK], F32)
    kb.dma(gk[:], gk_d[:, :], 'c0')

    def G(name):
        o, w = GK_COLS[name]
        return gk[:, o:o + w]
    identf, mle, mgt, mge, onesf = G('ident'), G('mle'), G('mgt'), G('mge'), G('ones')
    cb = nc.alloc_sbuf_tensor("cb", [128, 3, 128], BF16)
    for i, nm in enumerate(('ident', 'mle', 'mge')):
        copy_('dve', cb[:, i, :], G(nm))
    identb = cb[:, 0, :]
    hT = nc.alloc_sbuf_tensor("hT", [128, 8, S], BF16)
    pk = nc.alloc_sbuf_tensor("pk_sb", [128, NPK], F32)
    cond = nc.alloc_sbuf_tensor("cond", [128, 48], F32)
    gm = nc.alloc_sbuf_tensor("gm", [128, 16], F32)
    cact = nc.alloc_sbuf_tensor("cact", [128, 8], F32)
    dtraw = nc.alloc_sbuf_tensor("dtraw", [128, 16, 32], F32)
    act_(cact[:], G('c'), AF.Silu)

    def PK(name, rows=128):
        o, w = PK_COLS[name]
        return pk[0:rows, o:o + w]

    def load_x_phase():
        with ExitStack() as es:
            xin = [es.enter_context(nc.sbuf_tensor(f"xin{i}", [128, D], F32)) for i in range(2)]
            xo = [es.enter_context(nc.sbuf_tensor(f"xo{i}", [128, 8, 128], F32)) for i in range(2)]
            for t in range(16):
                xi = xin[t % 2]
                kb.dma(xi[:], x_d[t * 128:(t + 1) * 128, :], f'xin{t % 2}')
                for half in range(2):
                    p = P()
                    for j in range(4):
                        kc = half * 4 + j
                        tr(p[:, j * 128:(j + 1) * 128], xi[:, kc * 128:(kc + 1) * 128], identf)
                    copy_('dve' if half == 0 else 'act', xo[t % 2][:, half * 4:(half + 1) * 4, :],
                          p[:, :].rearrange("p (j t) -> p j t", j=4))
                kb.dma(xs_d[:, :, t * 128:(t + 1) * 128], xo[t % 2][:], f'xo{t % 2}')
            kb.barrier()

    def rope_phase():
        with ExitStack() as es:
            posi = es.enter_context(nc.sbuf_tensor("posi", [128, S], I32))
            ki = es.enter_context(nc.sbuf_tensor("rki", [128, S], I32))
            ang = es.enter_context(nc.sbuf_tensor("ang", [128, S], F32))
            u = es.enter_context(nc.sbuf_tensor("ru", [128, S], F32))
            kf = es.enter_context(nc.sbuf_tensor("rkf", [128, S], F32))
            r = es.enter_context(nc.sbuf_tensor("rr", [128, S], F32))
            m = es.enter_context(nc.sbuf_tensor("rm", [128, S], F32))
            kb.dma(posi[:], pos_d[:, :], 'c0')
            copy_('dve', ang[:], posi[:])
            ts_('dve', ang[:], ang[:], G('freq'), None, ALU.mult)
            TWO_PI = 2.0 * math.pi
            C1 = 6.28125
            C2 = TWO_PI - C1
            for which, phi in ((0, math.pi / 2), (1, 0.0)):
                ts_('dve', u[:], ang[:], 1.0 / TWO_PI, phi / TWO_PI + 0.5, ALU.mult, ALU.add)
                copy_('dve', ki[:], u[:])
                copy_('dve', kf[:], ki[:])
                stt_(r[:], kf[:], -C1, ang[:], ALU.mult, ALU.add)
                stt_(r[:], kf[:], -C2, r[:], ALU.mult, ALU.add)
                if phi != 0.0:
                    ts_('dve', r[:], r[:], phi, None, ALU.add)
                ts_('dve', m[:], r[:], -math.pi, None, ALU.is_lt)
                stt_(r[:], m[:], TWO_PI, r[:], ALU.mult, ALU.add)
                ts_('dve', m[:], r[:], math.pi, None, ALU.is_gt)
                stt_(r[:], m[:], -TWO_PI, r[:], ALU.mult, ALU.add)
                ts_('dve', r[:], r[:], -3.1415925, 3.1415925, ALU.max, ALU.min)
                act_(u[:], r[:], AF.Sin)
                if which == 1:
                    ts_('dve', u[:], u[:], G('phase'), None, ALU.mult)
                kb.dma(cs_d[:, which, :], u[:], 'cs_st')
            kb.barrier()

    def cond_phase(l):
        with ExitStack() as es:
            aw = [es.enter_context(nc.sbuf_tensor(f"aw{l}_{i}", [128, 8, 512], F32)) for i in range(2)]
            kb.dma(pk[:], pk_d[l], 'pk')
            pc = P()
            src = adaw_d[l].rearrange("(kc p) c -> p kc c", p=128)
            for cc in range(12):
                w = aw[cc % 2]
                kb.dma(w[:], src[:, :, cc * 512:(cc + 1) * 512], f'aw{cc % 2}')
                for j in range(4):
                    col = cc * 4 + j
                    for kc in range(8):
                        mm(pc[:, col:col + 1], w[:, kc, j * 128:(j + 1) * 128], cact[:, kc:kc + 1], kc == 0, kc == 7)
            tt_('dve', cond[:], pc[:, 0:48], PK('adab'), ALU.add)
            stt_(gm[:, 0:8], cond[:, 8:16], 1.0, PK('n1g'), ALU.add, ALU.mult)
            stt_(gm[:, 8:16], cond[:, 32:40], 1.0, PK('n2g'), ALU.add, ALU.mult)
            kb.barrier()

    def norm_phase(l, which, tag):
        with ExitStack() as es:
            xt = [es.enter_context(nc.sbuf_tensor(f"nx{tag}_{i}", [128, 8, 512], F32)) for i in range(2)]
            sq = [es.enter_context(nc.sbuf_tensor(f"nsq{tag}_{i}", [128, 512], F32)) for i in range(2)]
            rs = [es.enter_context(nc.sbuf_tensor(f"nrs{tag}_{i}", [128, 512], F32)) for i in range(2)]
            t2 = [es.enter_context(nc.sbuf_tensor(f"nt2{tag}_{i}", [128, 512], F32)) for i in range(2)]
            sh0 = 0 if which == 0 else 24
            for tt in range(4):
                X = xt[tt % 2]
                kb.dma(X[:], xs_d[:, :, tt * 512:(tt + 1) * 512], f'nx{tt % 2}')
                pss = P()
                for kc in range(8):
                    act_(sq[kc % 2][:], X[:, kc, :], AF.Square)
                    mm(pss[:, :], onesf, sq[kc % 2][:], kc == 0, kc == 7)
                R_ = rs[tt % 2]
                act_(R_[:], pss[:, :], AF.Sqrt, bias=EPSC[:, 0:1], scale=1.0 / D)
                V('dve', lambda: nc.vector.reciprocal(out=R_[:], in_=R_[:]), [R_[:]], [R_[:]])
                for kc in range(8):
                    T = t2[kc % 2]
                    tt_('dve', T[:], X[:, kc, :], R_[:], ALU.mult)
                    ts_('pool', hT[:, kc, tt * 512:(tt + 1) * 512], T[:], gm[:, which * 8 + kc:which * 8 + kc + 1],
                        cond[:, sh0 + kc:sh0 + kc + 1], ALU.mult, ALU.add)
            kb.barrier()

    epsc = nc.alloc_sbuf_tensor("epsc", [128, 2], F32)
    EPSC = epsc
    memset_('dve', epsc[:, 0:1], EPS)
    memset_('dve', epsc[:, 1:2], 1.0)

    def attn_tok(g, ti):
        if g == 0:
            return slice(128 * ti, 128 * ti + 128, 1)
        if g == 1:
            st = 512 * (ti // 4) + (ti % 4)
            return slice(st, st + 4 * 127 + 1, 4)
        return slice(ti, ti + 16 * 127 + 1, 16)

    def attn_prev(g, ti):
        if g == 0:
            return ti - 1 if ti >= 1 else None
        if g == 1:
            return ti - 4 if ti >= 4 else None
        return None

    def attn_phase(l):
        with ExitStack() as es:
            A = lambda n, sh, dt: es.enter_context(nc.sbuf_tensor(f"{n}_{l}", sh, dt))
            cs = A("acs", [128, 2, S], F32)
            qk = A("aqk", [128, 2, 2, S], BF16)
            vaug = A("avaug", [128, 16, 4, 128], BF16)
            acc = A("aacc", [128, 4, S], F32)
            wq = [A(f"awq{i}", [128, 8, 256], BF16) for i in range(3)]
            wst = [A(f"awst{i}", [128, 8, 256], F32) for i in range(2)]
            ta = [A(f"ata{i}", [128, 512], F32) for i in range(3)]
            tb = [A(f"atb{i}", [128, 512], F32) for i in range(3)]
            nrope = 0
            NPT = 6
            pt = [A(f"apt{i}", [128, 512], BF16) for i in range(NPT)]
            ptm = [A(f"aptm{i}", [128, 4, 128], BF16) for i in range(NPT)]
            rd = A("ard", [64, 4, 512], F32)
            yo = [A(f"ayo{i}", [64, 4, 512], BF16) for i in range(2)]
            kb.dma(cs[:], cs_d[:, :, :], 'acs')
            memset_('pool', vaug[:, :, :, 64:128], 1.0)
            wi = 0
            pi = 0
            for hh in range(2):
                for g in range(3):
                    for which, off in ((0, OQ), (1, OK_)):
                        c0 = off + g * 512 + hh * 256
                        W = wq[wi % 3]
                        kb.dma(wst[wi % 2][:], winv(l)[:, :, c0:c0 + 256], f'awst{wi % 2}')
                        copy_('pool', W[:], wst[wi % 2][:])
                        wi += 1
                        for j in range(2):
                            for tt in range(4):
                                tsl = slice(tt * 512, (tt + 1) * 512)
                                p = P()
                                for kc in range(8):
                                    mm(p[:, :], W[:, kc, j * 128:(j + 1) * 128], hT[:, kc, tsl], kc == 0, kc == 7)
                                TA, TB = ta[tt % 2], tb[tt % 2]
                                tt_('dve', TA[:], p[:, :], cs[:, 0, tsl], ALU.mult)
                                for q4 in range(4):
                                    o0 = q4 * 32
                                    i0 = o0 + 32 if q4 % 2 == 0 else o0 - 32
                                    tt_('dve', TB[o0:o0 + 32, :], p[i0:i0 + 32, :], cs[o0:o0 + 32, 1, tsl], ALU.mult)
                                tt_('pool', qk[:, which, j, tsl], TA[:], TB[:], ALU.add)
                    c0 = OV + g * 512 + hh * 256
                    W = wq[wi % 3]
                    kb.dma(wst[wi % 2][:], winv(l)[:, :, c0:c0 + 256], f'awst{wi % 2}')
                    copy_('pool', W[:], wst[wi % 2][:])
                    wi += 1
                    for ti in range(16):
                        tok = attn_tok(g, ti)
                        p = P()
                        for kc in range(8):
                            mm(p[:, 0:256], hT[:, kc, tok], W[:, kc, :], kc == 0, kc == 7)
                        copy_('act', vaug[:, ti, :, 0:64], p[:, 0:256].rearrange("p (h d) -> p h d", h=4))
                    def stage_s(ti):
                        nonlocal pi
                        tq = attn_tok(g, ti)
                        pv = attn_prev(g, ti)
                        kts = ([(pv, 2)] if pv is not None else []) + [(ti, 1)]
                        pms = []
                        for kt, mk in kts:
                            tk = attn_tok(g, kt)
                            PT, PM = pt[pi % NPT], ptm[pi % NPT]
                            pi += 1
                            PT3 = PT[:, :].rearrange("p (h q) -> p h q", h=4)
                            for par in range(2):
                                p = P()
                                bp = par * 64
                                for j in range(2):
                                    mm(p[:, j * 128:(j + 1) * 128], qk[bp:bp + 64, 1, j, tk], qk[bp:bp + 64, 0, j, tq])
                                act_(PT3[:, par::2, :], p[:, 0:256].rearrange("p (j q) -> p j q", j=2), AF.Exp, scale=0.125)
                            tt_('dve', PM[:], PT3, cb[:, mk, :][:, None, :].broadcast_to([128, 4, 128]), ALU.mult)
                            pms.append((kt, PM))
                        return pms

                    def stage_pv(ti, pms):
                        tq = attn_tok(g, ti)
                        po = P()
                        for h in range(4):
                            for idx, (kt, PM) in enumerate(pms):
                                mm(po[:, h * 128:(h + 1) * 128], vaug[:, kt, h, :], PM[:, h, :], idx == 0, idx == len(pms) - 1)
                        pov = po[:, :].rearrange("p (h q) -> p h q", h=4)
                        if g == 0:
                            copy_('dve', acc[:, :, tq], pov)
                        else:
                            tt_('dve', acc[:, :, tq], pov, acc[:, :, tq], ALU.add)

                    prev = None
                    for ti in range(16):
                        cur = (ti, stage_s(ti))
                        if prev is not None:
                            stage_pv(*prev)
                        prev = cur
                    stage_pv(*prev)
                for tt in range(4):
                    tsl = slice(tt * 512, (tt + 1) * 512)
                    V('dve', lambda: nc.vector.reciprocal(out=rd[:], in_=acc[64:128, :, tsl]), [rd[:]], [acc[64:128, :, tsl]])
                    Y = yo[tt % 2]
                    tt_('dve', Y[:], acc[0:64, :, tsl], rd[:], ALU.mult)
                    kb.dma(ya_d[:, hh * 4:(hh + 1) * 4, tsl], Y[:], f'ayo{tt % 2}')
            kb.barrier()

    def lru_phase(l):
        with ExitStack() as es:
            A = lambda n, sh, dt: es.enter_context(nc.sbuf_tensor(f"{n}_{l}", sh, dt))
            wlx = A("lwlx", [128, 8, 1280], BF16)
            wlg = A("lwlg", [128, 8, 1280], BF16)
            wr = A("lwr", [80, 16, 80], BF16)
            wi_ = A("lwi", [80, 16, 80], BF16)
            cneg = A("lcneg", [80, 16], F32)
            c2 = A("lc2", [80, 16], F32)
            xpre_ = [A(f"lxpre{i}", [80, 3 + S], F32) for i in range(2)]
            xc_ = [A(f"lxc{i}", [80, S], F32) for i in range(2)]
            xcb_ = [A(f"lxcb{i}", [80, S], BF16) for i in range(2)]
            rg_ = [A(f"lrg{i}", [80, S], F32) for i in range(2)]
            ig_ = [A(f"lig{i}", [80, S], F32) for i in range(2)]
            Aa = A("laa", [80, S], F32)
            T1 = A("lt1", [80, S], F32)
            uu = A("luu", [80, S], F32)
            hs = A("lhs", [80, S], F32)
            gl = A("lgl", [80, S], F32)
            yo = [A(f"lyo{i}", [80, S], BF16) for i in range(2)]
            for (w, off, nm) in ((wlx, OLX, 'lwlx'), (wlg, OLG, 'lwlg')):
                for c0 in range(0, 1280, 320):
                    kb.dma(w[:, :, c0:c0 + 320], winv(l)[:, :, off + c0:off + c0 + 320], nm, q='pool')
            kb.dma(wr[:], lwr_d[l].rearrange("k i j -> i k j"), 'lwr', q='pool')
            kb.dma(wi_[:], lwi_d[l].rearrange("k i j -> i k j"), 'lwi', q='pool')
            lcw = PK('lcw', 80)
            lcb, lbr, lbi, llam = PK('lcb', 80), PK('lbr', 80), PK('lbi', 80), PK('llam', 80)
            act_(cneg[:], llam, AF.Exp, scale=-1.0)
            act_(cneg[:], cneg[:], AF.Ln, bias=EPSC[0:80, 1:2])
            ts_('dve', c2[:], cneg[:], -16.0, None, ALU.mult)
            ts_('dve', cneg[:], cneg[:], -8.0, None, ALU.mult)
            for i in range(2):
                memset_('dve', xpre_[i][:, 0:3], 0.0)
            for k in range(16):
                ksl = slice(k * 80, (k + 1) * 80)
                xpre, xc, xcb, rg, ig = xpre_[k % 2], xc_[k % 2], xcb_[k % 2], rg_[k % 2], ig_[k % 2]
                for tt in range(4):
                    tsl = slice(tt * 512, (tt + 1) * 512)
                    p = P()
                    for kc in range(8):
                        mm(p[0:80, :], wlx[:, kc, ksl], hT[:, kc, tsl], kc == 0, kc == 7)
                    copy_('act', xpre[:, 3 + tt * 512:3 + (tt + 1) * 512], p[0:80, :])
                ts_('dve', xc[:], xpre[:, 0:S], lcw[:, k * 4:k * 4 + 1], lcb[:, k:k + 1], ALU.mult, ALU.add)
                for j in range(1, 4):
                    stt_(xc[:], xpre[:, j:j + S], lcw[:, k * 4 + j:k * 4 + j + 1], xc[:], ALU.mult, ALU.add)
                copy_('pool', xcb[:], xc[:])
                for (wg, bb, dst) in ((wr, lbr, rg), (wi_, lbi, ig)):
                    for tt in range(4):
                        tsl = slice(tt * 512, (tt + 1) * 512)
                        p = P()
                        mm(p[0:80, :], wg[:, k, :], xcb[:, tsl])
                        act_(dst[:, tsl], p[0:80, :], AF.Sigmoid, bias=bb[:, k:k + 1])
                act_(Aa[:], rg[:], AF.Exp, scale=cneg[:, k:k + 1])
                act_(T1[:], rg[:], AF.Exp, scale=c2[:, k:k + 1])
                ts_('dve', T1[:], T1[:], -1.0, 1.0, ALU.mult, ALU.add)
                ts_('dve', T1[:], T1[:], 1e-12, None, ALU.max)
                act_(T1[:], T1[:], AF.Sqrt)
                tt_('pool', uu[:], ig[:], xc[:], ALU.mult)
                tt_('dve', uu[:], uu[:], T1[:], ALU.mult)
                V('dve', lambda: nc.vector.tensor_tensor_scan(out=hs[:], data0=Aa[:], data1=uu[:], initial=0.0,
                                                              op0=ALU.mult, op1=ALU.add), [hs[:]], [Aa[:], uu[:]])
                for tt in range(4):
                    tsl = slice(tt * 512, (tt + 1) * 512)
                    p = P()
                    for kc in range(8):
                        mm(p[0:80, :], wlg[:, kc, ksl], hT[:, kc, tsl], kc == 0, kc == 7)
                    act_(gl[:, tsl], p[0:80, :], AF.Gelu)
                Y = yo[k % 2]
                tt_('dve', Y[:], hs[:], gl[:], ALU.mult)
                kb.dma(yl_d[:, k, :], Y[:], f'lyo{k % 2}')
            kb.barrier()

    def ssd_proj_phase(l):
        with ExitStack() as es:
            A = lambda n, sh, dt: es.enter_context(nc.sbuf_tensor(f"{n}_{l}", sh, dt))
            W = [A(f"sw{i}", [128, 8, 512], BF16) for i in range(2)]
            wdt = A("swdt", [128, 8, 32], BF16)
            pre = [A(f"spre{i}", [128, 3 + S], F32) for i in range(2)]
            cv = [A(f"scv{i}", [128, S], F32) for i in range(2)]
            ob = [A(f"sob{i}", [128, S], BF16) for i in range(2)]
            scw, scb = PK('scw'), PK('scb')
            for i in range(2):
                memset_('dve', pre[i][:, 0:3], 0.0)
            n = 0
            for cg in range(8):
                Wt = W[cg % 2]
                kb.dma(Wt[:], winv(l)[:, :, OXBC + cg * 512:OXBC + (cg + 1) * 512], f'sw{cg % 2}', q='pool')
                for j in range(4):
                    fc = cg * 4 + j
                    PR, CV, OB = pre[n % 2], cv[n % 2], ob[n % 2]
                    n += 1
                    for tt in range(4):
                        p = P()
                        for kc in range(8):
                            mm(p[:, :], Wt[:, kc, j * 128:(j + 1) * 128], hT[:, kc, tt * 512:(tt + 1) * 512], kc == 0, kc == 7)
                        copy_('act', PR[:, 3 + tt * 512:3 + (tt + 1) * 512], p[:, :])
                    ts_('dve', CV[:], PR[:, 0:S], scw[:, fc * 4:fc * 4 + 1], None, ALU.mult)
                    for jj in range(1, 4):
                        stt_(CV[:], PR[:, jj:jj + S], scw[:, fc * 4 + jj:fc * 4 + jj + 1], CV[:], ALU.mult, ALU.add)
                    act_(OB[:], CV[:], AF.Silu, bias=scb[:, fc:fc + 1])
                    kb.dma(xbc_d[:, fc, :], OB[:], f'sob{(n - 1) % 2}')
            for cg in range(4):
                Wt = W[cg % 2]
                kb.dma(Wt[:], winv(l)[:, :, OZ + cg * 512:OZ + (cg + 1) * 512], f'sw{cg % 2}', q='pool')
                for j in range(4):
                    fc = cg * 4 + j
                    OB = ob[n % 2]
                    n += 1
                    for tt in range(4):
                        p = P()
                        for kc in range(8):
                            mm(p[:, :], Wt[:, kc, j * 128:(j + 1) * 128], hT[:, kc, tt * 512:(tt + 1) * 512], kc == 0, kc == 7)
                        act_(OB[:, tt * 512:(tt + 1) * 512], p[:, :], AF.Silu)
                    kb.dma(z_d[:, fc, :], OB[:], f'sob{(n - 1) % 2}')
            kb.dma(wdt[:], winv(l)[:, :, ODT:ODT + 32], 'swdt', q='pool')
            for c in range(16):
                p = P()
                for kc in range(8):
                    mm(p[:, 0:32], hT[:, kc, c * 128:(c + 1) * 128], wdt[:, kc, :], kc == 0, kc == 7)
                tt_('dve', dtraw[:, c, :], p[:, 0:32], PK('dtb'), ALU.add)
            kb.barrier()

    def ssd_scan_phase(l):
        with ExitStack() as es:
            A = lambda n, sh, dt: es.enter_context(nc.sbuf_tensor(f"{n}_{l}", sh, dt))
            H = A("cH", [128, 32, 64], F32)
            Hb = A("cHb", [128, 32, 64], BF16)
            expA = A("cexpA", [128, 32], F32)
            xsT = [A(f"cxs{i}", [128, 16, 128], BF16) for i in range(2)]
            BT = [A(f"cbt{i}", [128, 8, 128], BF16) for i in range(2)]
            CT = [A(f"cct{i}", [128, 8, 128], BF16) for i in range(2)]
            zT = [A(f"czt{i}", [128, 16, 128], BF16) for i in range(2)]
            dt_ = A("cdt", [128, 32], F32)
            dtw = A("cdtw", [128, 32], F32)
            a_ = A("ca", [128, 32], F32)
            ex = A("cex", [128, 3, 32], F32)
            abig = A("cabig", [128, 32, 64], F32)
            xdt = A("cxdt", [128, 32, 64], BF16)
            xdtw = A("cxdtw", [128, 32, 64], BF16)
            Btok = A("cbtok", [128, 8, 128], BF16)
            ab = [A(f"cab{i}", [128, 4, 128], F32) for i in range(2)]
            CBm = [A(f"ccbm{i}", [128, 128], F32) for i in range(2)]
            Lm = [A(f"clm{i}", [128, 512], F32) for i in range(2)]
            Gm = [A(f"cgm{i}", [128, 4, 128], BF16) for i in range(2)]
            ds = [A(f"cds{i}", [128, 256], F32) for i in range(2)]
            t1 = [A(f"ct1{i}", [128, 256], F32) for i in range(2)]
            yT = A("cyT", [128, 16, 128], F32)
            yg = A("cyg", [128, 16, 128], F32)
            sq = A("csq", [128, 16, 128], F32)
            rs = A("crs", [128, 128], F32)
            yo = [A(f"cyo{i}", [128, 16, 128], BF16) for i in range(2)]
            sdd, sng = PK('sdd'), PK('sng')
            memset_('dve', H[:], 0.0)
            memset_('pool', Hb[:], 0.0)
            act_(expA[:], PK('alog'), AF.Exp)
            for c in range(16):
                csl = slice(c * 128, (c + 1) * 128)
                b = c % 2
                kb.dma(xsT[b][:], xbc_d[:, 0:16, csl], f'cxs{b}')
                kb.dma(BT[b][:], xbc_d[:, 16:24, csl], f'cbt{b}')
                kb.dma(CT[b][:], xbc_d[:, 24:32, csl], f'cct{b}')
                kb.dma(zT[b][:], z_d[:, :, csl], f'czt{b}')
                act_(dt_[:], dtraw[:, c, :], AF.Exp)
                act_(dt_[:], dt_[:], AF.Ln, bias=EPSC[:, 1:2])
                stt_(a_[:], dt_[:], -1.0, expA[:], ALU.mult, ALU.mult)
                p3 = P()
                mm(p3[:, 0:32], mle, a_[:])
                mm(p3[:, 32:64], mgt, a_[:])
                mm(p3[:, 64:96], onesf, a_[:])
                act_(ex[:], p3[:, 0:96].rearrange("p (k h) -> p k h", k=3), AF.Exp)
                tt_('dve', dtw[:], dt_[:], ex[:, 1, :], ALU.mult)
                copy_('pool', abig[:], a_[:, :][:, :, None].broadcast_to([128, 32, 64]))
                for bb in range(2):
                    pT = P()[:, :].bitcast(BF16)
                    for j in range(8):
                        tr(pT[:, j * 128:(j + 1) * 128], xsT[b][:, bb * 8 + j, :], identb)
                    pv = pT.rearrange("p (h d) -> p h d", h=16)
                    hsl = slice(bb * 16, (bb + 1) * 16)
                    tt_('dve', xdt[:, hsl, :], pv, dt_[:, hsl][:, :, None].broadcast_to([128, 16, 64]), ALU.mult)
                    tt_('dve', xdtw[:, hsl, :], pv, dtw[:, hsl][:, :, None].broadcast_to([128, 16, 64]), ALU.mult)
                pB = P()[:, :].bitcast(BF16)
                for g in range(8):
                    tr(pB[:, g * 128:(g + 1) * 128], BT[b][:, g, :], identb)
                copy_('act', Btok[:], pB.rearrange("p (g n) -> p g n", g=8))
                def stage_a(g):
                    i2 = g % 2
                    pcb = P()
                    mm(pcb[:, 0:128], BT[b][:, g, :], CT[b][:, g, :])
                    tt_('dve', CBm[i2][:], pcb[:, 0:128], mle, ALU.mult)
                    tt_('pool', ab[i2][:], mgt[:, None, :].broadcast_to([128, 4, 128]),
                        a_[:, 4 * g:4 * g + 4][:, :, None].broadcast_to([128, 4, 128]), ALU.mult)
                    pD = P()
                    for h in range(4):
                        mm(pD[:, h * 128:(h + 1) * 128], ab[i2][:, h, :], mle)
                    act_(Lm[i2][:], pD[:, :], AF.Exp)
                    tt_('dve', Gm[i2][:], Lm[i2][:, :].rearrange("p (h l) -> p h l", h=4),
                        CBm[i2][:, None, :].broadcast_to([128, 4, 128]), ALU.mult)
                    pS = P()
                    for hp in range(2):
                        h0 = 4 * g + 2 * hp
                        mm(pS[:, hp * 128:(hp + 1) * 128], abig[:, h0:h0 + 2, :].rearrange("p h d -> p (h d)"), mle)
                    act_(ds[i2][:], pS[:, 0:256], AF.Exp)

                def stage_b(g):
                    i2 = g % 2
                    pY = P()
                    for hp in range(2):
                        for hh in range(2):
                            h = 2 * hp + hh
                            mm(pY[hh * 64:(hh + 1) * 64, hp * 128:(hp + 1) * 128], xdt[:, 4 * g + h, :], Gm[i2][:, h, :])
                    pZ = P()
                    for hp in range(2):
                        h0 = 4 * g + 2 * hp
                        mm(pZ[:, hp * 128:(hp + 1) * 128], Hb[:, h0:h0 + 2, :].rearrange("p h d -> p (h d)"), CT[b][:, g, :])
                    tt_('dve', t1[i2][:], pZ[:, 0:256], ds[i2][:], ALU.mult)
                    tt_('dve', yT[:, 2 * g:2 * g + 2, :], pY[:, 0:256].rearrange("p (f l) -> p f l", f=2),
                        t1[i2][:, :].rearrange("p (f l) -> p f l", f=2), ALU.add)

                stage_a(0)
                for g in range(8):
                    if g + 1 < 8:
                        stage_a(g + 1)
                    stage_b(g)
                for fc in range(16):
                    stt_(yT[:, fc, :], xsT[b][:, fc, :], sdd[:, fc:fc + 1], yT[:, fc, :], ALU.mult, ALU.add)
                tt_('dve', H[:], H[:], ex[:, 2, :][:, :, None].broadcast_to([128, 32, 64]), ALU.mult)
                for gp in range(4):
                    pst = P()
                    for gg in range(2):
                        g = 2 * gp + gg
                        mm(pst[:, gg * 256:(gg + 1) * 256], Btok[:, g, :],
                           xdtw[:, 4 * g:4 * g + 4, :].rearrange("p h d -> p (h d)"))
                    tt_('dve', H[:, 8 * gp:8 * gp + 8, :], pst[:, :].rearrange("p (h d) -> p h d", h=8),
                        H[:, 8 * gp:8 * gp + 8, :], ALU.add)
                copy_('pool', Hb[:], H[:])
                tt_('dve', yg[:], yT[:], zT[b][:], ALU.mult)
                act_(sq[:], yg[:], AF.Square)
                pss = P()
                for fc in range(16):
                    mm(pss[:, 0:128], onesf, sq[:, fc, :], fc == 0, fc == 15)
                act_(rs[:], pss[:, 0:128], AF.Sqrt, bias=EPSC[:, 0:1], scale=1.0 / 2048)
                V('dve', lambda: nc.vector.reciprocal(out=rs[:], in_=rs[:]), [rs[:]], [rs[:]])
                tt_('dve', yg[:], yg[:], rs[:, None, :].broadcast_to([128, 16, 128]), ALU.mult)
                tt_('pool', yo[b][:], yg[:], sng[:, :, None].broadcast_to([128, 16, 128]), ALU.mult)
                kb.dma(ys_d[:, :, csl], yo[b][:], f'cyo{b}')
            kb.barrier()

    def merge_phase(l):
        with ExitStack() as es0:
            mT = es0.enter_context(nc.sbuf_tensor(f"gmT_{l}", [128, 8, S], BF16))
            with ExitStack() as es:
                A = lambda n, sh, dt: es.enter_context(nc.sbuf_tensor(f"{n}_{l}", sh, dt))
                wa = [A(f"gwa{i}", [64, 8, 256], BF16) for i in range(2)]
                ws = [A(f"gws{i}", [128, 16, 256], BF16) for i in range(2)]
                wl = [A(f"gwl{i}", [80, 16, 256], BF16) for i in range(2)]
                wm = [A(f"gwm{i}", [128, 3, 8, 256], BF16) for i in range(2)]
                ya = [A(f"gya{i}", [64, 8, 512], BF16) for i in range(1)]
                ys = [A(f"gys{i}", [128, 16, 512], BF16) for i in range(1)]
                yl = [A(f"gyl{i}", [80, 16, 512], BF16) for i in range(1)]
                sgt = [A(f"gsg{i}", [128, 512], F32) for i in range(2)]
                mac = [A(f"gmac{i}", [128, 512], F32) for i in range(2)]
                tmp = [A(f"gtmp{i}", [128, 512], F32) for i in range(2)]

                def load_q(qt):
                    i = qt % 2
                    qsl = slice(qt * 256, (qt + 1) * 256)
                    kb.dma(wa[i][:], wba_d[l].rearrange("(h d) c -> d h c", d=64)[:, :, qsl], f'gwa{i}', q='pool')
                    kb.dma(ws[i][:], wbs_d[l].rearrange("(k p) c -> p k c", p=128)[:, :, qsl], f'gws{i}', q='pool')
                    kb.dma(wl[i][:], wbl_d[l].rearrange("(k p) c -> p k c", p=80)[:, :, qsl], f'gwl{i}', q='pool')
                    for b in range(3):
                        c0 = OMG + b * 1024 + qt * 256
                        kb.dma(wm[i][:, b, :, :], winv(l)[:, :, c0:c0 + 256], f'gwm{i}', q='pool')
                n = 0
                ny = 0
                load_q(0)
                for qt in range(4):
                    if qt + 1 < 4:
                        load_q(qt + 1)
                    wi_ = qt % 2
                    for tt in range(4):
                        tsl = slice(tt * 512, (tt + 1) * 512)
                        yi = 0
                        ny += 1
                        kb.dma(ya[yi][:], ya_d[:, :, tsl], f'gya{yi}')
                        kb.dma(ys[yi][:], ys_d[:, :, tsl], f'gys{yi}')
                        kb.dma(yl[yi][:], yl_d[:, :, tsl], f'gyl{yi}')
                        for oc in range(2):
                            osl = slice(oc * 128, (oc + 1) * 128)
                            M = mac[n % 2]
                            n += 1
                            for b, (y, w, nk) in enumerate(((ya[yi], wa[wi_], 8), (ys[yi], ws[wi_], 16), (yl[yi], wl[wi_], 16))):
                                pP = P()
                                for k in range(nk):
                                    mm(pP[:, :], w[:, k, osl], y[:, k, :], k == 0, k == nk - 1)
                                pG = P()
                                for kc in range(8):
                                    mm(pG[:, :], wm[wi_][:, b, kc, osl], hT[:, kc, tsl], kc == 0, kc == 7)
                                SG = sgt[b % 2]
                                act_(SG[:], pG[:, :], AF.Sigmoid)
                                if b == 0:
                                    tt_('dve', M[:], SG[:], pP[:, :], ALU.mult)
                                else:
                                    T = tmp[b % 2]
                                    tt_('dve', T[:], SG[:], pP[:, :], ALU.mult)
                                    tt_('pool', M[:], M[:], T[:], ALU.add)
                            copy_('pool', mT[:, qt * 2 + oc, tsl], M[:])
                kb.barrier()
            with ExitStack() as es:
                A = lambda n, sh, dt: es.enter_context(nc.sbuf_tensor(f"{n}_{l}", sh, dt))
                wo = A("gwo", [128, 8, 1024], BF16)
                X = [A(f"gX{i}", [128, 8, 512], F32) for i in range(2)]
                for c0 in range(0, 1024, 512):
                    kb.dma(wo[:, :, c0:c0 + 512], wo_d[l].rearrange("(kc p) c -> p kc c", p=128)[:, :, c0:c0 + 512], 'gwo', q='pool')
                for tt in range(4):
                    tsl = slice(tt * 512, (tt + 1) * 512)
                    Xt = X[tt % 2]
                    kb.dma(Xt[:], xs_d[:, :, tsl], f'gX{tt % 2}')
                    for oc in range(8):
                        p = P()
                        for kc in range(8):
                            mm(p[:, :], wo[:, kc, oc * 128:(oc + 1) * 128], mT[:, kc, tsl], kc == 0, kc == 7)
                        stt_(Xt[:, oc, :], p[:, :], cond[:, 16 + oc:17 + oc], Xt[:, oc, :], ALU.mult, ALU.add)
                    kb.dma(xs_d[:, :, tsl], Xt[:], f'gX{tt % 2}')
                kb.barrier()

    def moe_phase(l):
        with ExitStack() as es:
            A = lambda n, sh, dt: es.enter_context(nc.sbuf_tensor(f"{n}_{l}", sh, dt))
            yacc = A("myacc", [128, 16, 1024], F32)
            wt = A("mwt", [128, 16, 32], F32)
            wrt = A("mwrt", [128, 8, 36], BF16)
            lg = A("mlg", [128, 36], F32)
            sm = A("msm", [128, 16], F32)
            oh = A("moh", [128, 4], F32)
            pen = A("mpen", [128, 4], F32)
            eg4 = A("meg4", [128, 4], F32)
            lem = A("mlem", [128, 4, 8], F32)
            m8 = A("mm8", [128, 8], F32)
            eq1 = A("meq1", [128, 32], F32)
            eq2 = A("meq2", [128, 32], F32)
            actb = [A(f"mact{i}", [128, 4, S], BF16) for i in range(2)]
            wg = [A(f"mwg{i}", [128, 8, 512], BF16) for i in range(2)]
            wu = [A(f"mwu{i}", [128, 8, 512], BF16) for i in range(2)]
            wd = [A(f"mwd{i}", [128, 4, 1024], BF16) for i in range(2)]
            sgt = [A(f"msg{i}", [128, 512], F32) for i in range(2)]
            X = [A(f"mX{i}", [128, 8, 128], F32) for i in range(2)]
            kb.dma(wrt[:, :, 0:4], rwg_d[l].rearrange("(kc p) g -> p kc g", p=128), 'mwrt', q='pool')
            kb.dma(wrt[:, :, 4:36], rwe_d[l].rearrange("(kc p) g -> p kc g", p=128), 'mwrt', q='pool')
            rb = PK('rb')
            for t in range(16):
                tk = slice(t * 128, (t + 1) * 128)
                p = P()
                for kc in range(8):
                    mm(p[:, 0:36], hT[:, kc, tk], wrt[:, kc, :], kc == 0, kc == 7)
                tt_('dve', lg[:], p[:, 0:36], rb, ALU.add)
                c = lambda i: sm[:, i:i + 1]
                V('dve', lambda: nc.vector.reduce_max(out=c(0), in_=lg[:, 0:4], axis=AX.X), [c(0)], [lg[:, 0:4]])
                ts_('dve', oh[:], lg[:, 0:4], c(0), None, ALU.is_ge)
                ts_('dve', c(1), c(0), -1.0, None, ALU.mult)
                act_(eg4[:], lg[:, 0:4], AF.Exp, bias=c(1))
                V('dve', lambda: nc.vector.reduce_sum(out=c(2), in_=eg4[:], axis=AX.X), [c(2)], [eg4[:]])
                V('dve', lambda: nc.vector.reciprocal(out=c(3), in_=c(2)), [c(3)], [c(2)])
                ts_('dve', pen[:], oh[:], -1.0, 30000.0, ALU.add, ALU.mult)
                tt_('dve', lem[:], lg[:, 4:36].rearrange("p (g e) -> p g e", g=4),
                    oh[:, :][:, :, None].broadcast_to([128, 4, 8]), ALU.mult)
                tt_('dve', lem[:], lem[:], pen[:, :][:, :, None].broadcast_to([128, 4, 8]), ALU.add)
                lemf = lem[:, :, :].rearrange("p g e -> p (g e)")
                V('dve', lambda: nc.vector.max(out=m8[:], in_=lemf), [m8[:]], [lemf])
                ts_('dve', eq1[:], lemf, m8[:, 0:1], None, ALU.is_ge)
                ts_('dve', eq2[:], lemf, m8[:, 1:2], None, ALU.is_ge)
                tt_('dve', c(4), m8[:, 1:2], m8[:, 0:1], ALU.subtract)
                act_(c(5), c(4), AF.Exp)
                ts_('dve', c(6), c(5), 1.0, None, ALU.add)
                V('dve', lambda: nc.vector.reciprocal(out=c(7), in_=c(6)), [c(7)], [c(6)])
                tt_('dve', c(8), c(3), c(7), ALU.mult)
                tt_('dve', c(9), c(8), c(5), ALU.mult)
                tt_('dve', c(10), c(8), c(9), ALU.subtract)
                ts_('dve', wt[:, t, :], eq2[:], c(9), None, ALU.mult)
                stt_(wt[:, t, :], eq1[:], c(10), wt[:, t, :], ALU.mult, ALU.add)
            for e in range(NEXP):
                b = e % 2
                kb.dma(wg[b][:], eg_d[l, e].rearrange("(kc p) f -> p kc f", p=128), f'mwg{b}', q='pool')
                kb.dma(wu[b][:], eu_d[l, e].rearrange("(kc p) f -> p kc f", p=128), f'mwu{b}', q='pool')
                for c0 in range(0, 1024, 512):
                    kb.dma(wd[b][:, :, c0:c0 + 512], ed_d[l, e].rearrange("(k p) c -> p k c", p=128)[:, :, c0:c0 + 512], f'mwd{b}', q='pool')
                n = 0
                for fcn in range(4):
                    fsl = slice(fcn * 128, (fcn + 1) * 128)
                    for tt in range(4):
                        tsl = slice(tt * 512, (tt + 1) * 512)
                        pg = P()
                        for kc in range(8):
                            mm(pg[:, :], wg[b][:, kc, fsl], hT[:, kc, tsl], kc == 0, kc == 7)
                        pu = P()
                        for kc in range(8):
                            mm(pu[:, :], wu[b][:, kc, fsl], hT[:, kc, tsl], kc == 0, kc == 7)
                        SG = sgt[n % 2]
                        n += 1
                        act_(SG[:], pg[:, :], AF.Silu)
                        tt_('dve', actb[b][:, fcn, tsl], SG[:], pu[:, :], ALU.mult)
                for t in range(16):
                    tk = slice(t * 128, (t + 1) * 128)
                    for half in range(2):
                        hsl = slice(half * 512, (half + 1) * 512)
                        pd = P()
                        for k in range(4):
                            mm(pd[:, :], actb[b][:, k, tk], wd[b][:, k, hsl], k == 0, k == 3)
                        if e == 0:
                            ts_('dve', yacc[:, t, hsl], pd[:, :], wt[:, t, e:e + 1], None, ALU.mult)
                        else:
                            stt_(yacc[:, t, hsl], pd[:, :], wt[:, t, e:e + 1], yacc[:, t, hsl], ALU.mult, ALU.add)
            for t in range(16):
                tk = slice(t * 128, (t + 1) * 128)
                Xt = X[t % 2]
                kb.dma(Xt[:], xs_d[:, :, tk], f'mX{t % 2}')
                for half in range(2):
                    p = P()
                    for j in range(4):
                        kc = half * 4 + j
                        tr(p[:, j * 128:(j + 1) * 128], yacc[:, t, kc * 128:(kc + 1) * 128], identf)
                    for j in range(4):
                        kc = half * 4 + j
                        stt_(Xt[:, kc, :], p[:, j * 128:(j + 1) * 128], cond[:, 40 + kc:41 + kc], Xt[:, kc, :], ALU.mult, ALU.add)
                kb.dma(xs_d[:, :, tk], Xt[:], f'mX{t % 2}')
            kb.barrier()

    def final_phase():
        with ExitStack() as es:
            A = lambda n, sh, dt: es.enter_context(nc.sbuf_tensor(n, sh, dt))
            xt = [A(f"fx{i}", [128, 8, 512], F32) for i in range(2)]
            sq = [A(f"fsq{i}", [128, 512], F32) for i in range(2)]
            rs = A("frs", [128, 512], F32)
            yf = A("fyf", [128, 8, 512], F32)
            ot = [A(f"fot{i}", [128, 1024], F32) for i in range(2)]
            fg = G('fg')
            n = 0
            for tt in range(4):
                Xt = xt[tt % 2]
                kb.dma(Xt[:], xs_d[:, :, tt * 512:(tt + 1) * 512], f'fx{tt % 2}')
                pss = P()
                for kc in range(8):
                    act_(sq[kc % 2][:], Xt[:, kc, :], AF.Square)
                    mm(pss[:, :], onesf, sq[kc % 2][:], kc == 0, kc == 7)
                act_(rs[:], pss[:, :], AF.Sqrt, bias=EPSC[:, 0:1], scale=1.0 / D)
                V('dve', lambda: nc.vector.reciprocal(out=rs[:], in_=rs[:]), [rs[:]], [rs[:]])
                for kc in range(8):
                    stt_(yf[:, kc, :], Xt[:, kc, :], fg[:, kc:kc + 1], rs[:], ALU.mult, ALU.mult)
                for t4 in range(4):
                    O = ot[n % 2]
                    n += 1
                    for half in range(2):
                        p = P()
                        for j in range(4):
                            kc = half * 4 + j
                            tr(p[:, j * 128:(j + 1) * 128], yf[:, kc, t4 * 128:(t4 + 1) * 128], identf)
                        copy_('act' if half else 'dve', O[:, half * 512:(half + 1) * 512], p[:, :])
                    r0 = tt * 512 + t4 * 128
                    kb.dma(out_d[r0:r0 + 128, :], O[:], f'fot{(n - 1) % 2}')
            kb.barrier()

    ph = phases
    load_x_phase()
    if ph is None or 'rope' in ph:
        rope_phase()
    for l in range(L):
        if ph is None or 'cond' in ph:
            cond_phase(l)
        if ph is None or 'norm1' in ph:
            norm_phase(l, 0, f"a{l}")
        if ph is None or 'attn' in ph:
            attn_phase(l)
        if ph is None or 'lru' in ph:
            lru_phase(l)
        if ph is None or 'ssd' in ph:
            ssd_proj_phase(l)
            ssd_scan_phase(l)
        if ph is None or 'merge' in ph:
            merge_phase(l)
        if ph is None or 'moe' in ph:
            norm_phase(l, 1, f"b{l}")
            moe_phase(l)
    if ph is None or 'final' in ph:
        final_phase()
    kb.finish()
    return nc, kb


_CACHE = {}


def _in_maps(inp, L, cores):
    pk = np.stack([_pack_layer(inp, l) for l in range(L)])
    maps = []
    shared = {
        "pk": pk,
        "ada_w": inp['ada_w'][:L], "w_in": inp['w_in'][:L],
        "lru_w_r": inp['lru_w_r'][:L], "lru_w_i": inp['lru_w_i'][:L],
        "w_br_attn": inp['w_br_attn'][:L], "w_br_ssd": inp['w_br_ssd'][:L], "w_br_lru": inp['w_br_lru'][:L],
        "w_out": inp['w_out'][:L], "router_wg": inp['router_wg'][:L],
        "router_we": inp['router_we'][:L].reshape(L, D, 32),
        "exp_w_gate": inp['exp_w_gate'][:L], "exp_w_up": inp['exp_w_up'][:L], "exp_w_down": inp['exp_w_down'][:L],
    }
    for b in cores:
        m = dict(shared)
        m["x"] = np.ascontiguousarray(inp['x'][b])
        m["pos"] = np.ascontiguousarray(np.broadcast_to(inp['positions'][b][None, :], (128, S))).astype(np.int32)
        m["gk"] = _pack_global(inp['c'][b], inp['final_g'])
        maps.append(m)
    return maps


def kernel(**inputs):
    inp = {k: np.asarray(v) for k, v in inputs.items()}
    if 'nc' not in _CACHE:
        _CACHE['nc'] = build(DEPTH)[0]
    nc = _CACHE['nc']
    maps = _in_maps(inp, DEPTH, list(range(8)))
    res = run_bass_kernel_spmd(nc, maps, core_ids=list(range(8)))
    out = np.stack([np.asarray(res.results[c]["out"]) for c in range(8)], axis=0)
    return out.astype(np.float32)
```

```python
import numpy as np
import concourse.bass as bass
import concourse.mybir as mybir
from concourse.bass_utils import run_bass_kernel_spmd

F32 = mybir.dt.float32
BF16 = mybir.dt.bfloat16
I32 = mybir.dt.int32
ALU = mybir.AluOpType
AF = mybir.ActivationFunctionType
AX = mybir.AxisListType

_DTSIZE = {F32: 4, BF16: 2, I32: 4}
SEM_ROT = 30000

D = 1024
S = 2048
DEPTH = 4
NEXP = 32
FF = 512
INC = 16416
EPS = 1e-6
OQ, OK_, OV, OZ, OXBC, ODT, OLG, OLX, OMG = 0, 1536, 3072, 4608, 6656, 10752, 10784, 12064, 13344


def _box(ap):
    t = ap.tensor
    es = _DTSIZE[ap.dtype]
    pat = ap.ap
    off = int(ap.offset)
    if type(t).__name__.startswith('DRam'):
        ext = 1
        for st, cnt in pat:
            ext += (cnt - 1) * abs(st)
        return (t.name, 0, 1, off * es, (off + ext) * es)
    if type(t).__name__.startswith('PSum'):
        return (t.name, 0, 128, 0, 2048)
    free = 1
    for s in list(t.shape)[1:]:
        free *= s
    p0 = off // free
    f0 = off % free
    np_ = pat[0][1] if pat[0][0] != 0 else 1
    ext = 1
    for st, cnt in pat[1:]:
        ext += (cnt - 1) * abs(st)
    return (t.name, p0, p0 + np_, f0 * es, (f0 + ext) * es)


class Chan:
    def __init__(self, kb, name):
        self.kb = kb
        self.name = name
        self.sem = kb.nc.alloc_semaphore(name)
        kb.chan_by_sem[self.sem.num] = self
        self.cnt = 0
        self.gen = 0

    def next_event(self):
        if self.cnt + 16 > SEM_ROT:
            self.gen += 1
            self.sem = self.kb.nc.alloc_semaphore(f"{self.name}_g{self.gen}")
            self.kb.chan_by_sem[self.sem.num] = self
            self.cnt = 0
        self.cnt += 16
        return (self.sem, self.cnt)


class KB:
    def __init__(self, nc):
        self.nc = nc
        self.engs = {'pe': nc.tensor, 'act': nc.scalar, 'dve': nc.vector, 'pool': nc.gpsimd, 'sp': nc.sync}
        self.sem, self.cnt, self.gen = {}, {}, {}
        for e in ('pe', 'act', 'dve', 'pool'):
            self.sem[e] = nc.alloc_semaphore(f"s_{e}")
            self.cnt[e] = 0
            self.gen[e] = 0
        self.oldsems = []
        self.seen = {e: {} for e in self.engs}
        self.recs = {}
        self.pending = {e: False for e in self.engs}
        self.ninst = {e: 0 for e in self.engs}
        self.chans = {}
        self.chan_by_sem = {}

    def chan(self, name):
        if name not in self.chans:
            self.chans[name] = Chan(self, name)
        return self.chans[name]

    def _deps(self, ins, outs):
        deps = []
        for ap in ins:
            b = _box(ap)
            for r in self.recs.get(b[0], ()):
                if r[5] and r[1] < b[2] and b[1] < r[2] and r[3] < b[4] and b[3] < r[4]:
                    deps.append(r[6])
        for ap in outs:
            b = _box(ap)
            for r in self.recs.get(b[0], ()):
                if r[1] < b[2] and b[1] < r[2] and r[3] < b[4] and b[3] < r[4]:
                    deps.append(r[6])
        return deps

    def _record(self, ins, outs, ev, tag):
        for ap in outs:
            b = _box(ap)
            lst = self.recs.setdefault(b[0], [])
            lst[:] = [r for r in lst if not (b[1] <= r[1] and r[2] <= b[2] and b[3] <= r[3] and r[4] <= b[4])]
            lst.append((b[0], b[1], b[2], b[3], b[4], True, ev, tag))
        for ap in ins:
            b = _box(ap)
            lst = self.recs.setdefault(b[0], [])
            lst[:] = [r for r in lst if not ((not r[5]) and r[7] == tag and r[1:5] == b[1:5])]
            lst.append((b[0], b[1], b[2], b[3], b[4], False, ev, tag))

    def _wait(self, e, deps, skip_self=False):
        eng = self.engs[e]
        seen = self.seen[e]
        best = {}
        for sem, val in deps:
            if skip_self and e in self.sem and sem is self.sem[e]:
                continue
            k = sem.num
            ch = self.chan_by_sem.get(k)
            if ch is not None and ch.sem.num == k:
                val = max(val, ch.cnt)
            if seen.get(k, 0) >= val:
                continue
            if k not in best or best[k][1] < val:
                best[k] = (sem, val)
        for k, (sem, val) in best.items():
            eng.wait_ge(sem, val)
            seen[k] = val

    def op(self, e, fn, outs, ins, signal=True):
        deps = self._deps(ins, outs)
        self._wait(e, deps, skip_self=(e == 'pe'))
        inst = fn()
        self.ninst[e] += 1
        if signal and self.cnt[e] + 1 > SEM_ROT:
            self.oldsems.append((self.sem[e], self.cnt[e]))
            self.gen[e] += 1
            self.sem[e] = self.nc.alloc_semaphore(f"s_{e}_g{self.gen[e]}")
            self.cnt[e] = 0
        if signal:
            self.cnt[e] += 1
            inst.then_inc(self.sem[e], 1)
            ev = (self.sem[e], self.cnt[e])
            self.pending[e] = False
        else:
            assert self.cnt[e] + 1 <= SEM_ROT
            ev = (self.sem[e], self.cnt[e] + 1)
            self.pending[e] = True
        self._record(ins, outs, ev, e)
        return inst

    def dma(self, out, in_, chan, q='sp', **kw):
        ch = self.chan(chan)
        deps = self._deps([in_], [out])
        self._wait(q, deps)
        ev = ch.next_event()
        inst = self.engs[q].dma_start(out=out, in_=in_, **kw)
        inst.then_inc(ev[0], 16)
        self.ninst[q] += 1
        self._record([in_], [out], ev, 'dma_' + ch.name)
        return inst

    def all_events(self):
        evs = [(self.sem[e], self.cnt[e]) for e in ('pe', 'act', 'dve', 'pool') if self.cnt[e] > 0]
        evs += [(ch.sem, ch.cnt) for ch in self.chans.values() if ch.cnt > 0]
        return evs

    def barrier(self, drop=()):
        for e in ('pe', 'act', 'dve', 'pool'):
            assert not self.pending[e], e
        evs = self.all_events()
        for e in self.engs:
            self._wait(e, evs)
        self.recs.clear()

    def finish(self):
        for e in ('pe', 'act', 'dve', 'pool'):
            assert not self.pending[e], e
        self._wait('sp', self.all_events())


PK_COLS = {}
_o = 0
for _n, _w in (('n1g', 8), ('n2g', 8), ('adab', 48), ('scw', 128), ('scb', 32), ('dtb', 32), ('alog', 32),
               ('sdd', 16), ('sng', 16), ('rb', 36), ('lcw', 64), ('lcb', 16), ('lbr', 16), ('lbi', 16), ('llam', 16)):
    PK_COLS[_n] = (_o, _w)
    _o += _w
NPK = _o
GK_COLS = {}
_o = 0
for _n, _w in (('c', 8), ('fg', 8), ('freq', 1), ('ident', 128), ('mle', 128), ('mgt', 128), ('mge', 128), ('ones', 128),
               ('phase', 1)):
    GK_COLS[_n] = (_o, _w)
    _o += _w
NGK = _o


def _fm(v, p=128):
    return np.ascontiguousarray(v.reshape(-1, p).T)


def _pack_layer(inp, l):
    pk = np.zeros((128, NPK), np.float32)

    def put(name, arr):
        o, w = PK_COLS[name]
        assert arr.shape[1] == w, (name, arr.shape)
        pk[:arr.shape[0], o:o + w] = arr
    put('n1g', _fm(inp['norm1_g'][l]))
    put('n2g', _fm(inp['norm2_g'][l]))
    put('adab', _fm(inp['ada_b'][l]))
    scw = inp['ssd_conv_w'][l]
    put('scw', np.ascontiguousarray(scw.T.reshape(32, 128, 4).transpose(1, 0, 2)).reshape(128, 128))
    put('scb', _fm(inp['ssd_conv_b'][l]))
    put('dtb', np.broadcast_to(inp['ssd_dt_bias'][l][None, :], (128, 32)))
    put('alog', np.broadcast_to(inp['ssd_a_log'][l][None, :], (128, 32)))
    put('sdd', _fm(np.repeat(inp['ssd_d'][l], 64)))
    put('sng', _fm(inp['ssd_norm_g'][l]))
    rb = np.concatenate([inp['router_bg'][l].reshape(-1), inp['router_be'][l].reshape(-1)])
    put('rb', np.broadcast_to(rb[None, :], (128, 36)))
    lcw = inp['lru_conv_w'][l]
    put('lcw', np.ascontiguousarray(lcw.T.reshape(16, 80, 4).transpose(1, 0, 2)).reshape(80, 64))
    put('lcb', _fm(inp['lru_conv_b'][l], 80))
    put('lbr', _fm(inp['lru_b_r'][l], 80))
    put('lbi', _fm(inp['lru_b_i'][l], 80))
    put('llam', _fm(inp['lru_lambda'][l], 80))
    return pk


def _pack_global(c_b, final_g):
    gk = np.zeros((128, NGK), np.float32)

    def put(name, arr):
        o, w = GK_COLS[name]
        gk[:, o:o + w] = arr
    put('c', _fm(c_b))
    put('fg', _fm(final_g))
    half = 32
    freqs = (10000.0 ** (-np.arange(half, dtype=np.float32) / half)).astype(np.float32)
    put('freq', np.tile(freqs, 4)[:, None])
    i = np.arange(128)
    put('ident', (i[:, None] == i[None, :]).astype(np.float32))
    put('mle', (i[:, None] <= i[None, :]).astype(np.float32))
    put('mgt', (i[:, None] > i[None, :]).astype(np.float32))
    put('mge', (i[:, None] >= i[None, :]).astype(np.float32))
    put('ones', np.ones((128, 128), np.float32))
    put('phase', np.where((i % 64) < 32, -1.0, 1.0).astype(np.float32)[:, None])
    return gk


from contextlib import ExitStack
import math


def build(L=DEPTH, dbg=False, phases=None):
    nc = bass.Bass("TRN2", target_bir_lowering=False)
    kb = KB(nc)

    def din(name, shape, dt=F32):
        return nc.dram_tensor(name, list(shape), dt, kind="ExternalInput").ap()

    def dscr(name, shape, dt):
        return nc.dram_tensor(name, list(shape), dt, kind=("ExternalOutput" if dbg else "Internal")).ap()

    x_d = din("x", [S, D])
    pos_d = din("pos", [128, S], I32)
    gk_d = din("gk", [128, NGK])
    pk_d = din("pk", [L, 128, NPK])
    adaw_d = din("ada_w", [L, D, 6 * D])
    win_d = din("w_in", [L, D, INC])
    lwr_d = din("lru_w_r", [L, 16, 80, 80])
    lwi_d = din("lru_w_i", [L, 16, 80, 80])
    wba_d = din("w_br_attn", [L, 512, D])
    wbs_d = din("w_br_ssd", [L, 2048, D])
    wbl_d = din("w_br_lru", [L, 1280, D])
    wo_d = din("w_out", [L, D, D])
    rwg_d = din("router_wg", [L, D, 4])
    rwe_d = din("router_we", [L, D, 32])
    eg_d = din("exp_w_gate", [L, NEXP, D, FF])
    eu_d = din("exp_w_up", [L, NEXP, D, FF])
    ed_d = din("exp_w_down", [L, NEXP, FF, D])
    out_d = nc.dram_tensor("out", [S, D], F32, kind="ExternalOutput").ap()

    xs_d = dscr("xs_scr", [128, 8, S], F32)
    ya_d = dscr("ya_scr", [64, 8, S], BF16)
    ys_d = dscr("ys_scr", [128, 16, S], BF16)
    yl_d = dscr("yl_scr", [80, 16, S], BF16)
    xbc_d = nc.dram_tensor("xbc_scr", [128, 32, S], BF16).ap()
    z_d = nc.dram_tensor("z_scr", [128, 16, S], BF16).ap()
    cs_d = nc.dram_tensor("cs_scr", [128, 2, S], F32).ap()

    ps = [nc.alloc_psum_tensor(f"ps{i}", [128, 512], F32) for i in range(8)]
    pctr = [0]

    def P():
        t = ps[pctr[0] % 8]
        pctr[0] += 1
        return t

    def mm(out, lhsT, rhs, start=True, stop=True):
        return kb.op('pe', lambda: nc.tensor.matmul(out, lhsT=lhsT, rhs=rhs, start=start, stop=stop),
                     [out], [lhsT, rhs], signal=True)

    def tr(out, in_, ident):
        return kb.op('pe', lambda: nc.tensor.transpose(out=out, in_=in_, identity=ident), [out], [in_, ident])

    def V(e, fn, outs, ins):
        return kb.op(e, fn, outs, ins)

    def tt_(e, out, in0, in1, op):
        eng = kb.engs[e]
        return kb.op(e, lambda: eng.tensor_tensor(out=out, in0=in0, in1=in1, op=op), [out], [in0, in1])

    def ts_(e, out, in0, s1, s2, op0, op1=ALU.bypass):
        eng = kb.engs[e]
        ins = [in0] + [s for s in (s1, s2) if not isinstance(s, (int, float)) and s is not None]
        if s2 is None:
            return kb.op(e, lambda: eng.tensor_scalar(out=out, in0=in0, scalar1=s1, scalar2=None, op0=op0), [out], ins)
        return kb.op(e, lambda: eng.tensor_scalar(out=out, in0=in0, scalar1=s1, scalar2=s2, op0=op0, op1=op1), [out], ins)

    def stt_(out, in0, scalar, in1, op0, op1):
        ins = [in0, in1] + ([] if isinstance(scalar, (int, float)) else [scalar])
        return kb.op('dve', lambda: nc.vector.scalar_tensor_tensor(out=out, in0=in0, scalar=scalar, in1=in1, op0=op0, op1=op1),
                     [out], ins)

    def act_(out, in_, func, bias=0.0, scale=1.0):
        ins = [in_] + [s for s in (bias, scale) if not isinstance(s, (int, float))]
        return kb.op('act', lambda: nc.scalar.activation(out=out, in_=in_, func=func, bias=bias, scale=scale), [out], ins)

    def copy_(e, out, in_):
        if e == 'act':
            return kb.op('act', lambda: nc.scalar.copy(out=out, in_=in_), [out], [in_])
        eng = kb.engs[e]
        return kb.op(e, lambda: eng.tensor_copy(out=out, in_=in_), [out], [in_])

    def memset_(e, ap, val):
        eng = kb.engs[e]
        return kb.op(e, lambda: eng.memset(ap, val), [ap], [])

    def winv(l):
        return win_d[l].rearrange("(kc p) c -> p kc c", p=128)

    gk = nc.alloc_sbuf_tensor("gk_sb", [128, NGK], F32)
    kb.dma(gk[:], gk_d[:, :], 'c0')

    def G(name):
        o, w = GK_COLS[name]
        return gk[:, o:o + w]
    identf, mle, mgt, mge, onesf = G('ident'), G('mle'), G('mgt'), G('mge'), G('ones')
    cb = nc.alloc_sbuf_tensor("cb", [128, 3, 128], BF16)
    for i, nm in enumerate(('ident', 'mle', 'mge')):
        copy_('dve', cb[:, i, :], G(nm))
    identb = cb[:, 0, :]
    hT = nc.alloc_sbuf_tensor("hT", [128, 8, S], BF16)
    pk = nc.alloc_sbuf_tensor("pk_sb", [128, NPK], F32)
    cond = nc.alloc_sbuf_tensor("cond", [128, 48], F32)
    gm = nc.alloc_sbuf_tensor("gm", [128, 16], F32)
    cact = nc.alloc_sbuf_tensor("cact", [128, 8], F32)
    dtraw = nc.alloc_sbuf_tensor("dtraw", [128, 16, 32], F32)
    act_(cact[:], G('c'), AF.Silu)

    def PK(name, rows=128):
        o, w = PK_COLS[name]
        return pk[0:rows, o:o + w]

    def load_x_phase():
        with ExitStack() as es:
            xin = [es.enter_context(nc.sbuf_tensor(f"xin{i}", [128, D], F32)) for i in range(2)]
            xo = [es.enter_context(nc.sbuf_tensor(f"xo{i}", [128, 8, 128], F32)) for i in range(2)]
            for t in range(16):
                xi = xin[t % 2]
                kb.dma(xi[:], x_d[t * 128:(t + 1) * 128, :], f'xin{t % 2}')
                for half in range(2):
                    p = P()
                    for j in range(4):
                        kc = half * 4 + j
                        tr(p[:, j * 128:(j + 1) * 128], xi[:, kc * 128:(kc + 1) * 128], identf)
                    copy_('dve' if half == 0 else 'act', xo[t % 2][:, half * 4:(half + 1) * 4, :],
                          p[:, :].rearrange("p (j t) -> p j t", j=4))
                kb.dma(xs_d[:, :, t * 128:(t + 1) * 128], xo[t % 2][:], f'xo{t % 2}')
            kb.barrier()

    def rope_phase():
        with ExitStack() as es:
            posi = es.enter_context(nc.sbuf_tensor("posi", [128, S], I32))
            ki = es.enter_context(nc.sbuf_tensor("rki", [128, S], I32))
            ang = es.enter_context(nc.sbuf_tensor("ang", [128, S], F32))
            u = es.enter_context(nc.sbuf_tensor("ru", [128, S], F32))
            kf = es.enter_context(nc.sbuf_tensor("rkf", [128, S], F32))
            r = es.enter_context(nc.sbuf_tensor("rr", [128, S], F32))
            m = es.enter_context(nc.sbuf_tensor("rm", [128, S], F32))
            kb.dma(posi[:], pos_d[:, :], 'c0')
            copy_('dve', ang[:], posi[:])
            ts_('dve', ang[:], ang[:], G('freq'), None, ALU.mult)
            TWO_PI = 2.0 * math.pi
            C1 = 6.28125
            C2 = TWO_PI - C1
            for which, phi in ((0, math.pi / 2), (1, 0.0)):
                ts_('dve', u[:], ang[:], 1.0 / TWO_PI, phi / TWO_PI + 0.5, ALU.mult, ALU.add)
                copy_('dve', ki[:], u[:])
                copy_('dve', kf[:], ki[:])
                stt_(r[:], kf[:], -C1, ang[:], ALU.mult, ALU.add)
                stt_(r[:], kf[:], -C2, r[:], ALU.mult, ALU.add)
                if phi != 0.0:
                    ts_('dve', r[:], r[:], phi, None, ALU.add)
                ts_('dve', m[:], r[:], -math.pi, None, ALU.is_lt)
                stt_(r[:], m[:], TWO_PI, r[:], ALU.mult, ALU.add)
                ts_('dve', m[:], r[:], math.pi, None, ALU.is_gt)
                stt_(r[:], m[:], -TWO_PI, r[:], ALU.mult, ALU.add)
                ts_('dve', r[:], r[:], -3.1415925, 3.1415925, ALU.max, ALU.min)
                act_(u[:], r[:], AF.Sin)
                if which == 1:
                    ts_('dve', u[:], u[:], G('phase'), None, ALU.mult)
                kb.dma(cs_d[:, which, :], u[:], 'cs_st')
            kb.barrier()

    def cond_phase(l):
        with ExitStack() as es:
            aw = [es.enter_context(nc.sbuf_tensor(f"aw{l}_{i}", [128, 8, 512], F32)) for i in range(2)]
            kb.dma(pk[:], pk_d[l], 'pk')
            pc = P()
            src = adaw_d[l].rearrange("(kc p) c -> p kc c", p=128)
            for cc in range(12):
                w = aw[cc % 2]
                kb.dma(w[:], src[:, :, cc * 512:(cc + 1) * 512], f'aw{cc % 2}')
                for j in range(4):
                    col = cc * 4 + j
                    for kc in range(8):
                        mm(pc[:, col:col + 1], w[:, kc, j * 128:(j + 1) * 128], cact[:, kc:kc + 1], kc == 0, kc == 7)
            tt_('dve', cond[:], pc[:, 0:48], PK('adab'), ALU.add)
            stt_(gm[:, 0:8], cond[:, 8:16], 1.0, PK('n1g'), ALU.add, ALU.mult)
            stt_(gm[:, 8:16], cond[:, 32:40], 1.0, PK('n2g'), ALU.add, ALU.mult)
            kb.barrier()

    def norm_phase(l, which, tag):
        with ExitStack() as es:
            xt = [es.enter_context(nc.sbuf_tensor(f"nx{tag}_{i}", [128, 8, 512], F32)) for i in range(2)]
            sq = [es.enter_context(nc.sbuf_tensor(f"nsq{tag}_{i}", [128, 512], F32)) for i in range(2)]
            rs = [es.enter_context(nc.sbuf_tensor(f"nrs{tag}_{i}", [128, 512], F32)) for i in range(2)]
            t2 = [es.enter_context(nc.sbuf_tensor(f"nt2{tag}_{i}", [128, 512], F32)) for i in range(2)]
            sh0 = 0 if which == 0 else 24
            for tt in range(4):
                X = xt[tt % 2]
                kb.dma(X[:], xs_d[:, :, tt * 512:(tt + 1) * 512], f'nx{tt % 2}')
                pss = P()
                for kc in range(8):
                    act_(sq[kc % 2][:], X[:, kc, :], AF.Square)
                    mm(pss[:, :], onesf, sq[kc % 2][:], kc == 0, kc == 7)
                R_ = rs[tt % 2]
                act_(R_[:], pss[:, :], AF.Sqrt, bias=EPSC[:, 0:1], scale=1.0 / D)
                V('dve', lambda: nc.vector.reciprocal(out=R_[:], in_=R_[:]), [R_[:]], [R_[:]])
                for kc in range(8):
                    T = t2[kc % 2]
                    tt_('dve', T[:], X[:, kc, :], R_[:], ALU.mult)
                    ts_('pool', hT[:, kc, tt * 512:(tt + 1) * 512], T[:], gm[:, which * 8 + kc:which * 8 + kc + 1],
                        cond[:, sh0 + kc:sh0 + kc + 1], ALU.mult, ALU.add)
            kb.barrier()

    epsc = nc.alloc_sbuf_tensor("epsc", [128, 2], F32)
    EPSC = epsc
    memset_('dve', epsc[:, 0:1], EPS)
    memset_('dve', epsc[:, 1:2], 1.0)

    def attn_tok(g, ti):
        if g == 0:
            return slice(128 * ti, 128 * ti + 128, 1)
        if g == 1:
            st = 512 * (ti // 4) + (ti % 4)
            return slice(st, st + 4 * 127 + 1, 4)
        return slice(ti, ti + 16 * 127 + 1, 16)

    def attn_prev(g, ti):
        if g == 0:
            return ti - 1 if ti >= 1 else None
        if g == 1:
            return ti - 4 if ti >= 4 else None
        return None

    def attn_phase(l):
        with ExitStack() as es:
            A = lambda n, sh, dt: es.enter_context(nc.sbuf_tensor(f"{n}_{l}", sh, dt))
            cs = A("acs", [128, 2, S], F32)
            qk = A("aqk", [128, 2, 2, S], BF16)
            vaug = A("avaug", [128, 16, 4, 128], BF16)
            acc = A("aacc", [128, 4, S], F32)
            wq = [A(f"awq{i}", [128, 8, 256], BF16) for i in range(3)]
            wst = [A(f"awst{i}", [128, 8, 256], F32) for i in range(2)]
            ta = [A(f"ata{i}", [128, 512], F32) for i in range(3)]
            tb = [A(f"atb{i}", [128, 512], F32) for i in range(3)]
            nrope = 0
            NPT = 6
            pt = [A(f"apt{i}", [128, 512], BF16) for i in range(NPT)]
            ptm = [A(f"aptm{i}", [128, 4, 128], BF16) for i in range(NPT)]
            rd = A("ard", [64, 4, 512], F32)
            yo = [A(f"ayo{i}", [64, 4, 512], BF16) for i in range(2)]
            kb.dma(cs[:], cs_d[:, :, :], 'acs')
            memset_('pool', vaug[:, :, :, 64:128], 1.0)
            wi = 0
            pi = 0
            for hh in range(2):
                for g in range(3):
                    for which, off in ((0, OQ), (1, OK_)):
                        c0 = off + g * 512 + hh * 256
                        W = wq[wi % 3]
                        kb.dma(wst[wi % 2][:], winv(l)[:, :, c0:c0 + 256], f'awst{wi % 2}')
                        copy_('pool', W[:], wst[wi % 2][:])
                        wi += 1
                        for j in range(2):
                            for tt in range(4):
                                tsl = slice(tt * 512, (tt + 1) * 512)
                                p = P()
                                for kc in range(8):
                                    mm(p[:, :], W[:, kc, j * 128:(j + 1) * 128], hT[:, kc, tsl], kc == 0, kc == 7)
                                TA, TB = ta[tt % 2], tb[tt % 2]
                                tt_('dve', TA[:], p[:, :], cs[:, 0, tsl], ALU.mult)
                                for q4 in range(4):
                                    o0 = q4 * 32
                                    i0 = o0 + 32 if q4 % 2 == 0 else o0 - 32
                                    tt_('dve', TB[o0:o0 + 32, :], p[i0:i0 + 32, :], cs[o0:o0 + 32, 1, tsl], ALU.mult)
                                tt_('pool', qk[:, which, j, tsl], TA[:], TB[:], ALU.add)
                    c0 = OV + g * 512 + hh * 256
                    W = wq[wi % 3]
                    kb.dma(wst[wi % 2][:], winv(l)[:, :, c0:c0 + 256], f'awst{wi % 2}')
                    copy_('pool', W[:], wst[wi % 2][:])
                    wi += 1
                    for ti in range(16):
                        tok = attn_tok(g, ti)
                        p = P()
                        for kc in range(8):
                            mm(p[:, 0:256], hT[:, kc, tok], W[:, kc, :], kc == 0, kc == 7)
                        copy_('act', vaug[:, ti, :, 0:64], p[:, 0:256].rearrange("p (h d) -> p h d", h=4))
                    def stage_s(ti):
                        nonlocal pi
                        tq = attn_tok(g, ti)
                        pv = attn_prev(g, ti)
                        kts = ([(pv, 2)] if pv is not None else []) + [(ti, 1)]
                        pms = []
                        for kt, mk in kts:
                            tk = attn_tok(g, kt)
                            PT, PM = pt[pi % NPT], ptm[pi % NPT]
                            pi += 1
                            PT3 = PT[:, :].rearrange("p (h q) -> p h q", h=4)
                            for par in range(2):
                                p = P()
                                bp = par * 64
                                for j in range(2):
                                    mm(p[:, j * 128:(j + 1) * 128], qk[bp:bp + 64, 1, j, tk], qk[bp:bp + 64, 0, j, tq])
                                act_(PT3[:, par::2, :], p[:, 0:256].rearrange("p (j q) -> p j q", j=2), AF.Exp, scale=0.125)
                            tt_('dve', PM[:], PT3, cb[:, mk, :][:, None, :].broadcast_to([128, 4, 128]), ALU.mult)
                            pms.append((kt, PM))
                        return pms

                    def stage_pv(ti, pms):
                        tq = attn_tok(g, ti)
                        po = P()
                        for h in range(4):
                            for idx, (kt, PM) in enumerate(pms):
                                mm(po[:, h * 128:(h + 1) * 128], vaug[:, kt, h, :], PM[:, h, :], idx == 0, idx == len(pms) - 1)
                        pov = po[:, :].rearrange("p (h q) -> p h q", h=4)
                        if g == 0:
                            copy_('dve', acc[:, :, tq], pov)
                        else:
                            tt_('dve', acc[:, :, tq], pov, acc[:, :, tq], ALU.add)

                    prev = None
                    for ti in range(16):
                        cur = (ti, stage_s(ti))
                        if prev is not None:
                            stage_pv(*prev)
                        prev = cur
                    stage_pv(*prev)
                for tt in range(4):
                    tsl = slice(tt * 512, (tt + 1) * 512)
                    V('dve', lambda: nc.vector.reciprocal(out=rd[:], in_=acc[64:128, :, tsl]), [rd[:]], [acc[64:128, :, tsl]])
                    Y = yo[tt % 2]
                    tt_('dve', Y[:], acc[0:64, :, tsl], rd[:], ALU.mult)
                    kb.dma(ya_d[:, hh * 4:(hh + 1) * 4, tsl], Y[:], f'ayo{tt % 2}')
            kb.barrier()

    def lru_phase(l):
        with ExitStack() as es:
            A = lambda n, sh, dt: es.enter_context(nc.sbuf_tensor(f"{n}_{l}", sh, dt))
            wlx = A("lwlx", [128, 8, 1280], BF16)
            wlg = A("lwlg", [128, 8, 1280], BF16)
            wr = A("lwr", [80, 16, 80], BF16)
            wi_ = A("lwi", [80, 16, 80], BF16)
            cneg = A("lcneg", [80, 16], F32)
            c2 = A("lc2", [80, 16], F32)
            xpre_ = [A(f"lxpre{i}", [80, 3 + S], F32) for i in range(2)]
            xc_ = [A(f"lxc{i}", [80, S], F32) for i in range(2)]
            xcb_ = [A(f"lxcb{i}", [80, S], BF16) for i in range(2)]
            rg_ = [A(f"lrg{i}", [80, S], F32) for i in range(2)]
            ig_ = [A(f"lig{i}", [80, S], F32) for i in range(2)]
            Aa = A("laa", [80, S], F32)
            T1 = A("lt1", [80, S], F32)
            uu = A("luu", [80, S], F32)
            hs = A("lhs", [80, S], F32)
            gl = A("lgl", [80, S], F32)
            yo = [A(f"lyo{i}", [80, S], BF16) for i in range(2)]
            for (w, off, nm) in ((wlx, OLX, 'lwlx'), (wlg, OLG, 'lwlg')):
                for c0 in range(0, 1280, 320):
                    kb.dma(w[:, :, c0:c0 + 320], winv(l)[:, :, off + c0:off + c0 + 320], nm, q='pool')
            kb.dma(wr[:], lwr_d[l].rearrange("k i j -> i k j"), 'lwr', q='pool')
            kb.dma(wi_[:], lwi_d[l].rearrange("k i j -> i k j"), 'lwi', q='pool')
            lcw = PK('lcw', 80)
            lcb, lbr, lbi, llam = PK('lcb', 80), PK('lbr', 80), PK('lbi', 80), PK('llam', 80)
            act_(cneg[:], llam, AF.Exp, scale=-1.0)
            act_(cneg[:], cneg[:], AF.Ln, bias=EPSC[0:80, 1:2])
            ts_('dve', c2[:], cneg[:], -16.0, None, ALU.mult)
            ts_('dve', cneg[:], cneg[:], -8.0, None, ALU.mult)
            for i in range(2):
                memset_('dve', xpre_[i][:, 0:3], 0.0)
            def lru_s1(k):
                ksl = slice(k * 80, (k + 1) * 80)
                xpre, xc, xcb, rg, ig = xpre_[k % 2], xc_[k % 2], xcb_[k % 2], rg_[k % 2], ig_[k % 2]
                for tt in range(4):
                    tsl = slice(tt * 512, (tt + 1) * 512)
                    p = P()
                    for kc in range(8):
                        mm(p[0:80, :], wlx[:, kc, ksl], hT[:, kc, tsl], kc == 0, kc == 7)
                    copy_('act', xpre[:, 3 + tt * 512:3 + (tt + 1) * 512], p[0:80, :])
                ts_('dve', xc[:], xpre[:, 0:S], lcw[:, k * 4:k * 4 + 1], lcb[:, k:k + 1], ALU.mult, ALU.add)
                for j in range(1, 4):
                    stt_(xc[:], xpre[:, j:j + S], lcw[:, k * 4 + j:k * 4 + j + 1], xc[:], ALU.mult, ALU.add)
                copy_('pool', xcb[:], xc[:])
                for (wg, bb, dst) in ((wr, lbr, rg), (wi_, lbi, ig)):
                    for tt in range(4):
                        tsl = slice(tt * 512, (tt + 1) * 512)
                        p = P()
                        mm(p[0:80, :], wg[:, k, :], xcb[:, tsl])
                        act_(dst[:, tsl], p[0:80, :], AF.Sigmoid, bias=bb[:, k:k + 1])

            def lru_s2(k):
                ksl = slice(k * 80, (k + 1) * 80)
                xpre, xc, xcb, rg, ig = xpre_[k % 2], xc_[k % 2], xcb_[k % 2], rg_[k % 2], ig_[k % 2]
                act_(Aa[:], rg[:], AF.Exp, scale=cneg[:, k:k + 1])
                act_(T1[:], rg[:], AF.Exp, scale=c2[:, k:k + 1])
                ts_('dve', T1[:], T1[:], -1.0, 1.0, ALU.mult, ALU.add)
                ts_('dve', T1[:], T1[:], 1e-12, None, ALU.max)
                act_(T1[:], T1[:], AF.Sqrt)
                tt_('pool', uu[:], ig[:], xc[:], ALU.mult)
                tt_('dve', uu[:], uu[:], T1[:], ALU.mult)
                V('dve', lambda: nc.vector.tensor_tensor_scan(out=hs[:], data0=Aa[:], data1=uu[:], initial=0.0,
                                                              op0=ALU.mult, op1=ALU.add), [hs[:]], [Aa[:], uu[:]])
                for tt in range(4):
                    tsl = slice(tt * 512, (tt + 1) * 512)
                    p = P()
                    for kc in range(8):
                        mm(p[0:80, :], wlg[:, kc, ksl], hT[:, kc, tsl], kc == 0, kc == 7)
                    act_(gl[:, tsl], p[0:80, :], AF.Gelu)
                Y = yo[k % 2]
                tt_('dve', Y[:], hs[:], gl[:], ALU.mult)
                kb.dma(yl_d[:, k, :], Y[:], f'lyo{k % 2}')

            lru_s1(0)
            for k in range(16):
                if k + 1 < 16:
                    lru_s1(k + 1)
                lru_s2(k)
            kb.barrier()

    def ssd_proj_phase(l):
        with ExitStack() as es:
            A = lambda n, sh, dt: es.enter_context(nc.sbuf_tensor(f"{n}_{l}", sh, dt))
            W = [A(f"sw{i}", [128, 8, 512], BF16) for i in range(2)]
            wdt = A("swdt", [128, 8, 32], BF16)
            pre = [A(f"spre{i}", [128, 3 + S], F32) for i in range(2)]
            cv = [A(f"scv{i}", [128, S], F32) for i in range(2)]
            ob = [A(f"sob{i}", [128, S], BF16) for i in range(2)]
            scw, scb = PK('scw'), PK('scb')
            for i in range(2):
                memset_('dve', pre[i][:, 0:3], 0.0)
            n = 0
            for cg in range(8):
                Wt = W[cg % 2]
                kb.dma(Wt[:], winv(l)[:, :, OXBC + cg * 512:OXBC + (cg + 1) * 512], f'sw{cg % 2}', q='pool')
                for j in range(4):
                    fc = cg * 4 + j
                    PR, CV, OB = pre[n % 2], cv[n % 2], ob[n % 2]
                    n += 1
                    for tt in range(4):
                        p = P()
                        for kc in range(8):
                            mm(p[:, :], Wt[:, kc, j * 128:(j + 1) * 128], hT[:, kc, tt * 512:(tt + 1) * 512], kc == 0, kc == 7)
                        copy_('act', PR[:, 3 + tt * 512:3 + (tt + 1) * 512], p[:, :])
                    ts_('dve', CV[:], PR[:, 0:S], scw[:, fc * 4:fc * 4 + 1], None, ALU.mult)
                    for jj in range(1, 4):
                        stt_(CV[:], PR[:, jj:jj + S], scw[:, fc * 4 + jj:fc * 4 + jj + 1], CV[:], ALU.mult, ALU.add)
                    act_(OB[:], CV[:], AF.Silu, bias=scb[:, fc:fc + 1])
                    kb.dma(xbc_d[:, fc, :], OB[:], f'sob{(n - 1) % 2}')
            for cg in range(4):
                Wt = W[cg % 2]
                kb.dma(Wt[:], winv(l)[:, :, OZ + cg * 512:OZ + (cg + 1) * 512], f'sw{cg % 2}', q='pool')
                for j in range(4):
                    fc = cg * 4 + j
                    OB = ob[n % 2]
                    n += 1
                    for tt in range(4):
                        p = P()
                        for kc in range(8):
                            mm(p[:, :], Wt[:, kc, j * 128:(j + 1) * 128], hT[:, kc, tt * 512:(tt + 1) * 512], kc == 0, kc == 7)
                        act_(OB[:, tt * 512:(tt + 1) * 512], p[:, :], AF.Silu)
                    kb.dma(z_d[:, fc, :], OB[:], f'sob{(n - 1) % 2}')
            kb.dma(wdt[:], winv(l)[:, :, ODT:ODT + 32], 'swdt', q='pool')
            for c in range(16):
                p = P()
                for kc in range(8):
                    mm(p[:, 0:32], hT[:, kc, c * 128:(c + 1) * 128], wdt[:, kc, :], kc == 0, kc == 7)
                tt_('dve', dtraw[:, c, :], p[:, 0:32], PK('dtb'), ALU.add)
            kb.barrier()

    def ssd_scan_phase(l):
        with ExitStack() as es:
            A = lambda n, sh, dt: es.enter_context(nc.sbuf_tensor(f"{n}_{l}", sh, dt))
            H = A("cH", [128, 32, 64], F32)
            Hb = A("cHb", [128, 32, 64], BF16)
            expA = A("cexpA", [128, 32], F32)
            xsT = [A(f"cxs{i}", [128, 16, 128], BF16) for i in range(2)]
            BT = [A(f"cbt{i}", [128, 8, 128], BF16) for i in range(2)]
            CT = [A(f"cct{i}", [128, 8, 128], BF16) for i in range(2)]
            zT = [A(f"czt{i}", [128, 16, 128], BF16) for i in range(2)]
            dt_2 = [A(f"cdt{i}", [128, 32], F32) for i in range(2)]
            dtw2 = [A(f"cdtw{i}", [128, 32], F32) for i in range(2)]
            a_2 = [A(f"ca{i}", [128, 32], F32) for i in range(2)]
            ex2 = [A(f"cex{i}", [128, 3, 32], F32) for i in range(2)]
            abig2 = [A(f"cabig{i}", [128, 32, 64], F32) for i in range(2)]
            xdt2 = [A(f"cxdt{i}", [128, 32, 64], BF16) for i in range(2)]
            xdtw2 = [A(f"cxdtw{i}", [128, 32, 64], BF16) for i in range(2)]
            Btok2 = [A(f"cbtok{i}", [128, 8, 128], BF16) for i in range(2)]
            ab = [A(f"cab{i}", [128, 4, 128], F32) for i in range(2)]
            CBm = [A(f"ccbm{i}", [128, 128], F32) for i in range(2)]
            Lm = [A(f"clm{i}", [128, 512], F32) for i in range(2)]
            Gm = [A(f"cgm{i}", [128, 4, 128], BF16) for i in range(2)]
            ds = [A(f"cds{i}", [128, 256], F32) for i in range(2)]
            t1 = [A(f"ct1{i}", [128, 256], F32) for i in range(2)]
            yT = A("cyT", [128, 16, 128], F32)
            yg = A("cyg", [128, 16, 128], F32)
            sq = A("csq", [128, 16, 128], F32)
            rs = A("crs", [128, 128], F32)
            yo = [A(f"cyo{i}", [128, 16, 128], BF16) for i in range(2)]
            sdd, sng = PK('sdd'), PK('sng')
            memset_('dve', H[:], 0.0)
            memset_('pool', Hb[:], 0.0)
            act_(expA[:], PK('alog'), AF.Exp)
            def chunk_head(c):
                csl = slice(c * 128, (c + 1) * 128)
                b = c % 2
                dt_, dtw, a_, ex, abig, xdt, xdtw, Btok = dt_2[b], dtw2[b], a_2[b], ex2[b], abig2[b], xdt2[b], xdtw2[b], Btok2[b]
                kb.dma(xsT[b][:], xbc_d[:, 0:16, csl], f'cxs{b}')
                kb.dma(BT[b][:], xbc_d[:, 16:24, csl], f'cbt{b}')
                kb.dma(CT[b][:], xbc_d[:, 24:32, csl], f'cct{b}')
                kb.dma(zT[b][:], z_d[:, :, csl], f'czt{b}')
                act_(dt_[:], dtraw[:, c, :], AF.Exp)
                act_(dt_[:], dt_[:], AF.Ln, bias=EPSC[:, 1:2])
                stt_(a_[:], dt_[:], -1.0, expA[:], ALU.mult, ALU.mult)
                p3 = P()
                mm(p3[:, 0:32], mle, a_[:])
                mm(p3[:, 32:64], mgt, a_[:])
                mm(p3[:, 64:96], onesf, a_[:])
                act_(ex[:], p3[:, 0:96].rearrange("p (k h) -> p k h", k=3), AF.Exp)
                tt_('dve', dtw[:], dt_[:], ex[:, 1, :], ALU.mult)
                copy_('pool', abig[:], a_[:, :][:, :, None].broadcast_to([128, 32, 64]))
                for bb in range(2):
                    pT = P()[:, :].bitcast(BF16)
                    for j in range(8):
                        tr(pT[:, j * 128:(j + 1) * 128], xsT[b][:, bb * 8 + j, :], identb)
                    pv = pT.rearrange("p (h d) -> p h d", h=16)
                    hsl = slice(bb * 16, (bb + 1) * 16)
                    tt_('dve', xdt[:, hsl, :], pv, dt_[:, hsl][:, :, None].broadcast_to([128, 16, 64]), ALU.mult)
                    tt_('dve', xdtw[:, hsl, :], pv, dtw[:, hsl][:, :, None].broadcast_to([128, 16, 64]), ALU.mult)
                pB = P()[:, :].bitcast(BF16)
                for g in range(8):
                    tr(pB[:, g * 128:(g + 1) * 128], BT[b][:, g, :], identb)
                copy_('act', Btok[:], pB.rearrange("p (g n) -> p g n", g=8))
            def chunk_body(c):
                csl = slice(c * 128, (c + 1) * 128)
                b = c % 2
                dt_, dtw, a_, ex, abig, xdt, xdtw, Btok = dt_2[b], dtw2[b], a_2[b], ex2[b], abig2[b], xdt2[b], xdtw2[b], Btok2[b]
                def stage_a(g):
                    i2 = g % 2
                    pcb = P()
                    mm(pcb[:, 0:128], BT[b][:, g, :], CT[b][:, g, :])
                    tt_('dve', CBm[i2][:], pcb[:, 0:128], mle, ALU.mult)
                    tt_('pool', ab[i2][:], mgt[:, None, :].broadcast_to([128, 4, 128]),
                        a_[:, 4 * g:4 * g + 4][:, :, None].broadcast_to([128, 4, 128]), ALU.mult)
                    pD = P()
                    for h in range(4):
                        mm(pD[:, h * 128:(h + 1) * 128], ab[i2][:, h, :], mle)
                    act_(Lm[i2][:], pD[:, :], AF.Exp)
                    tt_('dve', Gm[i2][:], Lm[i2][:, :].rearrange("p (h l) -> p h l", h=4),
                        CBm[i2][:, None, :].broadcast_to([128, 4, 128]), ALU.mult)
                    pS = P()
                    for hp in range(2):
                        h0 = 4 * g + 2 * hp
                        mm(pS[:, hp * 128:(hp + 1) * 128], abig[:, h0:h0 + 2, :].rearrange("p h d -> p (h d)"), mle)
                    act_(ds[i2][:], pS[:, 0:256], AF.Exp)

                def stage_b(g):
                    i2 = g % 2
                    pY = P()
                    for hp in range(2):
                        for hh in range(2):
                            h = 2 * hp + hh
                            mm(pY[hh * 64:(hh + 1) * 64, hp * 128:(hp + 1) * 128], xdt[:, 4 * g + h, :], Gm[i2][:, h, :])
                    pZ = P()
                    for hp in range(2):
                        h0 = 4 * g + 2 * hp
                        mm(pZ[:, hp * 128:(hp + 1) * 128], Hb[:, h0:h0 + 2, :].rearrange("p h d -> p (h d)"), CT[b][:, g, :])
                    tt_('dve', t1[i2][:], pZ[:, 0:256], ds[i2][:], ALU.mult)
                    tt_('dve', yT[:, 2 * g:2 * g + 2, :], pY[:, 0:256].rearrange("p (f l) -> p f l", f=2),
                        t1[i2][:, :].rearrange("p (f l) -> p f l", f=2), ALU.add)

                stage_a(0)
                for g in range(8):
                    if g + 1 < 8:
                        stage_a(g + 1)
                    stage_b(g)
                for fc in range(16):
                    stt_(yT[:, fc, :], xsT[b][:, fc, :], sdd[:, fc:fc + 1], yT[:, fc, :], ALU.mult, ALU.add)
                tt_('dve', H[:], H[:], ex[:, 2, :][:, :, None].broadcast_to([128, 32, 64]), ALU.mult)
                for gp in range(4):
                    pst = P()
                    for gg in range(2):
                        g = 2 * gp + gg
                        mm(pst[:, gg * 256:(gg + 1) * 256], Btok[:, g, :],
                           xdtw[:, 4 * g:4 * g + 4, :].rearrange("p h d -> p (h d)"))
                    tt_('dve', H[:, 8 * gp:8 * gp + 8, :], pst[:, :].rearrange("p (h d) -> p h d", h=8),
                        H[:, 8 * gp:8 * gp + 8, :], ALU.add)
                copy_('pool', Hb[:], H[:])
                tt_('dve', yg[:], yT[:], zT[b][:], ALU.mult)
                act_(sq[:], yg[:], AF.Square)
                pss = P()
                for fc in range(16):
                    mm(pss[:, 0:128], onesf, sq[:, fc, :], fc == 0, fc == 15)
                act_(rs[:], pss[:, 0:128], AF.Sqrt, bias=EPSC[:, 0:1], scale=1.0 / 2048)
                V('dve', lambda: nc.vector.reciprocal(out=rs[:], in_=rs[:]), [rs[:]], [rs[:]])
                tt_('dve', yg[:], yg[:], rs[:, None, :].broadcast_to([128, 16, 128]), ALU.mult)
                tt_('pool', yo[b][:], yg[:], sng[:, :, None].broadcast_to([128, 16, 128]), ALU.mult)
                kb.dma(ys_d[:, :, csl], yo[b][:], f'cyo{b}')
            chunk_head(0)
            for c in range(16):
                if c + 1 < 16:
                    chunk_head(c + 1)
                chunk_body(c)
            kb.barrier()

    def merge_phase(l):
        with ExitStack() as es0:
            mT = es0.enter_context(nc.sbuf_tensor(f"gmT_{l}", [128, 8, S], BF16))
            with ExitStack() as es:
                A = lambda n, sh, dt: es.enter_context(nc.sbuf_tensor(f"{n}_{l}", sh, dt))
                wa = [A(f"gwa{i}", [64, 8, 256], BF16) for i in range(1)]
                ws = [A(f"gws{i}", [128, 16, 256], BF16) for i in range(1)]
                wl = [A(f"gwl{i}", [80, 16, 256], BF16) for i in range(1)]
                wm = [A(f"gwm{i}", [128, 3, 8, 256], BF16) for i in range(1)]
                ya = [A(f"gya{i}", [64, 8, 512], BF16) for i in range(2)]
                ys = [A(f"gys{i}", [128, 16, 512], BF16) for i in range(2)]
                yl = [A(f"gyl{i}", [80, 16, 512], BF16) for i in range(2)]
                sgt = [A(f"gsg{i}", [128, 512], F32) for i in range(2)]
                mac = [A(f"gmac{i}", [128, 512], F32) for i in range(2)]
                tmp = [A(f"gtmp{i}", [128, 512], F32) for i in range(2)]

                def load_q(qt):
                    i = 0
                    qsl = slice(qt * 256, (qt + 1) * 256)
                    kb.dma(wa[i][:], wba_d[l].rearrange("(h d) c -> d h c", d=64)[:, :, qsl], f'gwa{i}', q='pool')
                    kb.dma(ws[i][:], wbs_d[l].rearrange("(k p) c -> p k c", p=128)[:, :, qsl], f'gws{i}', q='pool')
                    kb.dma(wl[i][:], wbl_d[l].rearrange("(k p) c -> p k c", p=80)[:, :, qsl], f'gwl{i}', q='pool')
                    for b in range(3):
                        c0 = OMG + b * 1024 + qt * 256
                        kb.dma(wm[i][:, b, :, :], winv(l)[:, :, c0:c0 + 256], f'gwm{i}', q='pool')
                n = 0
                ny = 0
                def load_y(nn):
                    yi_ = nn % 2
                    tsl_ = slice((nn % 4) * 512, (nn % 4 + 1) * 512)
                    kb.dma(ya[yi_][:], ya_d[:, :, tsl_], f'gya{yi_}')
                    kb.dma(ys[yi_][:], ys_d[:, :, tsl_], f'gys{yi_}')
                    kb.dma(yl[yi_][:], yl_d[:, :, tsl_], f'gyl{yi_}')
                load_y(0)
                for qt in range(4):
                    load_q(qt)
                    wi_ = 0
                    for tt in range(4):
                        tsl = slice(tt * 512, (tt + 1) * 512)
                        yi = ny % 2
                        ny += 1
                        if ny < 16:
                            load_y(ny)
                        for oc in range(2):
                            osl = slice(oc * 128, (oc + 1) * 128)
                            M = mac[n % 2]
                            n += 1
                            for b, (y, w, nk) in enumerate(((ya[yi], wa[wi_], 8), (ys[yi], ws[wi_], 16), (yl[yi], wl[wi_], 16))):
                                pP = P()
                                for k in range(nk):
                                    mm(pP[:, :], w[:, k, osl], y[:, k, :], k == 0, k == nk - 1)
                                pG = P()
                                for kc in range(8):
                                    mm(pG[:, :], wm[wi_][:, b, kc, osl], hT[:, kc, tsl], kc == 0, kc == 7)
                                SG = sgt[b % 2]
                                act_(SG[:], pG[:, :], AF.Sigmoid)
                                if b == 0:
                                    tt_('dve', M[:], SG[:], pP[:, :], ALU.mult)
                                else:
                                    T = tmp[b % 2]
                                    tt_('dve', T[:], SG[:], pP[:, :], ALU.mult)
                                    tt_('pool', M[:], M[:], T[:], ALU.add)
                            copy_('pool', mT[:, qt * 2 + oc, tsl], M[:])
                kb.barrier()
            with ExitStack() as es:
                A = lambda n, sh, dt: es.enter_context(nc.sbuf_tensor(f"{n}_{l}", sh, dt))
                wo = A("gwo", [128, 8, 1024], BF16)
                X = [A(f"gX{i}", [128, 8, 512], F32) for i in range(2)]
                for c0 in range(0, 1024, 512):
                    kb.dma(wo[:, :, c0:c0 + 512], wo_d[l].rearrange("(kc p) c -> p kc c", p=128)[:, :, c0:c0 + 512], 'gwo', q='pool')
                for tt in range(4):
                    tsl = slice(tt * 512, (tt + 1) * 512)
                    Xt = X[tt % 2]
                    kb.dma(Xt[:], xs_d[:, :, tsl], f'gX{tt % 2}')
                    for oc in range(8):
                        p = P()
                        for kc in range(8):
                            mm(p[:, :], wo[:, kc, oc * 128:(oc + 1) * 128], mT[:, kc, tsl], kc == 0, kc == 7)
                        stt_(Xt[:, oc, :], p[:, :], cond[:, 16 + oc:17 + oc], Xt[:, oc, :], ALU.mult, ALU.add)
                    kb.dma(xs_d[:, :, tsl], Xt[:], f'gX{tt % 2}')
                kb.barrier()

    def moe_phase(l):
        with ExitStack() as es:
            A = lambda n, sh, dt: es.enter_context(nc.sbuf_tensor(f"{n}_{l}", sh, dt))
            yacc = A("myacc", [128, 16, 1024], F32)
            wt = A("mwt", [128, 16, 32], F32)
            wrt = A("mwrt", [128, 8, 36], BF16)
            lg = A("mlg", [128, 36], F32)
            sm = A("msm", [128, 16], F32)
            oh = A("moh", [128, 4], F32)
            pen = A("mpen", [128, 4], F32)
            eg4 = A("meg4", [128, 4], F32)
            lem = A("mlem", [128, 4, 8], F32)
            m8 = A("mm8", [128, 8], F32)
            eq1 = A("meq1", [128, 32], F32)
            eq2 = A("meq2", [128, 32], F32)
            actb = [A(f"mact{i}", [128, 4, S], BF16) for i in range(2)]
            wg = [A(f"mwg{i}", [128, 8, 512], BF16) for i in range(2)]
            wu = [A(f"mwu{i}", [128, 8, 512], BF16) for i in range(2)]
            wd = [A(f"mwd{i}", [128, 4, 1024], BF16) for i in range(2)]
            sgt = [A(f"msg{i}", [128, 512], F32) for i in range(2)]
            X = [A(f"mX{i}", [128, 8, 128], F32) for i in range(2)]
            kb.dma(wrt[:, :, 0:4], rwg_d[l].rearrange("(kc p) g -> p kc g", p=128), 'mwrt', q='pool')
            kb.dma(wrt[:, :, 4:36], rwe_d[l].rearrange("(kc p) g -> p kc g", p=128), 'mwrt', q='pool')
            rb = PK('rb')
            for t in range(16):
                tk = slice(t * 128, (t + 1) * 128)
                p = P()
                for kc in range(8):
                    mm(p[:, 0:36], hT[:, kc, tk], wrt[:, kc, :], kc == 0, kc == 7)
                tt_('dve', lg[:], p[:, 0:36], rb, ALU.add)
                c = lambda i: sm[:, i:i + 1]
                V('dve', lambda: nc.vector.reduce_max(out=c(0), in_=lg[:, 0:4], axis=AX.X), [c(0)], [lg[:, 0:4]])
                ts_('dve', oh[:], lg[:, 0:4], c(0), None, ALU.is_ge)
                ts_('dve', c(1), c(0), -1.0, None, ALU.mult)
                act_(eg4[:], lg[:, 0:4], AF.Exp, bias=c(1))
                V('dve', lambda: nc.vector.reduce_sum(out=c(2), in_=eg4[:], axis=AX.X), [c(2)], [eg4[:]])
                V('dve', lambda: nc.vector.reciprocal(out=c(3), in_=c(2)), [c(3)], [c(2)])
                ts_('dve', pen[:], oh[:], -1.0, 30000.0, ALU.add, ALU.mult)
                tt_('dve', lem[:], lg[:, 4:36].rearrange("p (g e) -> p g e", g=4),
                    oh[:, :][:, :, None].broadcast_to([128, 4, 8]), ALU.mult)
                tt_('dve', lem[:], lem[:], pen[:, :][:, :, None].broadcast_to([128, 4, 8]), ALU.add)
                lemf = lem[:, :, :].rearrange("p g e -> p (g e)")
                V('dve', lambda: nc.vector.max(out=m8[:], in_=lemf), [m8[:]], [lemf])
                ts_('dve', eq1[:], lemf, m8[:, 0:1], None, ALU.is_ge)
                ts_('dve', eq2[:], lemf, m8[:, 1:2], None, ALU.is_ge)
                tt_('dve', c(4), m8[:, 1:2], m8[:, 0:1], ALU.subtract)
                act_(c(5), c(4), AF.Exp)
                ts_('dve', c(6), c(5), 1.0, None, ALU.add)
                V('dve', lambda: nc.vector.reciprocal(out=c(7), in_=c(6)), [c(7)], [c(6)])
                tt_('dve', c(8), c(3), c(7), ALU.mult)
                tt_('dve', c(9), c(8), c(5), ALU.mult)
                tt_('dve', c(10), c(8), c(9), ALU.subtract)
                ts_('dve', wt[:, t, :], eq2[:], c(9), None, ALU.mult)
                stt_(wt[:, t, :], eq1[:], c(10), wt[:, t, :], ALU.mult, ALU.add)
            for e in range(NEXP):
                b = e % 2
                kb.dma(wg[b][:], eg_d[l, e].rearrange("(kc p) f -> p kc f", p=128), f'mwg{b}', q='pool')
                kb.dma(wu[b][:], eu_d[l, e].rearrange("(kc p) f -> p kc f", p=128), f'mwu{b}', q='pool')
                for c0 in range(0, 1024, 512):
                    kb.dma(wd[b][:, :, c0:c0 + 512], ed_d[l, e].rearrange("(k p) c -> p k c", p=128)[:, :, c0:c0 + 512], f'mwd{b}', q='pool')
                n = 0
                for fcn in range(4):
                    fsl = slice(fcn * 128, (fcn + 1) * 128)
                    for tt in range(4):
                        tsl = slice(tt * 512, (tt + 1) * 512)
                        pg = P()
                        for kc in range(8):
                            mm(pg[:, :], wg[b][:, kc, fsl], hT[:, kc, tsl], kc == 0, kc == 7)
                        pu = P()
                        for kc in range(8):
                            mm(pu[:, :], wu[b][:, kc, fsl], hT[:, kc, tsl], kc == 0, kc == 7)
                        SG = sgt[n % 2]
                        n += 1
                        act_(SG[:], pg[:, :], AF.Silu)
                        tt_('dve', actb[b][:, fcn, tsl], SG[:], pu[:, :], ALU.mult)
                for t in range(16):
                    tk = slice(t * 128, (t + 1) * 128)
                    for half in range(2):
                        hsl = slice(half * 512, (half + 1) * 512)
                        pd = P()
                        for k in range(4):
                            mm(pd[:, :], actb[b][:, k, tk], wd[b][:, k, hsl], k == 0, k == 3)
                        if e == 0:
                            ts_('dve', yacc[:, t, hsl], pd[:, :], wt[:, t, e:e + 1], None, ALU.mult)
                        else:
                            stt_(yacc[:, t, hsl], pd[:, :], wt[:, t, e:e + 1], yacc[:, t, hsl], ALU.mult, ALU.add)
            for t in range(16):
                tk = slice(t * 128, (t + 1) * 128)
                Xt = X[t % 2]
                kb.dma(Xt[:], xs_d[:, :, tk], f'mX{t % 2}')
                for half in range(2):
                    p = P()
                    for j in range(4):
                        kc = half * 4 + j
                        tr(p[:, j * 128:(j + 1) * 128], yacc[:, t, kc * 128:(kc + 1) * 128], identf)
                    for j in range(4):
                        kc = half * 4 + j
                        stt_(Xt[:, kc, :], p[:, j * 128:(j + 1) * 128], cond[:, 40 + kc:41 + kc], Xt[:, kc, :], ALU.mult, ALU.add)
                kb.dma(xs_d[:, :, tk], Xt[:], f'mX{t % 2}')
            kb.barrier()

    def final_phase():
        with ExitStack() as es:
            A = lambda n, sh, dt: es.enter_context(nc.sbuf_tensor(n, sh, dt))
            xt = [A(f"fx{i}", [128, 8, 512], F32) for i in range(2)]
            sq = [A(f"fsq{i}", [128, 512], F32) for i in range(2)]
            rs = A("frs", [128, 512], F32)
            yf = A("fyf", [128, 8, 512], F32)
            ot = [A(f"fot{i}", [128, 1024], F32) for i in range(2)]
            fg = G('fg')
            n = 0
            for tt in range(4):
                Xt = xt[tt % 2]
                kb.dma(Xt[:], xs_d[:, :, tt * 512:(tt + 1) * 512], f'fx{tt % 2}')
                pss = P()
                for kc in range(8):
                    act_(sq[kc % 2][:], Xt[:, kc, :], AF.Square)
                    mm(pss[:, :], onesf, sq[kc % 2][:], kc == 0, kc == 7)
                act_(rs[:], pss[:, :], AF.Sqrt, bias=EPSC[:, 0:1], scale=1.0 / D)
                V('dve', lambda: nc.vector.reciprocal(out=rs[:], in_=rs[:]), [rs[:]], [rs[:]])
                for kc in range(8):
                    stt_(yf[:, kc, :], Xt[:, kc, :], fg[:, kc:kc + 1], rs[:], ALU.mult, ALU.mult)
                for t4 in range(4):
                    O = ot[n % 2]
                    n += 1
                    for half in range(2):
                        p = P()
                        for j in range(4):
                            kc = half * 4 + j
                            tr(p[:, j * 128:(j + 1) * 128], yf[:, kc, t4 * 128:(t4 + 1) * 128], identf)
                        copy_('act' if half else 'dve', O[:, half * 512:(half + 1) * 512], p[:, :])
                    r0 = tt * 512 + t4 * 128
                    kb.dma(out_d[r0:r0 + 128, :], O[:], f'fot{(n - 1) % 2}')
            kb.barrier()

    ph = phases
    load_x_phase()
    if ph is None or 'rope' in ph:
        rope_phase()
    for l in range(L):
        if ph is None or 'cond' in ph:
            cond_phase(l)
        if ph is None or 'norm1' in ph:
            norm_phase(l, 0, f"a{l}")
        if ph is None or 'attn' in ph:
            attn_phase(l)
        if ph is None or 'lru' in ph:
            lru_phase(l)
        if ph is None or 'ssd' in ph:
            ssd_proj_phase(l)
            ssd_scan_phase(l)
        if ph is None or 'merge' in ph:
            merge_phase(l)
        if ph is None or 'moe' in ph:
            norm_phase(l, 1, f"b{l}")
            moe_phase(l)
    if ph is None or 'final' in ph:
        final_phase()
    kb.finish()
    return nc, kb


_CACHE = {}


def _in_maps(inp, L, cores):
    pk = np.stack([_pack_layer(inp, l) for l in range(L)])
    maps = []
    shared = {
        "pk": pk,
        "ada_w": inp['ada_w'][:L], "w_in": inp['w_in'][:L],
        "lru_w_r": inp['lru_w_r'][:L], "lru_w_i": inp['lru_w_i'][:L],
        "w_br_attn": inp['w_br_attn'][:L], "w_br_ssd": inp['w_br_ssd'][:L], "w_br_lru": inp['w_br_lru'][:L],
        "w_out": inp['w_out'][:L], "router_wg": inp['router_wg'][:L],
        "router_we": inp['router_we'][:L].reshape(L, D, 32),
        "exp_w_gate": inp['exp_w_gate'][:L], "exp_w_up": inp['exp_w_up'][:L], "exp_w_down": inp['exp_w_down'][:L],
    }
    for b in cores:
        m = dict(shared)
        m["x"] = np.ascontiguousarray(inp['x'][b])
        m["pos"] = np.ascontiguousarray(np.broadcast_to(inp['positions'][b][None, :], (128, S))).astype(np.int32)
        m["gk"] = _pack_global(inp['c'][b], inp['final_g'])
        maps.append(m)
    return maps


def kernel(**inputs):
    inp = {k: np.asarray(v) for k, v in inputs.items()}
    if 'nc' not in _CACHE:
        _CACHE['nc'] = build(DEPTH)[0]
    nc = _CACHE['nc']
    maps = _in_maps(inp, DEPTH, list(range(8)))
    res = run_bass_kernel_spmd(nc, maps, core_ids=list(range(8)))
    out = np.stack([np.asarray(res.results[c]["out"]) for c in range(8)], axis=0)
    return out.astype(np.float32)
```

```python
import numpy as np
import concourse.bass as bass
import concourse.mybir as mybir
from concourse.bass_utils import run_bass_kernel_spmd

F32 = mybir.dt.float32
BF16 = mybir.dt.bfloat16
I32 = mybir.dt.int32
ALU = mybir.AluOpType
AF = mybir.ActivationFunctionType
AX = mybir.AxisListType

_DTSIZE = {F32: 4, BF16: 2, I32: 4}
SEM_ROT = 30000

D = 1024
S = 2048
DEPTH = 4
NEXP = 32
FF = 512
INC = 16416
EPS = 1e-6
OQ, OK_, OV, OZ, OXBC, ODT, OLG, OLX, OMG = 0, 1536, 3072, 4608, 6656, 10752, 10784, 12064, 13344


def _box(ap):
    t = ap.tensor
    es = _DTSIZE[ap.dtype]
    pat = ap.ap
    off = int(ap.offset)
    if type(t).__name__.startswith('DRam'):
        ext = 1
        for st, cnt in pat:
            ext += (cnt - 1) * abs(st)
        return (t.name, 0, 1, off * es, (off + ext) * es)
    if type(t).__name__.startswith('PSum'):
        return (t.name, 0, 128, 0, 2048)
    free = 1
    for s in list(t.shape)[1:]:
        free *= s
    p0 = off // free
    f0 = off % free
    np_ = pat[0][1] if pat[0][0] != 0 else 1
    ext = 1
    for st, cnt in pat[1:]:
        ext += (cnt - 1) * abs(st)
    return (t.name, p0, p0 + np_, f0 * es, (f0 + ext) * es)


class Chan:
    def __init__(self, kb, name):
        self.kb = kb
        self.name = name
        self.sem = kb.nc.alloc_semaphore(name)
        kb.chan_by_sem[self.sem.num] = self
        self.cnt = 0
        self.gen = 0

    def next_event(self):
        if self.cnt + 16 > SEM_ROT:
            self.gen += 1
            self.sem = self.kb.nc.alloc_semaphore(f"{self.name}_g{self.gen}")
            self.kb.chan_by_sem[self.sem.num] = self
            self.cnt = 0
        self.cnt += 16
        return (self.sem, self.cnt)


class KB:
    def __init__(self, nc):
        self.nc = nc
        self.engs = {'pe': nc.tensor, 'act': nc.scalar, 'dve': nc.vector, 'pool': nc.gpsimd, 'sp': nc.sync}
        self.sem, self.cnt, self.gen = {}, {}, {}
        for e in ('pe', 'act', 'dve', 'pool'):
            self.sem[e] = nc.alloc_semaphore(f"s_{e}")
            self.cnt[e] = 0
            self.gen[e] = 0
        self.oldsems = []
        self.seen = {e: {} for e in self.engs}
        self.recs = {}
        self.pending = {e: False for e in self.engs}
        self.ninst = {e: 0 for e in self.engs}
        self.chans = {}
        self.chan_by_sem = {}

    def chan(self, name):
        if name not in self.chans:
            self.chans[name] = Chan(self, name)
        return self.chans[name]

    def _deps(self, ins, outs):
        deps = []
        for ap in ins:
            b = _box(ap)
            for r in self.recs.get(b[0], ()):
                if r[5] and r[1] < b[2] and b[1] < r[2] and r[3] < b[4] and b[3] < r[4]:
                    deps.append(r[6])
        for ap in outs:
            b = _box(ap)
            for r in self.recs.get(b[0], ()):
                if r[1] < b[2] and b[1] < r[2] and r[3] < b[4] and b[3] < r[4]:
                    deps.append(r[6])
        return deps

    def _record(self, ins, outs, ev, tag):
        for ap in outs:
            b = _box(ap)
            lst = self.recs.setdefault(b[0], [])
            lst[:] = [r for r in lst if not (b[1] <= r[1] and r[2] <= b[2] and b[3] <= r[3] and r[4] <= b[4])]
            lst.append((b[0], b[1], b[2], b[3], b[4], True, ev, tag))
        for ap in ins:
            b = _box(ap)
            lst = self.recs.setdefault(b[0], [])
            lst[:] = [r for r in lst if not ((not r[5]) and r[7] == tag and r[1:5] == b[1:5])]
            lst.append((b[0], b[1], b[2], b[3], b[4], False, ev, tag))

    def _wait(self, e, deps, skip_self=False):
        eng = self.engs[e]
        seen = self.seen[e]
        best = {}
        for sem, val in deps:
            if skip_self and e in self.sem and sem is self.sem[e]:
                continue
            k = sem.num
            ch = self.chan_by_sem.get(k)
            if ch is not None and ch.sem.num == k:
                val = max(val, ch.cnt)
            if seen.get(k, 0) >= val:
                continue
            if k not in best or best[k][1] < val:
                best[k] = (sem, val)
        for k, (sem, val) in best.items():
            eng.wait_ge(sem, val)
            seen[k] = val

    def op(self, e, fn, outs, ins, signal=True):
        deps = self._deps(ins, outs)
        self._wait(e, deps, skip_self=(e == 'pe'))
        inst = fn()
        self.ninst[e] += 1
        if signal and self.cnt[e] + 1 > SEM_ROT:
            self.oldsems.append((self.sem[e], self.cnt[e]))
            self.gen[e] += 1
            self.sem[e] = self.nc.alloc_semaphore(f"s_{e}_g{self.gen[e]}")
            self.cnt[e] = 0
        if signal:
            self.cnt[e] += 1
            inst.then_inc(self.sem[e], 1)
            ev = (self.sem[e], self.cnt[e])
            self.pending[e] = False
        else:
            assert self.cnt[e] + 1 <= SEM_ROT
            ev = (self.sem[e], self.cnt[e] + 1)
            self.pending[e] = True
        self._record(ins, outs, ev, e)
        return inst

    def dma(self, out, in_, chan, q='sp', **kw):
        ch = self.chan(chan)
        deps = self._deps([in_], [out])
        self._wait(q, deps)
        ev = ch.next_event()
        inst = self.engs[q].dma_start(out=out, in_=in_, **kw)
        inst.then_inc(ev[0], 16)
        self.ninst[q] += 1
        self._record([in_], [out], ev, 'dma_' + ch.name)
        return inst

    def all_events(self):
        evs = [(self.sem[e], self.cnt[e]) for e in ('pe', 'act', 'dve', 'pool') if self.cnt[e] > 0]
        evs += [(ch.sem, ch.cnt) for ch in self.chans.values() if ch.cnt > 0]
        return evs

    def barrier(self, drop=()):
        for e in ('pe', 'act', 'dve', 'pool'):
            assert not self.pending[e], e
        evs = self.all_events()
        for e in self.engs:
            self._wait(e, evs)
        self.recs.clear()

    def finish(self):
        for e in ('pe', 'act', 'dve', 'pool'):
            assert not self.pending[e], e
        self._wait('sp', self.all_events())


PK_COLS = {}
_o = 0
for _n, _w in (('n1g', 8), ('n2g', 8), ('adab', 48), ('scw', 128), ('scb', 32), ('dtb', 32), ('alog', 32),
               ('sdd', 16), ('sng', 16), ('rb', 36), ('lcw', 64), ('lcb', 16), ('lbr', 16), ('lbi', 16), ('llam', 16)):
    PK_COLS[_n] = (_o, _w)
    _o += _w
NPK = _o
GK_COLS = {}
_o = 0
for _n, _w in (('c', 8), ('fg', 8), ('freq', 1), ('ident', 128), ('mle', 128), ('mgt', 128), ('mge', 128), ('ones', 128),
               ('phase', 1)):
    GK_COLS[_n] = (_o, _w)
    _o += _w
NGK = _o


def _fm(v, p=128):
    return np.ascontiguousarray(v.reshape(-1, p).T)


def _pack_layer(inp, l):
    pk = np.zeros((128, NPK), np.float32)

    def put(name, arr):
        o, w = PK_COLS[name]
        assert arr.shape[1] == w, (name, arr.shape)
        pk[:arr.shape[0], o:o + w] = arr
    put('n1g', _fm(inp['norm1_g'][l]))
    put('n2g', _fm(inp['norm2_g'][l]))
    put('adab', _fm(inp['ada_b'][l]))
    scw = inp['ssd_conv_w'][l]
    put('scw', np.ascontiguousarray(scw.T.reshape(32, 128, 4).transpose(1, 0, 2)).reshape(128, 128))
    put('scb', _fm(inp['ssd_conv_b'][l]))
    put('dtb', np.broadcast_to(inp['ssd_dt_bias'][l][None, :], (128, 32)))
    put('alog', np.broadcast_to(inp['ssd_a_log'][l][None, :], (128, 32)))
    put('sdd', _fm(np.repeat(inp['ssd_d'][l], 64)))
    put('sng', _fm(inp['ssd_norm_g'][l]))
    rb = np.concatenate([inp['router_bg'][l].reshape(-1), inp['router_be'][l].reshape(-1)])
    put('rb', np.broadcast_to(rb[None, :], (128, 36)))
    lcw = inp['lru_conv_w'][l]
    put('lcw', np.ascontiguousarray(lcw.T.reshape(16, 80, 4).transpose(1, 0, 2)).reshape(80, 64))
    put('lcb', _fm(inp['lru_conv_b'][l], 80))
    put('lbr', _fm(inp['lru_b_r'][l], 80))
    put('lbi', _fm(inp['lru_b_i'][l], 80))
    put('llam', _fm(inp['lru_lambda'][l], 80))
    return pk


def _pack_global(c_b, final_g):
    gk = np.zeros((128, NGK), np.float32)

    def put(name, arr):
        o, w = GK_COLS[name]
        gk[:, o:o + w] = arr
    put('c', _fm(c_b))
    put('fg', _fm(final_g))
    half = 32
    freqs = (10000.0 ** (-np.arange(half, dtype=np.float32) / half)).astype(np.float32)
    put('freq', np.tile(freqs, 4)[:, None])
    i = np.arange(128)
    put('ident', (i[:, None] == i[None, :]).astype(np.float32))
    put('mle', (i[:, None] <= i[None, :]).astype(np.float32))
    put('mgt', (i[:, None] > i[None, :]).astype(np.float32))
    put('mge', (i[:, None] >= i[None, :]).astype(np.float32))
    put('ones', np.ones((128, 128), np.float32))
    put('phase', np.where((i % 64) < 32, -1.0, 1.0).astype(np.float32)[:, None])
    return gk


from contextlib import ExitStack
import math


def build(L=DEPTH, dbg=False, phases=None):
    nc = bass.Bass("TRN2", target_bir_lowering=False)
    kb = KB(nc)

    def din(name, shape, dt=F32):
        return nc.dram_tensor(name, list(shape), dt, kind="ExternalInput").ap()

    def dscr(name, shape, dt):
        return nc.dram_tensor(name, list(shape), dt, kind=("ExternalOutput" if dbg else "Internal")).ap()

    x_d = din("x", [S, D])
    pos_d = din("pos", [128, S], I32)
    gk_d = din("gk", [128, NGK])
    pk_d = din("pk", [L, 128, NPK])
    adaw_d = din("ada_w", [L, D, 6 * D])
    win_d = din("w_in", [L, D, INC])
    lwr_d = din("lru_w_r", [L, 16, 80, 80])
    lwi_d = din("lru_w_i", [L, 16, 80, 80])
    wba_d = din("w_br_attn", [L, 512, D])
    wbs_d = din("w_br_ssd", [L, 2048, D])
    wbl_d = din("w_br_lru", [L, 1280, D])
    wo_d = din("w_out", [L, D, D])
    rwg_d = din("router_wg", [L, D, 4])
    rwe_d = din("router_we", [L, D, 32])
    eg_d = din("exp_w_gate", [L, NEXP, D, FF])
    eu_d = din("exp_w_up", [L, NEXP, D, FF])
    ed_d = din("exp_w_down", [L, NEXP, FF, D])
    out_d = nc.dram_tensor("out", [S, D], F32, kind="ExternalOutput").ap()

    xs_d = dscr("xs_scr", [128, 8, S], F32)
    ya_d = dscr("ya_scr", [64, 8, S], BF16)
    ys_d = dscr("ys_scr", [128, 16, S], BF16)
    yl_d = dscr("yl_scr", [80, 16, S], BF16)
    xbc_d = nc.dram_tensor("xbc_scr", [128, 32, S], BF16).ap()
    z_d = nc.dram_tensor("z_scr", [128, 16, S], BF16).ap()
    cs_d = nc.dram_tensor("cs_scr", [128, 2, S], F32).ap()

    ps = [nc.alloc_psum_tensor(f"ps{i}", [128, 512], F32) for i in range(8)]
    pctr = [0]

    def P():
        t = ps[pctr[0] % 8]
        pctr[0] += 1
        return t

    def mm(out, lhsT, rhs, start=True, stop=True):
        return kb.op('pe', lambda: nc.tensor.matmul(out, lhsT=lhsT, rhs=rhs, start=start, stop=stop),
                     [out], [lhsT, rhs], signal=True)

    def tr(out, in_, ident):
        return kb.op('pe', lambda: nc.tensor.transpose(out=out, in_=in_, identity=ident), [out], [in_, ident])

    def V(e, fn, outs, ins):
        return kb.op(e, fn, outs, ins)

    def tt_(e, out, in0, in1, op):
        eng = kb.engs[e]
        return kb.op(e, lambda: eng.tensor_tensor(out=out, in0=in0, in1=in1, op=op), [out], [in0, in1])

    def ts_(e, out, in0, s1, s2, op0, op1=ALU.bypass):
        eng = kb.engs[e]
        ins = [in0] + [s for s in (s1, s2) if not isinstance(s, (int, float)) and s is not None]
        if s2 is None:
            return kb.op(e, lambda: eng.tensor_scalar(out=out, in0=in0, scalar1=s1, scalar2=None, op0=op0), [out], ins)
        return kb.op(e, lambda: eng.tensor_scalar(out=out, in0=in0, scalar1=s1, scalar2=s2, op0=op0, op1=op1), [out], ins)

    def stt_(out, in0, scalar, in1, op0, op1):
        ins = [in0, in1] + ([] if isinstance(scalar, (int, float)) else [scalar])
        return kb.op('dve', lambda: nc.vector.scalar_tensor_tensor(out=out, in0=in0, scalar=scalar, in1=in1, op0=op0, op1=op1),
                     [out], ins)

    def act_(out, in_, func, bias=0.0, scale=1.0):
        ins = [in_] + [s for s in (bias, scale) if not isinstance(s, (int, float))]
        return kb.op('act', lambda: nc.scalar.activation(out=out, in_=in_, func=func, bias=bias, scale=scale), [out], ins)

    def copy_(e, out, in_):
        if e == 'act':
            return kb.op('act', lambda: nc.scalar.copy(out=out, in_=in_), [out], [in_])
        eng = kb.engs[e]
        return kb.op(e, lambda: eng.tensor_copy(out=out, in_=in_), [out], [in_])

    def memset_(e, ap, val):
        eng = kb.engs[e]
        return kb.op(e, lambda: eng.memset(ap, val), [ap], [])

    def winv(l):
        return win_d[l].rearrange("(kc p) c -> p kc c", p=128)

    gk = nc.alloc_sbuf_tensor("gk_sb", [128, NGK], F32)
    kb.dma(gk[:], gk_d[:, :], 'c0')

    def G(name):
        o, w = GK_COLS[name]
        return gk[:, o:o + w]
    identf, mle, mgt, mge, onesf = G('ident'), G('mle'), G('mgt'), G('mge'), G('ones')
    cb = nc.alloc_sbuf_tensor("cb", [128, 3, 128], BF16)
    for i, nm in enumerate(('ident', 'mle', 'mge')):
        copy_('dve', cb[:, i, :], G(nm))
    identb = cb[:, 0, :]
    hT = nc.alloc_sbuf_tensor("hT", [128, 8, S], BF16)
    pk = nc.alloc_sbuf_tensor("pk_sb", [128, NPK], F32)
    cond = nc.alloc_sbuf_tensor("cond", [128, 48], F32)
    gm = nc.alloc_sbuf_tensor("gm", [128, 16], F32)
    cact = nc.alloc_sbuf_tensor("cact", [128, 8], F32)
    dtraw = nc.alloc_sbuf_tensor("dtraw", [128, 16, 32], F32)
    act_(cact[:], G('c'), AF.Silu)

    def PK(name, rows=128):
        o, w = PK_COLS[name]
        return pk[0:rows, o:o + w]

    def load_x_phase():
        with ExitStack() as es:
            xin = [es.enter_context(nc.sbuf_tensor(f"xin{i}", [128, D], F32)) for i in range(2)]
            xo = [es.enter_context(nc.sbuf_tensor(f"xo{i}", [128, 8, 128], F32)) for i in range(2)]
            for t in range(16):
                xi = xin[t % 2]
                kb.dma(xi[:], x_d[t * 128:(t + 1) * 128, :], f'xin{t % 2}')
                for half in range(2):
                    p = P()
                    for j in range(4):
                        kc = half * 4 + j
                        tr(p[:, j * 128:(j + 1) * 128], xi[:, kc * 128:(kc + 1) * 128], identf)
                    copy_('dve' if half == 0 else 'act', xo[t % 2][:, half * 4:(half + 1) * 4, :],
                          p[:, :].rearrange("p (j t) -> p j t", j=4))
                kb.dma(xs_d[:, :, t * 128:(t + 1) * 128], xo[t % 2][:], f'xo{t % 2}')
            kb.barrier()

    def rope_phase():
        with ExitStack() as es:
            posi = es.enter_context(nc.sbuf_tensor("posi", [128, S], I32))
            ki = es.enter_context(nc.sbuf_tensor("rki", [128, S], I32))
            ang = es.enter_context(nc.sbuf_tensor("ang", [128, S], F32))
            u = es.enter_context(nc.sbuf_tensor("ru", [128, S], F32))
            kf = es.enter_context(nc.sbuf_tensor("rkf", [128, S], F32))
            r = es.enter_context(nc.sbuf_tensor("rr", [128, S], F32))
            m = es.enter_context(nc.sbuf_tensor("rm", [128, S], F32))
            kb.dma(posi[:], pos_d[:, :], 'c0')
            copy_('dve', ang[:], posi[:])
            ts_('dve', ang[:], ang[:], G('freq'), None, ALU.mult)
            TWO_PI = 2.0 * math.pi
            C1 = 6.28125
            C2 = TWO_PI - C1
            for which, phi in ((0, math.pi / 2), (1, 0.0)):
                ts_('dve', u[:], ang[:], 1.0 / TWO_PI, phi / TWO_PI + 0.5, ALU.mult, ALU.add)
                copy_('dve', ki[:], u[:])
                copy_('dve', kf[:], ki[:])
                stt_(r[:], kf[:], -C1, ang[:], ALU.mult, ALU.add)
                stt_(r[:], kf[:], -C2, r[:], ALU.mult, ALU.add)
                if phi != 0.0:
                    ts_('dve', r[:], r[:], phi, None, ALU.add)
                ts_('dve', m[:], r[:], -math.pi, None, ALU.is_lt)
                stt_(r[:], m[:], TWO_PI, r[:], ALU.mult, ALU.add)
                ts_('dve', m[:], r[:], math.pi, None, ALU.is_gt)
                stt_(r[:], m[:], -TWO_PI, r[:], ALU.mult, ALU.add)
                ts_('dve', r[:], r[:], -3.1415925, 3.1415925, ALU.max, ALU.min)
                act_(u[:], r[:], AF.Sin)
                if which == 1:
                    ts_('dve', u[:], u[:], G('phase'), None, ALU.mult)
                kb.dma(cs_d[:, which, :], u[:], 'cs_st')
            kb.barrier()

    def cond_phase(l):
        with ExitStack() as es:
            aw = [es.enter_context(nc.sbuf_tensor(f"aw{l}_{i}", [128, 8, 512], F32)) for i in range(2)]
            kb.dma(pk[:], pk_d[l], 'pk')
            pc = P()
            src = adaw_d[l].rearrange("(kc p) c -> p kc c", p=128)
            for cc in range(12):
                w = aw[cc % 2]
                kb.dma(w[:], src[:, :, cc * 512:(cc + 1) * 512], f'aw{cc % 2}')
                for j in range(4):
                    col = cc * 4 + j
                    for kc in range(8):
                        mm(pc[:, col:col + 1], w[:, kc, j * 128:(j + 1) * 128], cact[:, kc:kc + 1], kc == 0, kc == 7)
            tt_('dve', cond[:], pc[:, 0:48], PK('adab'), ALU.add)
            stt_(gm[:, 0:8], cond[:, 8:16], 1.0, PK('n1g'), ALU.add, ALU.mult)
            stt_(gm[:, 8:16], cond[:, 32:40], 1.0, PK('n2g'), ALU.add, ALU.mult)
            kb.barrier()

    def norm_phase(l, which, tag):
        with ExitStack() as es:
            xt = [es.enter_context(nc.sbuf_tensor(f"nx{tag}_{i}", [128, 8, 512], F32)) for i in range(2)]
            sq = [es.enter_context(nc.sbuf_tensor(f"nsq{tag}_{i}", [128, 512], F32)) for i in range(2)]
            rs = [es.enter_context(nc.sbuf_tensor(f"nrs{tag}_{i}", [128, 512], F32)) for i in range(2)]
            t2 = [es.enter_context(nc.sbuf_tensor(f"nt2{tag}_{i}", [128, 512], F32)) for i in range(2)]
            sh0 = 0 if which == 0 else 24
            for tt in range(4):
                X = xt[tt % 2]
                kb.dma(X[:], xs_d[:, :, tt * 512:(tt + 1) * 512], f'nx{tt % 2}')
                pss = P()
                for kc in range(8):
                    act_(sq[kc % 2][:], X[:, kc, :], AF.Square)
                    mm(pss[:, :], onesf, sq[kc % 2][:], kc == 0, kc == 7)
                R_ = rs[tt % 2]
                act_(R_[:], pss[:, :], AF.Sqrt, bias=EPSC[:, 0:1], scale=1.0 / D)
                V('dve', lambda: nc.vector.reciprocal(out=R_[:], in_=R_[:]), [R_[:]], [R_[:]])
                for kc in range(8):
                    T = t2[kc % 2]
                    tt_('dve', T[:], X[:, kc, :], R_[:], ALU.mult)
                    ts_('pool', hT[:, kc, tt * 512:(tt + 1) * 512], T[:], gm[:, which * 8 + kc:which * 8 + kc + 1],
                        cond[:, sh0 + kc:sh0 + kc + 1], ALU.mult, ALU.add)
            kb.barrier()

    epsc = nc.alloc_sbuf_tensor("epsc", [128, 2], F32)
    EPSC = epsc
    memset_('dve', epsc[:, 0:1], EPS)
    memset_('dve', epsc[:, 1:2], 1.0)

    def attn_tok(g, ti):
        if g == 0:
            return slice(128 * ti, 128 * ti + 128, 1)
        if g == 1:
            st = 512 * (ti // 4) + (ti % 4)
            return slice(st, st + 4 * 127 + 1, 4)
        return slice(ti, ti + 16 * 127 + 1, 16)

    def attn_prev(g, ti):
        if g == 0:
            return ti - 1 if ti >= 1 else None
        if g == 1:
            return ti - 4 if ti >= 4 else None
        return None

    def attn_phase(l):
        with ExitStack() as es:
            A = lambda n, sh, dt: es.enter_context(nc.sbuf_tensor(f"{n}_{l}", sh, dt))
            cs = A("acs", [128, 2, S], F32)
            qk = A("aqk", [128, 2, 2, S], BF16)
            vaug = A("avaug", [128, 16, 4, 128], BF16)
            acc = A("aacc", [128, 4, S], F32)
            wq = [A(f"awq{i}", [128, 8, 256], BF16) for i in range(3)]
            wst = [A(f"awst{i}", [128, 8, 256], F32) for i in range(2)]
            ta = [A(f"ata{i}", [128, 512], F32) for i in range(2)]
            tb = [A(f"atb{i}", [128, 512], F32) for i in range(2)]
            NPT = 6
            pt = [A(f"apt{i}", [128, 512], BF16) for i in range(NPT)]
            ptm = [A(f"aptm{i}", [128, 4, 128], BF16) for i in range(NPT)]
            rd = A("ard", [64, 4, 512], F32)
            yo = [A(f"ayo{i}", [64, 4, 512], BF16) for i in range(2)]
            kb.dma(cs[:], cs_d[:, :, :], 'acs')
            memset_('pool', vaug[:, :, :, 64:128], 1.0)
            wi = 0
            pi = 0
            for hh in range(2):
                for g in range(3):
                    for which, off in ((0, OQ), (1, OK_)):
                        c0 = off + g * 512 + hh * 256
                        W = wq[wi % 3]
                        kb.dma(wst[wi % 2][:], winv(l)[:, :, c0:c0 + 256], f'awst{wi % 2}')
                        copy_('pool', W[:], wst[wi % 2][:])
                        wi += 1
                        for j in range(2):
                            for tt in range(4):
                                tsl = slice(tt * 512, (tt + 1) * 512)
                                p = P()
                                for kc in range(8):
                                    mm(p[:, :], W[:, kc, j * 128:(j + 1) * 128], hT[:, kc, tsl], kc == 0, kc == 7)
                                TA, TB = ta[tt % 2], tb[tt % 2]
                                tt_('dve', TA[:], p[:, :], cs[:, 0, tsl], ALU.mult)
                                for q4 in range(4):
                                    o0 = q4 * 32
                                    i0 = o0 + 32 if q4 % 2 == 0 else o0 - 32
                                    tt_('dve', TB[o0:o0 + 32, :], p[i0:i0 + 32, :], cs[o0:o0 + 32, 1, tsl], ALU.mult)
                                tt_('pool', qk[:, which, j, tsl], TA[:], TB[:], ALU.add)
                    c0 = OV + g * 512 + hh * 256
                    W = wq[wi % 3]
                    kb.dma(wst[wi % 2][:], winv(l)[:, :, c0:c0 + 256], f'awst{wi % 2}')
                    copy_('pool', W[:], wst[wi % 2][:])
                    wi += 1
                    for ti in range(16):
                        tok = attn_tok(g, ti)
                        p = P()
                        for kc in range(8):
                            mm(p[:, 0:256], hT[:, kc, tok], W[:, kc, :], kc == 0, kc == 7)
                        copy_('act', vaug[:, ti, :, 0:64], p[:, 0:256].rearrange("p (h d) -> p h d", h=4))
                    def stage_s(ti):
                        nonlocal pi
                        tq = attn_tok(g, ti)
                        pv = attn_prev(g, ti)
                        kts = ([(pv, 2)] if pv is not None else []) + [(ti, 1)]
                        pms = []
                        for kt, mk in kts:
                            tk = attn_tok(g, kt)
                            PT, PM = pt[pi % NPT], ptm[pi % NPT]
                            pi += 1
                            PT3 = PT[:, :].rearrange("p (h q) -> p h q", h=4)
                            for par in range(2):
                                p = P()
                                bp = par * 64
                                for j in range(2):
                                    mm(p[:, j * 128:(j + 1) * 128], qk[bp:bp + 64, 1, j, tk], qk[bp:bp + 64, 0, j, tq])
                                act_(PT3[:, par::2, :], p[:, 0:256].rearrange("p (j q) -> p j q", j=2), AF.Exp, scale=0.125)
                            tt_('dve', PM[:], PT3, cb[:, mk, :][:, None, :].broadcast_to([128, 4, 128]), ALU.mult)
                            pms.append((kt, PM))
                        return pms

                    def stage_pv(ti, pms):
                        tq = attn_tok(g, ti)
                        po = P()
                        for h in range(4):
                            for idx, (kt, PM) in enumerate(pms):
                                mm(po[:, h * 128:(h + 1) * 128], vaug[:, kt, h, :], PM[:, h, :], idx == 0, idx == len(pms) - 1)
                        pov = po[:, :].rearrange("p (h q) -> p h q", h=4)
                        if g == 0:
                            copy_('dve', acc[:, :, tq], pov)
                        else:
                            tt_('dve', acc[:, :, tq], pov, acc[:, :, tq], ALU.add)

                    prev = None
                    for ti in range(16):
                        cur = (ti, stage_s(ti))
                        if prev is not None:
                            stage_pv(*prev)
                        prev = cur
                    stage_pv(*prev)
                for tt in range(4):
                    tsl = slice(tt * 512, (tt + 1) * 512)
                    V('dve', lambda: nc.vector.reciprocal(out=rd[:], in_=acc[64:128, :, tsl]), [rd[:]], [acc[64:128, :, tsl]])
                    Y = yo[tt % 2]
                    tt_('dve', Y[:], acc[0:64, :, tsl], rd[:], ALU.mult)
                    kb.dma(ya_d[:, hh * 4:(hh + 1) * 4, tsl], Y[:], f'ayo{tt % 2}')
            kb.barrier()

    def lru_phase(l):
        with ExitStack() as es:
            A = lambda n, sh, dt: es.enter_context(nc.sbuf_tensor(f"{n}_{l}", sh, dt))
            wlx = A("lwlx", [128, 8, 1280], BF16)
            wlg = A("lwlg", [128, 8, 1280], BF16)
            wr = A("lwr", [80, 16, 80], BF16)
            wi_ = A("lwi", [80, 16, 80], BF16)
            cneg = A("lcneg", [80, 16], F32)
            c2 = A("lc2", [80, 16], F32)
            xpre = A("lxpre", [80, 3 + S], F32)
            xc = A("lxc", [80, S], F32)
            xcb = A("lxcb", [80, S], BF16)
            rg = A("lrg", [80, S], F32)
            ig = A("lig", [80, S], F32)
            Aa = A("laa", [80, S], F32)
            T1 = A("lt1", [80, S], F32)
            uu = A("luu", [80, S], F32)
            hs = A("lhs", [80, S], F32)
            gl = A("lgl", [80, S], F32)
            yo = [A(f"lyo{i}", [80, S], BF16) for i in range(2)]
            for (w, off, nm) in ((wlx, OLX, 'lwlx'), (wlg, OLG, 'lwlg')):
                for c0 in range(0, 1280, 320):
                    kb.dma(w[:, :, c0:c0 + 320], winv(l)[:, :, off + c0:off + c0 + 320], nm, q='pool')
            kb.dma(wr[:], lwr_d[l].rearrange("k i j -> i k j"), 'lwr', q='pool')
            kb.dma(wi_[:], lwi_d[l].rearrange("k i j -> i k j"), 'lwi', q='pool')
            lcw = PK('lcw', 80)
            lcb, lbr, lbi, llam = PK('lcb', 80), PK('lbr', 80), PK('lbi', 80), PK('llam', 80)
            act_(cneg[:], llam, AF.Exp, scale=-1.0)
            act_(cneg[:], cneg[:], AF.Ln, bias=EPSC[0:80, 1:2])
            ts_('dve', c2[:], cneg[:], -16.0, None, ALU.mult)
            ts_('dve', cneg[:], cneg[:], -8.0, None, ALU.mult)
            memset_('dve', xpre[:, 0:3], 0.0)
            for k in range(16):
                ksl = slice(k * 80, (k + 1) * 80)
                for tt in range(4):
                    tsl = slice(tt * 512, (tt + 1) * 512)
                    p = P()
                    for kc in range(8):
                        mm(p[0:80, :], wlx[:, kc, ksl], hT[:, kc, tsl], kc == 0, kc == 7)
                    copy_('act', xpre[:, 3 + tt * 512:3 + (tt + 1) * 512], p[0:80, :])
                ts_('dve', xc[:], xpre[:, 0:S], lcw[:, k * 4:k * 4 + 1], lcb[:, k:k + 1], ALU.mult, ALU.add)
                for j in range(1, 4):
                    stt_(xc[:], xpre[:, j:j + S], lcw[:, k * 4 + j:k * 4 + j + 1], xc[:], ALU.mult, ALU.add)
                copy_('pool', xcb[:], xc[:])
                for (wg, bb, dst) in ((wr, lbr, rg), (wi_, lbi, ig)):
                    for tt in range(4):
                        tsl = slice(tt * 512, (tt + 1) * 512)
                        p = P()
                        mm(p[0:80, :], wg[:, k, :], xcb[:, tsl])
                        act_(dst[:, tsl], p[0:80, :], AF.Sigmoid, bias=bb[:, k:k + 1])
                act_(Aa[:], rg[:], AF.Exp, scale=cneg[:, k:k + 1])
                act_(T1[:], rg[:], AF.Exp, scale=c2[:, k:k + 1])
                ts_('dve', T1[:], T1[:], -1.0, 1.0, ALU.mult, ALU.add)
                ts_('dve', T1[:], T1[:], 1e-12, None, ALU.max)
                act_(T1[:], T1[:], AF.Sqrt)
                tt_('pool', uu[:], ig[:], xc[:], ALU.mult)
                tt_('dve', uu[:], uu[:], T1[:], ALU.mult)
                V('dve', lambda: nc.vector.tensor_tensor_scan(out=hs[:], data0=Aa[:], data1=uu[:], initial=0.0,
                                                              op0=ALU.mult, op1=ALU.add), [hs[:]], [Aa[:], uu[:]])
                for tt in range(4):
                    tsl = slice(tt * 512, (tt + 1) * 512)
                    p = P()
                    for kc in range(8):
                        mm(p[0:80, :], wlg[:, kc, ksl], hT[:, kc, tsl], kc == 0, kc == 7)
                    act_(gl[:, tsl], p[0:80, :], AF.Gelu)
                Y = yo[k % 2]
                tt_('dve', Y[:], hs[:], gl[:], ALU.mult)
                kb.dma(yl_d[:, k, :], Y[:], f'lyo{k % 2}')
            kb.barrier()

    def ssd_proj_phase(l):
        with ExitStack() as es:
            A = lambda n, sh, dt: es.enter_context(nc.sbuf_tensor(f"{n}_{l}", sh, dt))
            W = [A(f"sw{i}", [128, 8, 512], BF16) for i in range(2)]
            wdt = A("swdt", [128, 8, 32], BF16)
            pre = [A(f"spre{i}", [128, 3 + S], F32) for i in range(2)]
            cv = [A(f"scv{i}", [128, S], F32) for i in range(2)]
            ob = [A(f"sob{i}", [128, S], BF16) for i in range(2)]
            scw, scb = PK('scw'), PK('scb')
            for i in range(2):
                memset_('dve', pre[i][:, 0:3], 0.0)
            n = 0
            for cg in range(8):
                Wt = W[cg % 2]
                kb.dma(Wt[:], winv(l)[:, :, OXBC + cg * 512:OXBC + (cg + 1) * 512], f'sw{cg % 2}', q='pool')
                for j in range(4):
                    fc = cg * 4 + j
                    PR, CV, OB = pre[n % 2], cv[n % 2], ob[n % 2]
                    n += 1
                    for tt in range(4):
                        p = P()
                        for kc in range(8):
                            mm(p[:, :], Wt[:, kc, j * 128:(j + 1) * 128], hT[:, kc, tt * 512:(tt + 1) * 512], kc == 0, kc == 7)
                        copy_('act', PR[:, 3 + tt * 512:3 + (tt + 1) * 512], p[:, :])
                    ts_('dve', CV[:], PR[:, 0:S], scw[:, fc * 4:fc * 4 + 1], None, ALU.mult)
                    for jj in range(1, 4):
                        stt_(CV[:], PR[:, jj:jj + S], scw[:, fc * 4 + jj:fc * 4 + jj + 1], CV[:], ALU.mult, ALU.add)
                    act_(OB[:], CV[:], AF.Silu, bias=scb[:, fc:fc + 1])
                    kb.dma(xbc_d[:, fc, :], OB[:], f'sob{(n - 1) % 2}')
            for cg in range(4):
                Wt = W[cg % 2]
                kb.dma(Wt[:], winv(l)[:, :, OZ + cg * 512:OZ + (cg + 1) * 512], f'sw{cg % 2}', q='pool')
                for j in range(4):
                    fc = cg * 4 + j
                    OB = ob[n % 2]
                    n += 1
                    for tt in range(4):
                        p = P()
                        for kc in range(8):
                            mm(p[:, :], Wt[:, kc, j * 128:(j + 1) * 128], hT[:, kc, tt * 512:(tt + 1) * 512], kc == 0, kc == 7)
                        act_(OB[:, tt * 512:(tt + 1) * 512], p[:, :], AF.Silu)
                    kb.dma(z_d[:, fc, :], OB[:], f'sob{(n - 1) % 2}')
            kb.dma(wdt[:], winv(l)[:, :, ODT:ODT + 32], 'swdt', q='pool')
            for c in range(16):
                p = P()
                for kc in range(8):
                    mm(p[:, 0:32], hT[:, kc, c * 128:(c + 1) * 128], wdt[:, kc, :], kc == 0, kc == 7)
                tt_('dve', dtraw[:, c, :], p[:, 0:32], PK('dtb'), ALU.add)
            kb.barrier()

    def lru_ssdproj_phase(l):
        with ExitStack() as es:
            A = lambda n, sh, dt: es.enter_context(nc.sbuf_tensor(f"{n}_{l}", sh, dt))
            wlxk = [A(f"qwlx{i}", [128, 8, 80], BF16) for i in range(2)]
            wlgk = [A(f"qwlg{i}", [128, 8, 80], BF16) for i in range(2)]
            wr = A("qwr", [80, 16, 80], BF16)
            wi_ = A("qwi", [80, 16, 80], BF16)
            cneg = A("qcneg", [80, 16], F32)
            c2 = A("qc2", [80, 16], F32)
            xpre = A("qxpre", [80, 3 + S], F32)
            xc = A("qxc", [80, S], F32)
            xcb = A("qxcb", [80, S], BF16)
            rg = A("qrg", [80, S], F32)
            ig = A("qig", [80, S], F32)
            Aa = A("qaa", [80, S], F32)
            T1 = A("qt1", [80, S], F32)
            uu = A("quu", [80, S], F32)
            hs = A("qhs", [80, S], F32)
            gl = A("qgl", [80, S], F32)
            yo = [A(f"qyo{i}", [80, S], BF16) for i in range(2)]
            W = [A(f"qsw{i}", [128, 8, 512], BF16) for i in range(2)]
            wdt = A("qswdt", [128, 8, 32], BF16)
            pre = [A(f"qspre{i}", [128, 3 + S], F32) for i in range(2)]
            cv = [A(f"qscv{i}", [128, S], F32) for i in range(2)]
            ob = [A(f"qsob{i}", [128, S], BF16) for i in range(2)]
            scw, scb = PK('scw'), PK('scb')
            kb.dma(wr[:], lwr_d[l].rearrange("k i j -> i k j"), 'qwr', q='pool')
            kb.dma(wi_[:], lwi_d[l].rearrange("k i j -> i k j"), 'qwi', q='pool')
            lcw = PK('lcw', 80)
            lcb, lbr, lbi, llam = PK('lcb', 80), PK('lbr', 80), PK('lbi', 80), PK('llam', 80)
            act_(cneg[:], llam, AF.Exp, scale=-1.0)
            act_(cneg[:], cneg[:], AF.Ln, bias=EPSC[0:80, 1:2])
            ts_('dve', c2[:], cneg[:], -16.0, None, ALU.mult)
            ts_('dve', cneg[:], cneg[:], -8.0, None, ALU.mult)
            memset_('dve', xpre[:, 0:3], 0.0)
            for i in range(2):
                memset_('dve', pre[i][:, 0:3], 0.0)

            def lru_block(k):
                wx, wg_ = wlxk[k % 2], wlgk[k % 2]
                kb.dma(wx[:], winv(l)[:, :, OLX + k * 80:OLX + (k + 1) * 80], f'qwlx{k % 2}', q='pool')
                kb.dma(wg_[:], winv(l)[:, :, OLG + k * 80:OLG + (k + 1) * 80], f'qwlg{k % 2}', q='pool')
                for tt in range(4):
                    tsl = slice(tt * 512, (tt + 1) * 512)
                    p = P()
                    for kc in range(8):
                        mm(p[0:80, :], wx[:, kc, :], hT[:, kc, tsl], kc == 0, kc == 7)
                    copy_('act', xpre[:, 3 + tt * 512:3 + (tt + 1) * 512], p[0:80, :])
                ts_('dve', xc[:], xpre[:, 0:S], lcw[:, k * 4:k * 4 + 1], lcb[:, k:k + 1], ALU.mult, ALU.add)
                for j in range(1, 4):
                    stt_(xc[:], xpre[:, j:j + S], lcw[:, k * 4 + j:k * 4 + j + 1], xc[:], ALU.mult, ALU.add)
                copy_('pool', xcb[:], xc[:])
                for (wgt, bb, dst) in ((wr, lbr, rg), (wi_, lbi, ig)):
                    for tt in range(4):
                        tsl = slice(tt * 512, (tt + 1) * 512)
                        p = P()
                        mm(p[0:80, :], wgt[:, k, :], xcb[:, tsl])
                        act_(dst[:, tsl], p[0:80, :], AF.Sigmoid, bias=bb[:, k:k + 1])
                act_(Aa[:], rg[:], AF.Exp, scale=cneg[:, k:k + 1])
                act_(T1[:], rg[:], AF.Exp, scale=c2[:, k:k + 1])
                ts_('dve', T1[:], T1[:], -1.0, 1.0, ALU.mult, ALU.add)
                ts_('dve', T1[:], T1[:], 1e-12, None, ALU.max)
                act_(T1[:], T1[:], AF.Sqrt)
                tt_('pool', uu[:], ig[:], xc[:], ALU.mult)
                tt_('dve', uu[:], uu[:], T1[:], ALU.mult)
                V('dve', lambda: nc.vector.tensor_tensor_scan(out=hs[:], data0=Aa[:], data1=uu[:], initial=0.0,
                                                              op0=ALU.mult, op1=ALU.add), [hs[:]], [Aa[:], uu[:]])
                for tt in range(4):
                    tsl = slice(tt * 512, (tt + 1) * 512)
                    p = P()
                    for kc in range(8):
                        mm(p[0:80, :], wg_[:, kc, :], hT[:, kc, tsl], kc == 0, kc == 7)
                    act_(gl[:, tsl], p[0:80, :], AF.Gelu)
                Y = yo[k % 2]
                tt_('dve', Y[:], hs[:], gl[:], ALU.mult)
                kb.dma(yl_d[:, k, :], Y[:], f'qyo{k % 2}')

            def ssd_unit(u):
                if u < 32:
                    cg, j, fc = u // 4, u % 4, u
                    Wt = W[cg % 2]
                    if j == 0:
                        kb.dma(Wt[:], winv(l)[:, :, OXBC + cg * 512:OXBC + (cg + 1) * 512], f'qsw{cg % 2}', q='pool')
                    PR, CV, OB = pre[u % 2], cv[u % 2], ob[u % 2]
                    for tt in range(4):
                        p = P()
                        for kc in range(8):
                            mm(p[:, :], Wt[:, kc, j * 128:(j + 1) * 128], hT[:, kc, tt * 512:(tt + 1) * 512], kc == 0, kc == 7)
                        copy_('act', PR[:, 3 + tt * 512:3 + (tt + 1) * 512], p[:, :])
                    ts_('dve', CV[:], PR[:, 0:S], scw[:, fc * 4:fc * 4 + 1], None, ALU.mult)
                    for jj in range(1, 4):
                        stt_(CV[:], PR[:, jj:jj + S], scw[:, fc * 4 + jj:fc * 4 + jj + 1], CV[:], ALU.mult, ALU.add)
                    act_(OB[:], CV[:], AF.Silu, bias=scb[:, fc:fc + 1])
                    kb.dma(xbc_d[:, fc, :], OB[:], f'qsob{u % 2}')
                else:
                    v = u - 32
                    cg, j, fc = v // 4, v % 4, v
                    Wt = W[cg % 2]
                    if j == 0:
                        kb.dma(Wt[:], winv(l)[:, :, OZ + cg * 512:OZ + (cg + 1) * 512], f'qsw{cg % 2}', q='pool')
                    OB = ob[u % 2]
                    for tt in range(4):
                        p = P()
                        for kc in range(8):
                            mm(p[:, :], Wt[:, kc, j * 128:(j + 1) * 128], hT[:, kc, tt * 512:(tt + 1) * 512], kc == 0, kc == 7)
                        act_(OB[:, tt * 512:(tt + 1) * 512], p[:, :], AF.Silu)
                    kb.dma(z_d[:, fc, :], OB[:], f'qsob{u % 2}')

            u = 0
            for k in range(16):
                lru_block(k)
                for _ in range(3):
                    ssd_unit(u)
                    u += 1
            kb.dma(wdt[:], winv(l)[:, :, ODT:ODT + 32], 'qswdt', q='pool')
            for c in range(16):
                p = P()
                for kc in range(8):
                    mm(p[:, 0:32], hT[:, kc, c * 128:(c + 1) * 128], wdt[:, kc, :], kc == 0, kc == 7)
                tt_('dve', dtraw[:, c, :], p[:, 0:32], PK('dtb'), ALU.add)
            kb.barrier()

    def ssd_scan_phase(l):
        with ExitStack() as es:
            A = lambda n, sh, dt: es.enter_context(nc.sbuf_tensor(f"{n}_{l}", sh, dt))
            H = A("cH", [128, 32, 64], F32)
            Hb = A("cHb", [128, 32, 64], BF16)
            expA = A("cexpA", [128, 32], F32)
            xsT = [A(f"cxs{i}", [128, 16, 128], BF16) for i in range(2)]
            BT = [A(f"cbt{i}", [128, 8, 128], BF16) for i in range(2)]
            CT = [A(f"cct{i}", [128, 8, 128], BF16) for i in range(2)]
            zT = [A(f"czt{i}", [128, 16, 128], BF16) for i in range(2)]
            dt_ = A("cdt", [128, 32], F32)
            dtw = A("cdtw", [128, 32], F32)
            a_ = A("ca", [128, 32], F32)
            ex = A("cex", [128, 3, 32], F32)
            abig = A("cabig", [128, 32, 64], F32)
            xdt = A("cxdt", [128, 32, 64], BF16)
            xdtw = A("cxdtw", [128, 32, 64], BF16)
            Btok = A("cbtok", [128, 8, 128], BF16)
            ab = [A(f"cab{i}", [128, 4, 128], F32) for i in range(2)]
            CBm = [A(f"ccbm{i}", [128, 128], F32) for i in range(2)]
            Lm = [A(f"clm{i}", [128, 512], F32) for i in range(2)]
            Gm = [A(f"cgm{i}", [128, 4, 128], BF16) for i in range(2)]
            ds = [A(f"cds{i}", [128, 256], F32) for i in range(2)]
            t1 = [A(f"ct1{i}", [128, 256], F32) for i in range(2)]
            yT = A("cyT", [128, 16, 128], F32)
            yg = A("cyg", [128, 16, 128], F32)
            sq = A("csq", [128, 16, 128], F32)
            rs = A("crs", [128, 128], F32)
            yo = [A(f"cyo{i}", [128, 16, 128], BF16) for i in range(2)]
            sdd, sng = PK('sdd'), PK('sng')
            memset_('dve', H[:], 0.0)
            memset_('pool', Hb[:], 0.0)
            act_(expA[:], PK('alog'), AF.Exp)
            for c in range(16):
                csl = slice(c * 128, (c + 1) * 128)
                b = c % 2
                kb.dma(xsT[b][:], xbc_d[:, 0:16, csl], f'cxs{b}')
                kb.dma(BT[b][:], xbc_d[:, 16:24, csl], f'cbt{b}')
                kb.dma(CT[b][:], xbc_d[:, 24:32, csl], f'cct{b}')
                kb.dma(zT[b][:], z_d[:, :, csl], f'czt{b}')
                act_(dt_[:], dtraw[:, c, :], AF.Exp)
                act_(dt_[:], dt_[:], AF.Ln, bias=EPSC[:, 1:2])
                stt_(a_[:], dt_[:], -1.0, expA[:], ALU.mult, ALU.mult)
                p3 = P()
                mm(p3[:, 0:32], mle, a_[:])
                mm(p3[:, 32:64], mgt, a_[:])
                mm(p3[:, 64:96], onesf, a_[:])
                act_(ex[:], p3[:, 0:96].rearrange("p (k h) -> p k h", k=3), AF.Exp)
                tt_('dve', dtw[:], dt_[:], ex[:, 1, :], ALU.mult)
                copy_('pool', abig[:], a_[:, :][:, :, None].broadcast_to([128, 32, 64]))
                for bb in range(2):
                    pT = P()[:, :].bitcast(BF16)
                    for j in range(8):
                        tr(pT[:, j * 128:(j + 1) * 128], xsT[b][:, bb * 8 + j, :], identb)
                    pv = pT.rearrange("p (h d) -> p h d", h=16)
                    hsl = slice(bb * 16, (bb + 1) * 16)
                    tt_('dve', xdt[:, hsl, :], pv, dt_[:, hsl][:, :, None].broadcast_to([128, 16, 64]), ALU.mult)
                    tt_('dve', xdtw[:, hsl, :], pv, dtw[:, hsl][:, :, None].broadcast_to([128, 16, 64]), ALU.mult)
                pB = P()[:, :].bitcast(BF16)
                for g in range(8):
                    tr(pB[:, g * 128:(g + 1) * 128], BT[b][:, g, :], identb)
                copy_('act', Btok[:], pB.rearrange("p (g n) -> p g n", g=8))
                def stage_a(g):
                    i2 = g % 2
                    pcb = P()
                    mm(pcb[:, 0:128], BT[b][:, g, :], CT[b][:, g, :])
                    tt_('dve', CBm[i2][:], pcb[:, 0:128], mle, ALU.mult)
                    tt_('pool', ab[i2][:], mgt[:, None, :].broadcast_to([128, 4, 128]),
                        a_[:, 4 * g:4 * g + 4][:, :, None].broadcast_to([128, 4, 128]), ALU.mult)
                    pD = P()
                    for h in range(4):
                        mm(pD[:, h * 128:(h + 1) * 128], ab[i2][:, h, :], mle)
                    act_(Lm[i2][:], pD[:, :], AF.Exp)
                    tt_('dve', Gm[i2][:], Lm[i2][:, :].rearrange("p (h l) -> p h l", h=4),
                        CBm[i2][:, None, :].broadcast_to([128, 4, 128]), ALU.mult)
                    pS = P()
                    for hp in range(2):
                        h0 = 4 * g + 2 * hp
                        mm(pS[:, hp * 128:(hp + 1) * 128], abig[:, h0:h0 + 2, :].rearrange("p h d -> p (h d)"), mle)
                    act_(ds[i2][:], pS[:, 0:256], AF.Exp)

                def stage_b(g):
                    i2 = g % 2
                    pY = P()
                    for hp in range(2):
                        for hh in range(2):
                            h = 2 * hp + hh
                            mm(pY[hh * 64:(hh + 1) * 64, hp * 128:(hp + 1) * 128], xdt[:, 4 * g + h, :], Gm[i2][:, h, :])
                    pZ = P()
                    for hp in range(2):
                        h0 = 4 * g + 2 * hp
                        mm(pZ[:, hp * 128:(hp + 1) * 128], Hb[:, h0:h0 + 2, :].rearrange("p h d -> p (h d)"), CT[b][:, g, :])
                    tt_('dve', t1[i2][:], pZ[:, 0:256], ds[i2][:], ALU.mult)
                    tt_('dve', yT[:, 2 * g:2 * g + 2, :], pY[:, 0:256].rearrange("p (f l) -> p f l", f=2),
                        t1[i2][:, :].rearrange("p (f l) -> p f l", f=2), ALU.add)

                stage_a(0)
                for g in range(8):
                    if g + 1 < 8:
                        stage_a(g + 1)
                    stage_b(g)
                for fc in range(16):
                    stt_(yT[:, fc, :], xsT[b][:, fc, :], sdd[:, fc:fc + 1], yT[:, fc, :], ALU.mult, ALU.add)
                tt_('dve', H[:], H[:], ex[:, 2, :][:, :, None].broadcast_to([128, 32, 64]), ALU.mult)
                for gp in range(4):
                    pst = P()
                    for gg in range(2):
                        g = 2 * gp + gg
                        mm(pst[:, gg * 256:(gg + 1) * 256], Btok[:, g, :],
                           xdtw[:, 4 * g:4 * g + 4, :].rearrange("p h d -> p (h d)"))
                    tt_('dve', H[:, 8 * gp:8 * gp + 8, :], pst[:, :].rearrange("p (h d) -> p h d", h=8),
                        H[:, 8 * gp:8 * gp + 8, :], ALU.add)
                copy_('pool', Hb[:], H[:])
                tt_('dve', yg[:], yT[:], zT[b][:], ALU.mult)
                act_(sq[:], yg[:], AF.Square)
                pss = P()
                for fc in range(16):
                    mm(pss[:, 0:128], onesf, sq[:, fc, :], fc == 0, fc == 15)
                act_(rs[:], pss[:, 0:128], AF.Sqrt, bias=EPSC[:, 0:1], scale=1.0 / 2048)
                V('dve', lambda: nc.vector.reciprocal(out=rs[:], in_=rs[:]), [rs[:]], [rs[:]])
                tt_('dve', yg[:], yg[:], rs[:, None, :].broadcast_to([128, 16, 128]), ALU.mult)
                tt_('pool', yo[b][:], yg[:], sng[:, :, None].broadcast_to([128, 16, 128]), ALU.mult)
                kb.dma(ys_d[:, :, csl], yo[b][:], f'cyo{b}')
            kb.barrier()

    def merge_phase(l):
        with ExitStack() as es0:
            mT = es0.enter_context(nc.sbuf_tensor(f"gmT_{l}", [128, 8, S], BF16))
            with ExitStack() as es:
                A = lambda n, sh, dt: es.enter_context(nc.sbuf_tensor(f"{n}_{l}", sh, dt))
                wa = A("gwa", [64, 8, 512], BF16)
                ws = A("gws", [128, 16, 512], BF16)
                wl = A("gwl", [80, 16, 512], BF16)
                wm = A("gwm", [128, 3, 8, 512], BF16)
                ya = A("gya", [64, 8, 512], BF16)
                ys = A("gys", [128, 16, 512], BF16)
                yl = A("gyl", [80, 16, 512], BF16)
                sgt = [A(f"gsg{i}", [128, 512], F32) for i in range(2)]
                mac = [A(f"gmac{i}", [128, 512], F32) for i in range(2)]
                tmp = [A(f"gtmp{i}", [128, 512], F32) for i in range(2)]
                n = 0
                for half in range(2):
                    hsl = slice(half * 512, (half + 1) * 512)
                    kb.dma(wa[:], wba_d[l].rearrange("(h d) c -> d h c", d=64)[:, :, hsl], 'gwa', q='pool')
                    for k0 in range(0, 16, 8):
                        kb.dma(ws[:, k0:k0 + 8, :], wbs_d[l].rearrange("(k p) c -> p k c", p=128)[:, k0:k0 + 8, hsl], 'gws', q='pool')
                        kb.dma(wl[:, k0:k0 + 8, :], wbl_d[l].rearrange("(k p) c -> p k c", p=80)[:, k0:k0 + 8, hsl], 'gwl', q='pool')
                    for b in range(3):
                        c0 = OMG + b * 1024 + half * 512
                        kb.dma(wm[:, b, :, :], winv(l)[:, :, c0:c0 + 512], 'gwm', q='pool')
                    for tt in range(4):
                        tsl = slice(tt * 512, (tt + 1) * 512)
                        kb.dma(ya[:], ya_d[:, :, tsl], 'gya')
                        kb.dma(ys[:], ys_d[:, :, tsl], 'gys')
                        kb.dma(yl[:], yl_d[:, :, tsl], 'gyl')
                        for oc in range(4):
                            osl = slice(oc * 128, (oc + 1) * 128)
                            M = mac[n % 2]
                            n += 1
                            for b, (y, w, nk) in enumerate(((ya, wa, 8), (ys, ws, 16), (yl, wl, 16))):
                                pP = P()
                                for k in range(nk):
                                    mm(pP[:, :], w[:, k, osl], y[:, k, :], k == 0, k == nk - 1)
                                pG = P()
                                for kc in range(8):
                                    mm(pG[:, :], wm[:, b, kc, osl], hT[:, kc, tsl], kc == 0, kc == 7)
                                SG = sgt[b % 2]
                                act_(SG[:], pG[:, :], AF.Sigmoid)
                                if b == 0:
                                    tt_('dve', M[:], SG[:], pP[:, :], ALU.mult)
                                else:
                                    T = tmp[b % 2]
                                    tt_('dve', T[:], SG[:], pP[:, :], ALU.mult)
                                    tt_('pool', M[:], M[:], T[:], ALU.add)
                            copy_('pool', mT[:, half * 4 + oc, tsl], M[:])
                kb.barrier()
            with ExitStack() as es:
                A = lambda n, sh, dt: es.enter_context(nc.sbuf_tensor(f"{n}_{l}", sh, dt))
                wo = A("gwo", [128, 8, 1024], BF16)
                X = [A(f"gX{i}", [128, 8, 512], F32) for i in range(2)]
                for c0 in range(0, 1024, 512):
                    kb.dma(wo[:, :, c0:c0 + 512], wo_d[l].rearrange("(kc p) c -> p kc c", p=128)[:, :, c0:c0 + 512], 'gwo', q='pool')
                for tt in range(4):
                    tsl = slice(tt * 512, (tt + 1) * 512)
                    Xt = X[tt % 2]
                    kb.dma(Xt[:], xs_d[:, :, tsl], f'gX{tt % 2}')
                    for oc in range(8):
                        p = P()
                        for kc in range(8):
                            mm(p[:, :], wo[:, kc, oc * 128:(oc + 1) * 128], mT[:, kc, tsl], kc == 0, kc == 7)
                        stt_(Xt[:, oc, :], p[:, :], cond[:, 16 + oc:17 + oc], Xt[:, oc, :], ALU.mult, ALU.add)
                    kb.dma(xs_d[:, :, tsl], Xt[:], f'gX{tt % 2}')
                kb.barrier()

    def moe_phase(l):
        with ExitStack() as es:
            A = lambda n, sh, dt: es.enter_context(nc.sbuf_tensor(f"{n}_{l}", sh, dt))
            yacc = A("myacc", [128, 16, 1024], F32)
            wt = A("mwt", [128, 16, 32], F32)
            wrt = A("mwrt", [128, 8, 36], BF16)
            lg = A("mlg", [128, 36], F32)
            sm = A("msm", [128, 16], F32)
            oh = A("moh", [128, 4], F32)
            pen = A("mpen", [128, 4], F32)
            eg4 = A("meg4", [128, 4], F32)
            lem = A("mlem", [128, 4, 8], F32)
            m8 = A("mm8", [128, 8], F32)
            eq1 = A("meq1", [128, 32], F32)
            eq2 = A("meq2", [128, 32], F32)
            actb = [A(f"mact{i}", [128, 4, S], BF16) for i in range(2)]
            wg = [A(f"mwg{i}", [128, 8, 512], BF16) for i in range(2)]
            wu = [A(f"mwu{i}", [128, 8, 512], BF16) for i in range(2)]
            wd = [A(f"mwd{i}", [128, 4, 1024], BF16) for i in range(2)]
            sgt = [A(f"msg{i}", [128, 512], F32) for i in range(2)]
            X = [A(f"mX{i}", [128, 8, 128], F32) for i in range(2)]
            kb.dma(wrt[:, :, 0:4], rwg_d[l].rearrange("(kc p) g -> p kc g", p=128), 'mwrt', q='pool')
            kb.dma(wrt[:, :, 4:36], rwe_d[l].rearrange("(kc p) g -> p kc g", p=128), 'mwrt', q='pool')
            rb = PK('rb')
            for t in range(16):
                tk = slice(t * 128, (t + 1) * 128)
                p = P()
                for kc in range(8):
                    mm(p[:, 0:36], hT[:, kc, tk], wrt[:, kc, :], kc == 0, kc == 7)
                tt_('dve', lg[:], p[:, 0:36], rb, ALU.add)
                c = lambda i: sm[:, i:i + 1]
                V('dve', lambda: nc.vector.reduce_max(out=c(0), in_=lg[:, 0:4], axis=AX.X), [c(0)], [lg[:, 0:4]])
                ts_('dve', oh[:], lg[:, 0:4], c(0), None, ALU.is_ge)
                ts_('dve', c(1), c(0), -1.0, None, ALU.mult)
                act_(eg4[:], lg[:, 0:4], AF.Exp, bias=c(1))
                V('dve', lambda: nc.vector.reduce_sum(out=c(2), in_=eg4[:], axis=AX.X), [c(2)], [eg4[:]])
                V('dve', lambda: nc.vector.reciprocal(out=c(3), in_=c(2)), [c(3)], [c(2)])
                ts_('dve', pen[:], oh[:], -1.0, 30000.0, ALU.add, ALU.mult)
                tt_('dve', lem[:], lg[:, 4:36].rearrange("p (g e) -> p g e", g=4),
                    oh[:, :][:, :, None].broadcast_to([128, 4, 8]), ALU.mult)
                tt_('dve', lem[:], lem[:], pen[:, :][:, :, None].broadcast_to([128, 4, 8]), ALU.add)
                lemf = lem[:, :, :].rearrange("p g e -> p (g e)")
                V('dve', lambda: nc.vector.max(out=m8[:], in_=lemf), [m8[:]], [lemf])
                ts_('dve', eq1[:], lemf, m8[:, 0:1], None, ALU.is_ge)
                ts_('dve', eq2[:], lemf, m8[:, 1:2], None, ALU.is_ge)
                tt_('dve', c(4), m8[:, 1:2], m8[:, 0:1], ALU.subtract)
                act_(c(5), c(4), AF.Exp)
                ts_('dve', c(6), c(5), 1.0, None, ALU.add)
                V('dve', lambda: nc.vector.reciprocal(out=c(7), in_=c(6)), [c(7)], [c(6)])
                tt_('dve', c(8), c(3), c(7), ALU.mult)
                tt_('dve', c(9), c(8), c(5), ALU.mult)
                tt_('dve', c(10), c(8), c(9), ALU.subtract)
                ts_('dve', wt[:, t, :], eq2[:], c(9), None, ALU.mult)
                stt_(wt[:, t, :], eq1[:], c(10), wt[:, t, :], ALU.mult, ALU.add)
            for e in range(NEXP):
                b = e % 2
                kb.dma(wg[b][:], eg_d[l, e].rearrange("(kc p) f -> p kc f", p=128), f'mwg{b}', q='pool')
                kb.dma(wu[b][:], eu_d[l, e].rearrange("(kc p) f -> p kc f", p=128), f'mwu{b}', q='pool')
                for c0 in range(0, 1024, 512):
                    kb.dma(wd[b][:, :, c0:c0 + 512], ed_d[l, e].rearrange("(k p) c -> p k c", p=128)[:, :, c0:c0 + 512], f'mwd{b}', q='pool')
                n = 0
                for fcn in range(4):
                    fsl = slice(fcn * 128, (fcn + 1) * 128)
                    for tt in range(4):
                        tsl = slice(tt * 512, (tt + 1) * 512)
                        pg = P()
                        for kc in range(8):
                            mm(pg[:, :], wg[b][:, kc, fsl], hT[:, kc, tsl], kc == 0, kc == 7)
                        pu = P()
                        for kc in range(8):
                            mm(pu[:, :], wu[b][:, kc, fsl], hT[:, kc, tsl], kc == 0, kc == 7)
                        SG = sgt[n % 2]
                        n += 1
                        act_(SG[:], pg[:, :], AF.Silu)
                        tt_('dve', actb[b][:, fcn, tsl], SG[:], pu[:, :], ALU.mult)
                for t in range(16):
                    tk = slice(t * 128, (t + 1) * 128)
                    for half in range(2):
                        hsl = slice(half * 512, (half + 1) * 512)
                        pd = P()
                        for k in range(4):
                            mm(pd[:, :], actb[b][:, k, tk], wd[b][:, k, hsl], k == 0, k == 3)
                        if e == 0:
                            ts_('dve', yacc[:, t, hsl], pd[:, :], wt[:, t, e:e + 1], None, ALU.mult)
                        else:
                            stt_(yacc[:, t, hsl], pd[:, :], wt[:, t, e:e + 1], yacc[:, t, hsl], ALU.mult, ALU.add)
            for t in range(16):
                tk = slice(t * 128, (t + 1) * 128)
                Xt = X[t % 2]
                kb.dma(Xt[:], xs_d[:, :, tk], f'mX{t % 2}')
                for half in range(2):
                    p = P()
                    for j in range(4):
                        kc = half * 4 + j
                        tr(p[:, j * 128:(j + 1) * 128], yacc[:, t, kc * 128:(kc + 1) * 128], identf)
                    for j in range(4):
                        kc = half * 4 + j
                        stt_(Xt[:, kc, :], p[:, j * 128:(j + 1) * 128], cond[:, 40 + kc:41 + kc], Xt[:, kc, :], ALU.mult, ALU.add)
                kb.dma(xs_d[:, :, tk], Xt[:], f'mX{t % 2}')
            kb.barrier()

    def final_phase():
        with ExitStack() as es:
            A = lambda n, sh, dt: es.enter_context(nc.sbuf_tensor(n, sh, dt))
            xt = [A(f"fx{i}", [128, 8, 512], F32) for i in range(2)]
            sq = [A(f"fsq{i}", [128, 512], F32) for i in range(2)]
            rs = A("frs", [128, 512], F32)
            yf = A("fyf", [128, 8, 512], F32)
            ot = [A(f"fot{i}", [128, 1024], F32) for i in range(2)]
            fg = G('fg')
            n = 0
            for tt in range(4):
                Xt = xt[tt % 2]
                kb.dma(Xt[:], xs_d[:, :, tt * 512:(tt + 1) * 512], f'fx{tt % 2}')
                pss = P()
                for kc in range(8):
                    act_(sq[kc % 2][:], Xt[:, kc, :], AF.Square)
                    mm(pss[:, :], onesf, sq[kc % 2][:], kc == 0, kc == 7)
                act_(rs[:], pss[:, :], AF.Sqrt, bias=EPSC[:, 0:1], scale=1.0 / D)
                V('dve', lambda: nc.vector.reciprocal(out=rs[:], in_=rs[:]), [rs[:]], [rs[:]])
                for kc in range(8):
                    stt_(yf[:, kc, :], Xt[:, kc, :], fg[:, kc:kc + 1], rs[:], ALU.mult, ALU.mult)
                for t4 in range(4):
                    O = ot[n % 2]
                    n += 1
                    for half in range(2):
                        p = P()
                        for j in range(4):
                            kc = half * 4 + j
                            tr(p[:, j * 128:(j + 1) * 128], yf[:, kc, t4 * 128:(t4 + 1) * 128], identf)
                        copy_('act' if half else 'dve', O[:, half * 512:(half + 1) * 512], p[:, :])
                    r0 = tt * 512 + t4 * 128
                    kb.dma(out_d[r0:r0 + 128, :], O[:], f'fot{(n - 1) % 2}')
            kb.barrier()

    ph = phases
    load_x_phase()
    if ph is None or 'rope' in ph:
        rope_phase()
    for l in range(L):
        if ph is None or 'cond' in ph:
            cond_phase(l)
        if ph is None or 'norm1' in ph:
            norm_phase(l, 0, f"a{l}")
        if ph is None or 'attn' in ph:
            attn_phase(l)
        if ph is None or 'lru' in ph or 'ssd' in ph:
            lru_ssdproj_phase(l)
        if ph is None or 'ssd' in ph:
            ssd_scan_phase(l)
        if ph is None or 'merge' in ph:
            merge_phase(l)
        if ph is None or 'moe' in ph:
            norm_phase(l, 1, f"b{l}")
            moe_phase(l)
    if ph is None or 'final' in ph:
        final_phase()
    kb.finish()
    return nc, kb


_CACHE = {}


def _in_maps(inp, L, cores):
    pk = np.stack([_pack_layer(inp, l) for l in range(L)])
    maps = []
    shared = {
        "pk": pk,
        "ada_w": inp['ada_w'][:L], "w_in": inp['w_in'][:L],
        "lru_w_r": inp['lru_w_r'][:L], "lru_w_i": inp['lru_w_i'][:L],
        "w_br_attn": inp['w_br_attn'][:L], "w_br_ssd": inp['w_br_ssd'][:L], "w_br_lru": inp['w_br_lru'][:L],
        "w_out": inp['w_out'][:L], "router_wg": inp['router_wg'][:L],
        "router_we": inp['router_we'][:L].reshape(L, D, 32),
        "exp_w_gate": inp['exp_w_gate'][:L], "exp_w_up": inp['exp_w_up'][:L], "exp_w_down": inp['exp_w_down'][:L],
    }
    for b in cores:
        m = dict(shared)
        m["x"] = np.ascontiguousarray(inp['x'][b])
        m["pos"] = np.ascontiguousarray(np.broadcast_to(inp['positions'][b][None, :], (128, S))).astype(np.int32)
        m["gk"] = _pack_global(inp['c'][b], inp['final_g'])
        maps.append(m)
    return maps


def kernel(**inputs):
    inp = {k: np.asarray(v) for k, v in inputs.items()}
    if 'nc' not in _CACHE:
        _CACHE['nc'] = build(DEPTH)[0]
    nc = _CACHE['nc']
    maps = _in_maps(inp, DEPTH, list(range(8)))
    res = run_bass_kernel_spmd(nc, maps, core_ids=list(range(8)))
    out = np.stack([np.asarray(res.results[c]["out"]) for c in range(8)], axis=0)
    return out.astype(np.float32)
```
